# Optimizing a Trainium2 kernel written in Bass

```python
import jax, jax.numpy as jnp
from jax import lax
import numpy as np

D_MODEL = 4096
BATCH = 4
SEQ = 4096
DEPTH = 4

CTX_LEN = 256
GRID_W = 64
HEAD_DIM = 128
N_HEADS_TOTAL = D_MODEL // HEAD_DIM
N_HEADS_NA = N_HEADS_TOTAL // 2
N_HEADS_WIN = N_HEADS_TOTAL - N_HEADS_NA
N_KV_WIN = max(1, N_HEADS_WIN // 4)
NA_ROWS = 8
NA_COLS = 16
WINDOW = 128
WIN_BLOCK = 128
ROPE_BASE = 10000.0
MLSTM_HEADS = 8
MLSTM_V = D_MODEL // MLSTM_HEADS
MLSTM_QK = MLSTM_V // 2
MLSTM_CHUNK = 64
N_EXPERTS = 16
EXPERT_FF = D_MODEL // 16
CAPACITY_FACTOR = 2
ADA_MODS = 6
NORM_EPS = 1e-6
NEG_INF = -1e30
N_EVEN = (DEPTH + 1) // 2
N_ODD = DEPTH // 2
NA_W = N_HEADS_NA * HEAD_DIM
WIN_Q_W = N_HEADS_WIN * HEAD_DIM
WIN_KV_W = N_KV_WIN * HEAD_DIM
AB_IN = 3 * NA_W + WIN_Q_W + 2 * WIN_KV_W
AB_OUT = NA_W + WIN_Q_W
MQK_W = MLSTM_HEADS * MLSTM_QK
MV_W = MLSTM_HEADS * MLSTM_V
N_GATES = 4 * MLSTM_HEADS
ML_IN = 2 * MQK_W + 2 * MV_W + N_GATES

kernel_name = 'hybrid_na_swa_mlstm_ecmoe_dit'


def rmsnorm(x, w):
    xf = x.astype(jnp.float32)
    y = xf * lax.rsqrt(jnp.mean(xf * xf, axis=-1, keepdims=True) + NORM_EPS)
    return (y * w.astype(jnp.float32)).astype(x.dtype)


def modulate(h, shift, scale):
    return h * (1 + scale) + shift


def to_heads(x, n_heads):
    b, t, w = x.shape
    return x.reshape(b, t, n_heads, w // n_heads).transpose(0, 2, 1, 3)


def from_heads(x):
    b, h, t, d = x.shape
    return x.transpose(0, 2, 1, 3).reshape(b, t, h * d)


def axial_rope(x):
    n, dh = x.shape[2], x.shape[3]
    half = dh // 2
    nf = half // 2
    t = jnp.arange(n)
    inv = ROPE_BASE ** (-jnp.arange(nf, dtype=jnp.float32) / nf)
    ang_r = (t // GRID_W).astype(jnp.float32)[:, None] * inv
    ang_c = (t % GRID_W).astype(jnp.float32)[:, None] * inv

    def rot(u, ang):
        u1, u2 = u[..., :nf], u[..., nf:]
        cos, sin = jnp.cos(ang), jnp.sin(ang)
        return jnp.concatenate([u1 * cos - u2 * sin, u2 * cos + u1 * sin], axis=-1)

    xf = x.astype(jnp.float32)
    return jnp.concatenate([rot(xf[..., :half], ang_r), rot(xf[..., half:], ang_c)], axis=-1).astype(x.dtype)


def dense_ctx_attn(q, k, v, sink):
    b, hq, lq, dh = q.shape
    hkv = k.shape[1]
    g = hq // hkv
    qg = q.reshape(b, hkv, g, lq, dh) * (dh ** -0.5)
    s = jnp.einsum('bkgqd,bkcd->bkgqc', qg, k).astype(jnp.float32)
    if sink is not None:
        sk = jnp.broadcast_to(sink.astype(jnp.float32).reshape(1, hkv, g, 1, 1), s.shape[:-1] + (1,))
        s = jnp.concatenate([s, sk], axis=-1)
    p = jax.nn.softmax(s, axis=-1)[..., :k.shape[2]]
    o = jnp.einsum('bkgqc,bkcd->bkgqd', p.astype(v.dtype), v)
    return o.reshape(b, hq, lq, dh)


def neighbourhood_attn(q, k, v, kc, vc, rpb):
    b, h, n, dh = q.shape
    rows = n // GRID_W
    kh, kw = min(NA_ROWS, rows), NA_COLS
    nk = kh * kw
    q = q * (dh ** -0.5)
    kg = k.reshape(b, h, rows, GRID_W, dh)
    vg = v.reshape(b, h, rows, GRID_W, dh)
    cols = jnp.arange(GRID_W)
    col_idx = jnp.clip(cols - kw // 2, 0, GRID_W - kw)[:, None] + jnp.arange(kw)[None, :]
    dc = col_idx - cols[:, None] + (NA_COLS - 1)
    row_ids = jnp.arange(rows)
    row_start = jnp.clip(row_ids - kh // 2, 0, rows - kh)

    def row_block(args):
        q_r, r, rs = args
        k_nb = lax.dynamic_slice_in_dim(kg, rs, kh, axis=2)[:, :, :, col_idx]
        v_nb = lax.dynamic_slice_in_dim(vg, rs, kh, axis=2)[:, :, :, col_idx]
        dr = rs + jnp.arange(kh) - r + (NA_ROWS - 1)
        bias = rpb[:, dr[:, None, None], dc[None]].transpose(0, 2, 1, 3)
        s_nb = (jnp.einsum('bhqd,bhiqjd->bhqij', q_r, k_nb) + bias[None]).reshape(b, h, GRID_W, nk)
        s_c = jnp.einsum('bhqd,bhcd->bhqc', q_r, kc)
        p = jax.nn.softmax(jnp.concatenate([s_nb, s_c], axis=-1).astype(jnp.float32), axis=-1).astype(v.dtype)
        p_nb = p[..., :nk].reshape(b, h, GRID_W, kh, kw)
        return jnp.einsum('bhqij,bhiqjd->bhqd', p_nb, v_nb) + jnp.einsum('bhqc,bhcd->bhqd', p[..., nk:], vc)

    q_rows = jnp.moveaxis(q.reshape(b, h, rows, GRID_W, dh), 2, 0)
    out = lax.map(row_block, (q_rows, row_ids, row_start))
    return jnp.moveaxis(out, 0, 2).reshape(b, h, n, dh)


def window_gqa_sink(q, k, v, kc, vc, sink):
    b, hq, n, dh = q.shape
    hkv = k.shape[1]
    g = hq // hkv
    nb = n // WIN_BLOCK
    span = WIN_BLOCK + 2 * WINDOW
    qb = (q * (dh ** -0.5)).reshape(b, hkv, g, nb, WIN_BLOCK, dh)
    pad = ((0, 0), (0, 0), (WINDOW, WINDOW), (0, 0))
    key_idx = jnp.arange(nb)[:, None] * WIN_BLOCK + jnp.arange(span)[None, :]
    kb = jnp.pad(k, pad)[:, :, key_idx]
    vb = jnp.pad(v, pad)[:, :, key_idx]
    qpos = jnp.arange(nb)[:, None] * WIN_BLOCK + jnp.arange(WIN_BLOCK)[None, :]
    kpos = (key_idx - WINDOW)[:, None, :]
    valid = (jnp.abs(qpos[:, :, None] - kpos) <= WINDOW) & (kpos >= 0) & (kpos < n)
    s_w = jnp.where(valid, jnp.einsum('bkgnqd,bknjd->bkgnqj', qb, kb).astype(jnp.float32), NEG_INF)
    s_c = jnp.einsum('bkgnqd,bkcd->bkgnqc', qb, kc).astype(jnp.float32)
    s_sink = jnp.broadcast_to(sink.astype(jnp.float32).reshape(1, hkv, g, 1, 1, 1), s_c.shape[:-1] + (1,))
    p = jax.nn.softmax(jnp.concatenate([s_w, s_c, s_sink], axis=-1), axis=-1).astype(v.dtype)
    lc = kc.shape[2]
    o = (jnp.einsum('bkgnqj,bknjd->bkgnqd', p[..., :span], vb)
         + jnp.einsum('bkgnqc,bkcd->bkgnqd', p[..., span:span + lc], vc))
    return o.reshape(b, hq, n, dh)


def mixer_ab(hc, hl, w_in, w_out, rpb, sink, need_ctx):
    splits = [NA_W, 2 * NA_W, 3 * NA_W, 3 * NA_W + WIN_Q_W, 3 * NA_W + WIN_Q_W + WIN_KV_W]

    def project(h):
        qa, ka, va, qb, kb, vb = jnp.split(h @ w_in, splits, axis=-1)
        return (to_heads(qa, N_HEADS_NA), to_heads(ka, N_HEADS_NA), to_heads(va, N_HEADS_NA),
                to_heads(qb, N_HEADS_WIN), to_heads(kb, N_KV_WIN), to_heads(vb, N_KV_WIN))

    qa_c, ka_c, va_c, qb_c, kb_c, vb_c = project(hc)
    qa_l, ka_l, va_l, qb_l, kb_l, vb_l = project(hl)
    out_a = neighbourhood_attn(qa_l, ka_l, va_l, ka_c, va_c, rpb)
    out_b = window_gqa_sink(axial_rope(qb_l), axial_rope(kb_l), vb_l, kb_c, vb_c, sink)
    yl = jnp.concatenate([from_heads(out_a), from_heads(out_b)], axis=-1) @ w_out
    yc = None
    if need_ctx:
        oa_c = dense_ctx_attn(qa_c, ka_c, va_c, None)
        ob_c = dense_ctx_attn(qb_c, kb_c, vb_c, sink)
        yc = jnp.concatenate([from_heads(oa_c), from_heads(ob_c)], axis=-1) @ w_out
    return yc, yl


def mlstm_scan(q, k, v, i_pre, f_pre, state, return_h):
    b, h, t, dv = v.shape
    nc = t // MLSTM_CHUNK

    def chunks(a):
        return jnp.moveaxis(a.reshape(a.shape[:2] + (nc, MLSTM_CHUNK) + a.shape[3:]), 2, 0)

    bcum = jnp.cumsum(chunks(jax.nn.log_sigmoid(f_pre)), axis=-1)
    tril = jnp.tril(jnp.ones((MLSTM_CHUNK, MLSTM_CHUNK), dtype=bool))

    def step(carry, xs):
        cmat, nvec, m = carry
        q_c, k_c, v_c, i_c, b_c = xs
        qf, kf, vf = q_c.astype(jnp.float32), k_c.astype(jnp.float32), v_c.astype(jnp.float32)
        g = b_c[..., -1]
        a = g[..., None] - b_c + i_c
        m_new = jnp.maximum(g + m, jnp.max(a, axis=-1))
        wk = jnp.exp(a - m_new[..., None])
        decay = jnp.exp(g + m - m_new)
        c_new = decay[..., None, None] * cmat + jnp.einsum('bhsd,bhse->bhde', kf * wk[..., None], vf)
        n_new = decay[..., None] * nvec + jnp.einsum('bhsd,bhs->bhd', kf, wk)
        if not return_h:
            return (c_new, n_new, m_new), None
        dmat = jnp.where(tril, b_c[..., :, None] - b_c[..., None, :] + i_c[..., None, :], -jnp.inf)
        m_inter = b_c + m[..., None]
        m_t = jnp.maximum(m_inter, jnp.max(dmat, axis=-1))
        w_inter = jnp.exp(m_inter - m_t)
        pw = jnp.exp(dmat - m_t[..., None]) * jnp.einsum('bhtd,bhsd->bhts', qf, kf)
        num = w_inter[..., None] * jnp.einsum('bhtd,bhde->bhte', qf, cmat) + jnp.einsum('bhts,bhse->bhte', pw, vf)
        den = w_inter * jnp.einsum('bhtd,bhd->bht', qf, nvec) + jnp.sum(pw, axis=-1)
        hc = num / jnp.maximum(jnp.abs(den), jnp.exp(-m_t))[..., None]
        return (c_new, n_new, m_new), hc

    final, hs = lax.scan(step, state, (chunks(q), chunks(k), chunks(v), chunks(i_pre), bcum))
    hout = None
    if return_h:
        hout = jnp.moveaxis(hs, 0, 2).reshape(b, h, t, dv).astype(v.dtype)
    return hout, final


def mlstm_out(hsum, o, norm_w, w_out):
    b, nh, t, dv = hsum.shape
    hf = jnp.swapaxes(hsum, 1, 2).astype(jnp.float32)
    hf = hf * lax.rsqrt(jnp.mean(hf * hf, axis=-1, keepdims=True) + NORM_EPS)
    hn = (hf.reshape(b, t, nh * dv) * norm_w.astype(jnp.float32)).astype(o.dtype)
    return (hn * jax.nn.sigmoid(o)) @ w_out


def mixer_c(hc, hl, w_in, b_gates, norm_w, w_out, need_ctx):
    splits = [MQK_W, 2 * MQK_W, 2 * MQK_W + MV_W, 2 * MQK_W + 2 * MV_W]

    def project(h):
        q, k, v, o, gt = jnp.split(h @ w_in, splits, axis=-1)
        b, t, _ = gt.shape
        gt = (gt + b_gates).astype(jnp.float32).reshape(b, t, 4, MLSTM_HEADS).transpose(2, 0, 3, 1)
        return (to_heads(q, MLSTM_HEADS) * (MLSTM_QK ** -0.5), to_heads(k, MLSTM_HEADS),
                to_heads(v, MLSTM_HEADS), o, gt)

    qc, kc, vc, oc, gc = project(hc)
    ql, kl, vl, ol, gl = project(hl)
    b = hl.shape[0]
    state0 = (jnp.zeros((b, MLSTM_HEADS, MLSTM_QK, MLSTM_V), jnp.float32),
              jnp.zeros((b, MLSTM_HEADS, MLSTM_QK), jnp.float32),
              jnp.zeros((b, MLSTM_HEADS), jnp.float32))
    flip = lambda a: jnp.flip(a, axis=2)
    hc_f, st_f = mlstm_scan(qc, kc, vc, gc[0], gc[1], state0, need_ctx)
    hl_f, _ = mlstm_scan(ql, kl, vl, gl[0], gl[1], st_f, True)
    hc_b, st_b = mlstm_scan(flip(qc), flip(kc), flip(vc), flip(gc[2]), flip(gc[3]), state0, need_ctx)
    hl_b, _ = mlstm_scan(flip(ql), flip(kl), flip(vl), flip(gl[2]), flip(gl[3]), st_b, True)
    yl = mlstm_out(hl_f + flip(hl_b), ol, norm_w, w_out)
    yc = None
    if need_ctx:
        yc = mlstm_out(hc_f + flip(hc_b), oc, norm_w, w_out)
    return yc, yl


def ec_moe(h, w_router, w1, w3, w2):
    b, t, d = h.shape
    cap = (CAPACITY_FACTOR * t) // N_EXPERTS
    aff = jax.nn.softmax((h @ w_router).astype(jnp.float32), axis=-1)
    gate, idx = lax.top_k(jnp.swapaxes(aff, 1, 2), cap)
    xs = jax.vmap(lambda hb, ib: hb[ib])(h, idx)
    a = jnp.einsum('becd,edf->becf', xs, w1)
    u = jnp.einsum('becd,edf->becf', xs, w3)
    y = jnp.einsum('becf,efd->becd', jax.nn.silu(a) * u, w2)
    y = (y * gate[..., None]).astype(h.dtype)
    return jax.vmap(lambda ib, yb: jnp.zeros((t, d), h.dtype).at[ib.reshape(-1)].add(yb.reshape(-1, d)))(idx, y)


def setup_inputs(seed: int = 0) -> dict:
    key = jax.random.key(seed)
    ks = jax.random.split(key, 24)

    def nrm(k, shape, scale):
        return jax.random.normal(k, shape, jnp.float32) * scale

    gate_base = jnp.repeat(jnp.array([0.0, 3.0, 0.0, 3.0], jnp.float32), MLSTM_HEADS)
    return {
        'x': nrm(ks[0], (BATCH, SEQ, D_MODEL), 1.0),
        'c': nrm(ks[1], (BATCH, D_MODEL), 1.0),
        'ctx': nrm(ks[2], (BATCH, CTX_LEN, D_MODEL), 1.0),
        'c_ctx': nrm(ks[3], (D_MODEL,), 1.0),
        'ada_w': nrm(ks[4], (DEPTH, D_MODEL, ADA_MODS * D_MODEL), 0.3 * D_MODEL ** -0.5),
        'ada_b': nrm(ks[5], (DEPTH, ADA_MODS * D_MODEL), 0.02),
        'norm1_w': 1.0 + nrm(ks[6], (DEPTH, D_MODEL), 0.1),
        'norm2_w': 1.0 + nrm(ks[7], (DEPTH, D_MODEL), 0.1),
        'ab_w_in': nrm(ks[8], (N_EVEN, D_MODEL, AB_IN), D_MODEL ** -0.5),
        'ab_w_out': nrm(ks[9], (N_EVEN, AB_OUT, D_MODEL), AB_OUT ** -0.5),
        'na_rpb': nrm(ks[10], (N_EVEN, N_HEADS_NA, 2 * NA_ROWS - 1, 2 * NA_COLS - 1), 0.5),
        'win_sink': nrm(ks[11], (N_EVEN, N_HEADS_WIN), 1.0),
        'ml_w_in': nrm(ks[12], (N_ODD, D_MODEL, ML_IN), D_MODEL ** -0.5),
        'ml_b_gates': gate_base + nrm(ks[13], (N_ODD, N_GATES), 0.5),
        'ml_norm_w': 1.0 + nrm(ks[14], (N_ODD, MV_W), 0.1),
        'ml_w_out': nrm(ks[15], (N_ODD, MV_W, D_MODEL), MV_W ** -0.5),
        'moe_router': nrm(ks[16], (DEPTH, D_MODEL, N_EXPERTS), D_MODEL ** -0.5),
        'moe_w1': nrm(ks[17], (DEPTH, N_EXPERTS, D_MODEL, EXPERT_FF), D_MODEL ** -0.5),
        'moe_w3': nrm(ks[18], (DEPTH, N_EXPERTS, D_MODEL, EXPERT_FF), D_MODEL ** -0.5),
        'moe_w2': nrm(ks[19], (DEPTH, N_EXPERTS, EXPERT_FF, D_MODEL), EXPERT_FF ** -0.5),
        'final_norm_w': 1.0 + nrm(ks[20], (D_MODEL,), 0.1),
    }


def reference(x, c, ctx, c_ctx, ada_w, ada_b, norm1_w, norm2_w, ab_w_in, ab_w_out, na_rpb, win_sink,
              ml_w_in, ml_b_gates, ml_norm_w, ml_w_out, moe_router, moe_w1, moe_w3, moe_w2, final_norm_w):
    xl, xc = x, ctx
    sc = jax.nn.silu(c)
    scc = jax.nn.silu(c_ctx)
    for l in range(DEPTH):
        need_ctx = l < DEPTH - 1
        mod_l = (sc @ ada_w[l] + ada_b[l])[:, None, :]
        mod_c = scc @ ada_w[l] + ada_b[l]
        sh1, sc1, g1, sh2, sc2, g2 = jnp.split(mod_l, ADA_MODS, axis=-1)
        ch1, cs1, cg1, ch2, cs2, cg2 = jnp.split(mod_c, ADA_MODS, axis=-1)
        hl = modulate(rmsnorm(xl, norm1_w[l]), sh1, sc1)
        hc = modulate(rmsnorm(xc, norm1_w[l]), ch1, cs1)
        if l % 2 == 0:
            e = l // 2
            yc, yl = mixer_ab(hc, hl, ab_w_in[e], ab_w_out[e], na_rpb[e], win_sink[e], need_ctx)
        else:
            o = l // 2
            yc, yl = mixer_c(hc, hl, ml_w_in[o], ml_b_gates[o], ml_norm_w[o], ml_w_out[o], need_ctx)
        xl = xl + g1 * yl
        hl2 = modulate(rmsnorm(xl, norm2_w[l]), sh2, sc2)
        xl = xl + g2 * ec_moe(hl2, moe_router[l], moe_w1[l], moe_w3[l], moe_w2[l])
        if need_ctx:
            xc = xc + cg1 * yc
            hc2 = modulate(rmsnorm(xc, norm2_w[l]), ch2, cs2)
            xc = xc + cg2 * ec_moe(hc2, moe_router[l], moe_w1[l], moe_w3[l], moe_w2[l])
    return rmsnorm(xl, final_norm_w)
```

```python
import numpy as np
import ml_dtypes
from contextlib import ExitStack
import concourse.bass as bass
import concourse.mybir as mybir
from concourse.bass_utils import run_bass_kernel_spmd

F32 = mybir.dt.float32
BF16 = mybir.dt.bfloat16
AF = mybir.ActivationFunctionType
ALU = mybir.AluOpType
AX = mybir.AxisListType

D = 4096
NTOK = 4352
NCTX = 256
NLAT = 4096
DEPTH = 4
KC = 32
EPS = 1e-6
NEG = -30000.0


class Eng:
    def __init__(self, k, name, e, sem):
        self.k, self.name, self.e, self.sem = k, name, e, sem
        self.seq = 0
        self.insts = {}
        self.tick = []
        self.count = 0
        self.known = {}

    def ticket(self, seq):
        for s, c in reversed(self.tick[-64:]):
            if s < seq:
                break
        lo = None
        for s, c in reversed(self.tick):
            if s >= seq:
                lo = c
            else:
                break
        if lo is not None:
            return lo
        if seq not in self.insts:
            seq = max(self.insts)
        ins = self.insts[seq]
        self.count += 1
        ins.then_inc(self.sem, 1)
        self.tick.append((seq, self.count))
        if len(self.tick) > 256:
            self.tick = self.tick[-128:]
        for s in [s for s in self.insts if s <= seq]:
            del self.insts[s]
        return self.count


class DmaSem:
    def __init__(self, sem):
        self.sem = sem
        self.issued = 0


class Buf:
    def __init__(self, k, name, t, partial=False):
        self.k, self.name, self.t, self.partial = k, name, t, partial
        self.w = {}
        self.r = {}
        self.dsem = None

    def __getitem__(self, idx):
        return self.t[idx]


class K:
    def __init__(self, nc):
        self.nc = nc
        self.stack = ExitStack()
        self.pe = Eng(self, "pe", nc.tensor, self._sem("s_pe"))
        self.act = Eng(self, "act", nc.scalar, self._sem("s_act"))
        self.dve = Eng(self, "dve", nc.vector, self._sem("s_dve"))
        self.pool = Eng(self, "pool", nc.gpsimd, self._sem("s_pool"))
        self.sp = Eng(self, "sp", nc.sync, self._sem("s_sp"))
        self.engs = [self.pe, self.act, self.dve, self.pool, self.sp]
        self.dsem_free = [DmaSem(self._sem(f"s_dma{i}")) for i in range(64)]
        self.bar = self._sem("s_bar")
        self.bar_n = 0
        self.phase_stack = None
        self.phase_bufs = []
        self.n_inst = 0

    def _sem(self, name):
        return self.stack.enter_context(self.nc.semaphore(name))

    def begin_phase(self):
        self.phase_stack = ExitStack()
        self.phase_bufs = []

    def sbuf(self, name, shape, dt, partial=False):
        self.uid = getattr(self, "uid", 0) + 1
        t = self.phase_stack.enter_context(self.nc.sbuf_tensor(f"{name}_u{self.uid}", list(shape), dt))
        b = Buf(self, name, t, partial)
        self.phase_bufs.append(b)
        return b

    def psum(self, name, shape, dt, partial=False):
        self.uid = getattr(self, "uid", 0) + 1
        t = self.phase_stack.enter_context(self.nc.psum_tensor(f"{name}_u{self.uid}", list(shape), dt))
        b = Buf(self, name, t, partial)
        self.phase_bufs.append(b)
        return b

    def end_phase(self):
        for b in self.phase_bufs:
            if b.dsem is not None:
                self._wait(self.sp, b.dsem.sem, 16 * b.dsem.issued)
        self.barrier()
        for b in self.phase_bufs:
            if b.dsem is not None:
                self.dsem_free.append(b.dsem)
                b.dsem = None
        self.phase_stack.close()
        self.phase_stack = None
        self.phase_bufs = []

    def barrier(self):
        for e in self.engs:
            if e.seq > 0 and e is not self.sp:
                last = e.seq
                if last in e.insts or any(s >= last for s, _ in e.tick):
                    t = e.ticket(last)
                    self._wait(e, e.sem, t)
        self.bar_n += 1
        for e in self.engs:
            e.e.sem_inc(self.bar, 1)
        for e in self.engs:
            e.e.wait_ge(self.bar, len(self.engs) * self.bar_n)

    def _wait(self, eng, sem, val):
        key = id(sem)
        if eng.known.get(key, 0) >= val:
            return
        eng.known[key] = val
        eng.e.wait_ge(sem, val)

    def _wait_dep(self, eng, key, val, same_engine_ok):
        if isinstance(key, Eng):
            if key is eng and not same_engine_ok:
                return
            if key is eng and key is self.pe:
                return
            t = key.ticket(val)
            self._wait(eng, key.sem, t)
        else:
            b = key[1]
            if b.dsem is None:
                return
            self._wait(eng, b.dsem.sem, 16 * b.dsem.issued)

    def _deps(self, eng, R, W):
        for b in R:
            for key, val in list(b.w.items()):
                self._wait_dep(eng, key, val, same_engine_ok=True)
        for b in W:
            for key, val in list(b.w.items()):
                self._wait_dep(eng, key, val, same_engine_ok=False)
            for key, val in list(b.r.items()):
                self._wait_dep(eng, key, val, same_engine_ok=False)

    def _record(self, key, val, R, W):
        for b in W:
            if b.partial:
                b.w[key] = val
            else:
                b.w = {key: val}
                b.r = {}
        for b in R:
            b.r[key] = val

    def op(self, eng, fn, R=(), W=()):
        self._deps(eng, R, W)
        ins = fn(eng.e)
        eng.seq += 1
        eng.insts[eng.seq] = ins
        if len(eng.insts) > 4096:
            for s in sorted(eng.insts)[:2048]:
                del eng.insts[s]
        self._record(eng, eng.seq, R, W)
        self.n_inst += 1
        return ins

    def dma(self, eng, out, in_, R=(), W=(), **kw):
        self._deps(eng, R, W)
        sb = (list(W) + list(R))[0]
        if sb.dsem is None:
            sb.dsem = self.dsem_free.pop()
        ins = eng.e.dma_start(out=out, in_=in_, **kw)
        ins.then_inc(sb.dsem.sem, 16)
        sb.dsem.issued += 1
        self._record(("dma", sb), True, R, W)
        self.n_inst += 1
        return ins


TOK_TILES = [(0, 256)] + [(256 + 512 * i, 512) for i in range(8)]


class Prog:
    def __init__(self, kinds=None, layers=(0, 1, 2, 3), only=None, shapes=None):
        self.nc = nc = bass.Bass("TRN2", target_bir_lowering=False)
        self.kinds = kinds or {}
        self.layers = layers
        self.only = only
        self.shapes = shapes or {}
        self.d = {}

    def dram(self, name, shape, dt, kind="Internal"):
        kind = self.kinds.get(name, kind)
        if kind == "ExternalInput" and self.only is not None and name not in self.only:
            return None
        shape = self.shapes.get(name, shape)
        self.d[name] = self.nc.dram_tensor(name, list(shape), dt, kind=kind).ap()
        return self.d[name]

    def declare(self):
        I = "ExternalInput"
        d = self.dram
        d("x", [NLAT, D], F32, I); d("ctx", [NCTX, D], F32, I); d("cvec", [2, D], F32, I)
        d("ada_w", [DEPTH, D, 6 * D], F32, I); d("ada_b", [DEPTH, 6 * D], F32, I)
        d("norm1_w", [DEPTH, D], F32, I); d("norm2_w", [DEPTH, D], F32, I)
        d("ab_w_in", [2, D, 9216], F32, I); d("ab_w_out", [2, D, D], F32, I)
        d("win_sink", [2, 16], F32, I)
        d("ml_w_in", [2, D, 12320], F32, I); d("ml_b_gates", [2, 32], F32, I)
        d("ml_norm_w", [2, D], F32, I); d("ml_w_out", [2, D, D], F32, I)
        d("moe_router", [DEPTH, D, 16], F32, I)
        d("moe_w1", [DEPTH, 16, D, 256], F32, I); d("moe_w3", [DEPTH, 16, D, 256], F32, I)
        d("moe_w2", [DEPTH, 16, 256, D], F32, I)
        d("final_norm_w", [1, D], F32, I)
        d("c_ident", [128, 128], F32, I)
        d("c_bm", [2, 16, 128, 21, 128], F32, I)
        d("c_cos", [128, NLAT], F32, I); d("c_sin", [128, NLAT], F32, I); d("c_perm", [128, 128], F32, I)
        d("c_wmask", [128, 2, 128], F32, I)
        d("c_tri", [64, 4, 64], F32, I)
        d("y", [NLAT, D], F32, "ExternalOutput")
        d("xl", [NTOK, D], F32); d("xm", [NTOK, D], F32); d("mod", [DEPTH, 2, 6 * D], F32)
        d("QaT", [16, 128, NTOK], BF16); d("KaT", [16, 128, NTOK], BF16); d("Va", [NTOK, 2048], BF16)
        d("QbT", [16, 128, NTOK], BF16); d("KbT", [4, 128, NTOK], BF16); d("Vb", [NTOK, 512], BF16)
        d("OT", [32, 128, NTOK], BF16)
        d("gm", [16, NTOK], F32)
        d("mqT", [16, 128, NTOK], BF16); d("mkT", [16, 128, NTOK], BF16)
        d("mK", [NTOK, 2048], BF16); d("mV", [NTOK, D], BF16); d("mO", [NTOK, D], BF16)
        d("mG", [NTOK, 32], F32); d("hf", [NTOK, D], F32); d("hb", [NTOK, D], F32)


def mm(K, ps_buf, out_ap, lhsT, rhs, start, stop, R):
    return K.op(K.pe, lambda e: e.matmul(out_ap, lhsT, rhs, start=start, stop=stop), R=R, W=[ps_buf])


class Common:
    pass


def setup_common(C, K, copy_inputs=True):
    nc = C.nc
    st = K.stack
    cm = Common()
    def gbuf(name, shape, dt):
        t = st.enter_context(nc.sbuf_tensor(name, list(shape), dt))
        return Buf(K, name, t)
    cm.ident_f = gbuf("ident_f", [128, 128], F32)
    cm.ident_b = gbuf("ident_b", [128, 128], BF16)
    cm.ones_b = gbuf("ones_b", [128, 128], BF16)
    cm.ones_f = gbuf("ones_f", [128, 128], F32)
    K.dma(K.sp, cm.ident_f[:], C.d["c_ident"][:, :], W=[cm.ident_f])
    K.dma(K.pool, cm.ident_b[:], C.d["c_ident"][:, :], W=[cm.ident_b])
    K.op(K.dve, lambda e: e.memset(cm.ones_b[:], 1.0), W=[cm.ones_b])
    K.op(K.dve, lambda e: e.memset(cm.ones_f[:], 1.0), W=[cm.ones_f])
    cm.eps = gbuf("eps_t", [128, 1], F32)
    K.op(K.dve, lambda e: e.memset(cm.eps[:], EPS), W=[cm.eps])
    C.cm = cm
    if not copy_inputs:
        return
    tmp = gbuf("cp_sem_holder", [1, 2], F32)
    K.dma(K.sp, C.d["xl"][0:NCTX, :], C.d["ctx"][:, :], W=[tmp])
    K.dma(K.sp, C.d["xl"][NCTX:NTOK, :], C.d["x"][:, :], W=[tmp])
    K._wait(K.sp, tmp.dsem.sem, 16 * tmp.dsem.issued)
    K.barrier()


def load_vecs_pp(C, K, rows, out_buf, scratch_v, ps_buf):
    cm = C.cm
    n = len(rows)
    for j, r in enumerate(rows):
        K.dma(K.sp, scratch_v[0:32, j, :], r.rearrange("(c p) -> c p", p=128), W=[scratch_v])
    for j in range(n):
        K.op(K.pe, lambda e, j=j: e.transpose(ps_buf[:, j * 32:(j + 1) * 32], scratch_v[0:32, j, :], cm.ident_f[0:32, 0:32]),
             R=[scratch_v, cm.ident_f], W=[ps_buf])
    K.op(K.dve, lambda e: e.tensor_copy(out_buf[:, 0:n, :].rearrange("p j c -> p (j c)"), ps_buf[:, 0:n * 32]), R=[ps_buf], W=[out_buf])


def phase_mods(C, K, layers):
    nc, d, cm = C.nc, C.d, C.cm
    K.begin_phase()
    vs = K.sbuf("vs", [32, 2, 128], F32)
    ps = K.psum("ps_m", [128, 512], F32)
    cpp = K.sbuf("cpp", [128, 2, 32], F32)
    scT = K.sbuf("scT", [128, 32, 2], BF16)
    load_vecs_pp(C, K, [d["cvec"][0], d["cvec"][1]], cpp, vs, ps)
    K.op(K.act, lambda e: e.activation(scT[:].rearrange("p c r -> p r c"), cpp[:], AF.Silu), R=[cpp], W=[scT])
    wbs = [K.sbuf(f"wb{i}", [128, KC, 512], BF16) for i in range(2)]
    bbs = [K.sbuf(f"bb{i}", [2, 512], F32) for i in range(2)]
    obs = [K.sbuf(f"ob{i}", [2, 512], F32) for i in range(2)]
    pss = [K.psum(f"ps_mod{i}", [128, 512], F32) for i in range(2)]
    it = 0
    for l in layers:
        for g in range(48):
            wb, bb, ob, pq = wbs[it % 2], bbs[it % 2], obs[it % 2], pss[it % 2]
            it += 1
            cs = slice(g * 512, (g + 1) * 512)
            K.dma(K.pool, wb[:], d["ada_w"][l][:, cs].rearrange("(c p) n -> p c n", p=128), W=[wb])
            K.dma(K.sp, bb[:], d["ada_b"][l:l + 1, cs].partition_broadcast(2) if False else d["ada_b"][l:l + 1, cs].broadcast_to([2, 512]), W=[bb])
            for kc in range(KC):
                mm(K, pq, pq[0:2, :], scT[:, kc, :], wb[:, kc, :], kc == 0, kc == KC - 1, R=[scT, wb])
            K.op(K.dve, lambda e, ob=ob, pq=pq, bb=bb: e.tensor_tensor(ob[:], pq[0:2, :], bb[:], ALU.add), R=[pq, bb], W=[ob])
            K.dma(K.sp, d["mod"][l][:, cs], ob[:], R=[ob])
    K.end_phase()


class NormBufs:
    def __init__(self, K, tag=""):
        self.xt = K.sbuf("nb_xt" + tag, [128, D], F32)
        self.xs = K.sbuf("nb_xs" + tag, [128, D], BF16)
        self.ss = K.sbuf("nb_ss" + tag, [128, 2], F32)
        self.tp = [K.psum(f"nb_tp{i}" + tag, [128, 1024], BF16) for i in range(2)]
        self.n = 0


def norm_block(C, K, nb, src_rows, gsh, row, hT, col0):
    cm = C.cm
    xt, xs, ss = nb.xt, nb.xs, nb.ss
    K.dma(K.sp, xt[:], src_rows, W=[xt])
    K.op(K.act, lambda e: e.activation(xs[:], xt[:], AF.Square, accum_out=ss[:, 0:1]), R=[xt], W=[xs, ss])
    K.op(K.act, lambda e: e.activation(ss[:, 1:2], ss[:, 0:1], AF.Sqrt, scale=1.0 / D, bias=cm.eps[:, 0:1]), R=[ss, cm.eps], W=[ss])
    K.op(K.dve, lambda e: e.reciprocal(ss[:, 1:2], ss[:, 1:2]), R=[ss], W=[ss])
    K.op(K.act, lambda e: e.activation(xs[:], xt[:], AF.Copy, scale=ss[:, 1:2]), R=[xt, ss], W=[xs])
    for g in range(4):
        tp = nb.tp[nb.n % 2]
        nb.n += 1
        for c in range(8):
            ch = g * 8 + c
            K.op(K.pe, lambda e, c=c, ch=ch, tp=tp: e.transpose(tp[:, c * 128:(c + 1) * 128], xs[:, ch * 128:(ch + 1) * 128], cm.ident_b[:]),
                 R=[xs, cm.ident_b], W=[tp])
        for c in range(8):
            ch = g * 8 + c
            o = hT[:, ch, col0:col0 + 128]
            i = tp[:, c * 128:(c + 1) * 128]
            gs = gsh[:, 2 * row, ch:ch + 1]
            sh = gsh[:, 2 * row + 1, ch:ch + 1]
            if c % 2 == 0:
                K.op(K.act, lambda e, o=o, i=i, gs=gs, sh=sh: e.activation(o, i, AF.Identity, scale=gs, bias=sh), R=[tp, gsh], W=[hT])
            else:
                K.op(K.dve, lambda e, o=o, i=i, gs=gs, sh=sh: e.tensor_scalar(o, i, gs, sh, ALU.mult, ALU.add), R=[tp, gsh], W=[hT])


def build_gsh(C, K, l, which, gsh, vs, ps, tmp):
    d = C.d
    nw = d["norm1_w"] if which == 1 else d["norm2_w"]
    o = 0 if which == 1 else 3
    m = d["mod"][l]
    rows = [nw[l], m[0, (o + 1) * D:(o + 2) * D], m[0, o * D:(o + 1) * D], m[1, (o + 1) * D:(o + 2) * D], m[1, o * D:(o + 1) * D]]
    load_vecs_pp(C, K, rows, tmp, vs, ps)
    for r in range(2):
        K.op(K.dve, lambda e, r=r: e.scalar_tensor_tensor(gsh[:, 2 * r, :], tmp[:, 1 + 2 * r, :], 1.0, tmp[:, 0, :], ALU.add, ALU.mult),
             R=[tmp], W=[gsh])
        K.op(K.dve, lambda e, r=r: e.tensor_copy(gsh[:, 2 * r + 1, :], tmp[:, 2 + 2 * r, :]), R=[tmp], W=[gsh])


def phase_inproj(C, K, l, groups, extra_setup=None):
    nc, d, cm = C.nc, C.d, C.cm
    K.begin_phase()
    S = type("S", (), {})()
    nb = NormBufs(K)
    vs = K.sbuf("vs", [32, 5, 128], F32)
    tmpv = K.sbuf("tmpv", [128, 5, 32], F32)
    gsh = K.sbuf("gsh", [128, 4, 32], F32)
    acc = [K.psum(f"acc{i}", [128, 512], F32) for i in range(4)]
    S.rp = [K.psum(f"rp{i}", [128, 512], F32) for i in range(2)]
    build_gsh(C, K, l, 1, gsh, vs, acc[0], tmpv)
    hTs = [K.sbuf(f"hT{i}", [128, KC, 512], BF16, partial=True) for i in range(2)]
    wbs = [K.sbuf(f"wb{i}", [128, KC, 512], BF16) for i in range(2)]
    S.stg = [K.sbuf(f"stg{i}", [128, 512], BF16) for i in range(4)]
    S.stgf = [K.sbuf(f"stgf{i}", [128, 512], F32) for i in range(3)]
    S.n = 0
    S.K, S.C = K, C
    if extra_setup:
        extra_setup(S)
    wi = 0
    ai = 0
    for ti, (tok0, ntok) in enumerate(TOK_TILES):
        hT = hTs[ti % 2]
        row = 1 if ti == 0 else 0
        nsub = ntok // 128
        for sb in range(nsub):
            norm_block(C, K, nb, d["xl"][tok0 + sb * 128: tok0 + (sb + 1) * 128, :], gsh, row, hT, sb * 128)
        S.tok0, S.ntok, S.is_ctx = tok0, ntok, ti == 0
        if hasattr(S, "tile_setup"):
            S.tile_setup(S)
        for g in groups:
            wb = wbs[wi % 2]
            wi += 1
            ncols = g["ncols"]
            K.dma(K.pool, wb[:, :, 0:ncols], g["w"].rearrange("(c p) n -> p c n", p=128), W=[wb])
            if g["kind"] == "FM":
                for b in range(ncols // 128):
                    ps = acc[ai % 4]
                    ai += 1
                    for kc in range(KC):
                        mm(K, ps, ps[:, 0:ntok], wb[:, kc, b * 128:(b + 1) * 128], hT[:, kc, 0:ntok], kc == 0, kc == KC - 1, R=[wb, hT])
                    g["evac"](S, g, b, ps)
            else:
                for sb in range(nsub):
                    ps = acc[ai % 4]
                    ai += 1
                    for kc in range(KC):
                        mm(K, ps, ps[:, 0:ncols], hT[:, kc, sb * 128:(sb + 1) * 128], wb[:, kc, 0:ncols], kc == 0, kc == KC - 1, R=[wb, hT])
                    g["evac"](S, g, sb, ps)
    K.end_phase()


def ev_fm_plain(dst, scale=None):
    def f(S, g, b, ps):
        K = S.K
        st = S.stg[S.n % 4]
        S.n += 1
        n = S.ntok
        if scale is None:
            K.op(K.act, lambda e: e.copy(st[:, 0:n], ps[:, 0:n]), R=[ps], W=[st])
        else:
            K.op(K.act, lambda e: e.activation(st[:, 0:n], ps[:, 0:n], AF.Copy, scale=scale), R=[ps], W=[st])
        K.dma(K.sp, dst[g["b0"] + b, :, S.tok0:S.tok0 + n], st[:, 0:n], R=[st])
    return f


def ev_tm_plain(dst, dt=BF16):
    def f(S, g, sb, ps):
        K = S.K
        nco = g["ncols"]
        if dt == BF16:
            st = S.stg[S.n % 4]
        else:
            st = S.stgf[S.n % 3]
        S.n += 1
        if S.n % 2 == 0:
            K.op(K.act, lambda e: e.copy(st[:, 0:nco], ps[:, 0:nco]), R=[ps], W=[st])
        else:
            K.op(K.dve, lambda e: e.tensor_copy(st[:, 0:nco], ps[:, 0:nco]), R=[ps], W=[st])
        r0 = S.tok0 + sb * 128
        K.dma(K.sp, dst[r0:r0 + 128, g["c0"]:g["c0"] + nco], st[:, 0:nco], R=[st])
    return f


def ev_fm_rope(dst, scale):
    plain = ev_fm_plain(dst, scale)
    def f(S, g, b, ps):
        K, C = S.K, S.C
        if S.is_ctx:
            return plain(S, g, b, ps)
        n = S.ntok
        xs = S.stgf[S.n % 3]
        t1 = S.stgf[(S.n + 1) % 3]
        t2 = S.stgf[(S.n + 2) % 3]
        st = S.stg[S.n % 4]
        rp = S.rp[S.n % 2]
        S.n += 1
        K.op(K.act, lambda e: e.activation(xs[:, 0:n], ps[:, 0:n], AF.Copy, scale=(1.0 if scale is None else scale)), R=[ps], W=[xs])
        mm(K, rp, rp[:, 0:n], S.perm[:], xs[:, 0:n], True, True, R=[S.perm, xs])
        K.op(K.dve, lambda e: e.tensor_tensor(t1[:, 0:n], xs[:, 0:n], S.cos[:, 0:n], ALU.mult), R=[xs, S.cos], W=[t1])
        K.op(K.dve, lambda e: e.tensor_tensor(t2[:, 0:n], rp[:, 0:n], S.sin[:, 0:n], ALU.mult), R=[rp, S.sin], W=[t2])
        K.op(K.pool, lambda e: e.tensor_tensor(st[:, 0:n], t1[:, 0:n], t2[:, 0:n], ALU.add), R=[t1, t2], W=[st])
        K.dma(K.sp, dst[g["b0"] + b, :, S.tok0:S.tok0 + n], st[:, 0:n], R=[st])
    return f


def phase_inproj_attn(C, K, l):
    d = C.d
    e = l // 2
    w = d["ab_w_in"][e]
    qs = 128 ** -0.5
    groups = []
    for i in range(4):
        groups.append(dict(kind="FM", w=w[:, i * 512:(i + 1) * 512], ncols=512, b0=4 * i, evac=ev_fm_plain(d["QaT"], qs)))
    for i in range(4):
        groups.append(dict(kind="FM", w=w[:, 2048 + i * 512:2048 + (i + 1) * 512], ncols=512, b0=4 * i, evac=ev_fm_plain(d["KaT"])))
    for i in range(4):
        groups.append(dict(kind="TM", w=w[:, 4096 + i * 512:4096 + (i + 1) * 512], ncols=512, c0=512 * i, evac=ev_tm_plain(d["Va"])))
    for i in range(4):
        groups.append(dict(kind="FM", w=w[:, 6144 + i * 512:6144 + (i + 1) * 512], ncols=512, b0=4 * i, evac=ev_fm_rope(d["QbT"], qs)))
    groups.append(dict(kind="FM", w=w[:, 8192:8704], ncols=512, b0=0, evac=ev_fm_rope(d["KbT"], None)))
    groups.append(dict(kind="TM", w=w[:, 8704:9216], ncols=512, c0=0, evac=ev_tm_plain(d["Vb"])))

    def setup(S):
        S.perm = K.sbuf("perm", [128, 128], F32)
        K.dma(K.sp, S.perm[:], d["c_perm"][:, :], W=[S.perm])
        S.cos = K.sbuf("cos", [128, 512], F32)
        S.sin = K.sbuf("sin", [128, 512], F32)

        def tile_setup(S):
            if not S.is_ctx:
                p0 = S.tok0 - NCTX
                K.dma(K.sp, S.cos[:], d["c_cos"][:, p0:p0 + 512], W=[S.cos])
                K.dma(K.sp, S.sin[:], d["c_sin"][:, p0:p0 + 512], W=[S.sin])
        S.tile_setup = tile_setup
    phase_inproj(C, K, l, groups, setup)


def host_consts():
    c = {}
    c["c_ident"] = np.eye(128, dtype=np.float32)
    perm = np.zeros((128, 128), np.float32)
    for m in range(128):
        p = m + 32 if (m % 64) < 32 else m - 32
        perm[p, m] = 1.0
    c["c_perm"] = perm
    inv = (np.float32(10000.0) ** (-np.arange(32, dtype=np.float32) / np.float32(32))).astype(np.float32)
    t = np.arange(NLAT)
    rowp = (t // 64).astype(np.float32)
    colp = (t % 64).astype(np.float32)
    cos = np.zeros((128, NLAT), np.float32)
    sin = np.zeros((128, NLAT), np.float32)
    for dd in range(128):
        pos = rowp if dd < 64 else colp
        ang = (pos * inv[dd % 32]).astype(np.float32)
        cos[dd] = np.cos(ang)
        sin[dd] = np.sin(ang) * (-1.0 if (dd % 64) < 32 else 1.0)
    c["c_cos"], c["c_sin"] = cos, sin
    return c


def na_key_tiles(j):
    if j <= 1:
        kps = [0, 1, 2, 3]
        base = 5 + 4 * j
        idx = [base + i for i in range(4)]
    elif j >= 30:
        kps = [28, 29, 30, 31]
        base = 13 + 4 * (j - 30)
        idx = [base + i for i in range(4)]
    else:
        kps = [j - 2, j - 1, j, j + 1, j + 2]
        idx = [0, 1, 2, 3, 4]
    return kps, idx


def phase_na(C, K, l, need_ctx):
    nc, d, cm = C.nc, C.d, C.cm
    e = l // 2
    K.begin_phase()
    QT = [K.sbuf(f"QT{i}", [128, NTOK], BF16) for i in range(2)]
    KT = [K.sbuf(f"KT{i}", [128, NTOK], BF16) for i in range(2)]
    V = [K.sbuf(f"V{i}", [128, 34, 128], BF16) for i in range(2)]
    BM = [K.sbuf(f"BM{i}", [128, 21, 128], F32) for i in range(2)]
    OS = [K.sbuf(f"OS{i}", [128, NTOK], BF16, partial=True) for i in range(2)]
    sA = [K.psum(f"sA{i}", [128, 512], F32) for i in range(2)]
    sB = [K.psum(f"sB{i}", [128, 512], F32) for i in range(2)]
    po = [K.psum(f"po{i}", [128, 256], F32) for i in range(2)]
    pT = [K.sbuf(f"pT{i}", [128, 896], BF16) for i in range(2)]
    rc = [K.sbuf(f"rc{i}", [128, 128], F32) for i in range(2)]
    blocks = [("lat", j) for j in range(32)] + ([("ctx", 0), ("ctx", 1)] if need_ctx else [])
    it = 0
    for h in range(16):
        qt, kt, v, bm, osb = QT[h % 2], KT[h % 2], V[h % 2], BM[h % 2], OS[h % 2]
        K.dma(K.sp, qt[:], d["QaT"][h], W=[qt])
        K.dma(K.sp, kt[:], d["KaT"][h], W=[kt])
        K.dma(K.sp, v[:], d["Va"].rearrange("(t p) c -> p t c", p=128)[:, :, h * 128:(h + 1) * 128], W=[v])
        K.dma(K.sp, bm[:], d["c_bm"][e, h], W=[bm])

        def qk(blk, i):
            kind, j = blk
            if kind == "lat":
                kps, idx = na_key_tiles(j)
                tiles = [(2 + kp, ix) for kp, ix in zip(kps, idx)] + [(0, None), (1, None)]
                q0 = NCTX + 128 * j
            else:
                tiles = [(0, None), (1, None)]
                q0 = 128 * j
            for s, (t, ix) in enumerate(tiles):
                bank = sA[i % 2] if s < 4 else sB[i % 2]
                o = bank[:, (s % 4) * 128:(s % 4 + 1) * 128]
                mm(K, bank, o, kt[:, t * 128:(t + 1) * 128], qt[:, q0:q0 + 128], True, ix is None, R=[kt, qt])
                if ix is not None:
                    mm(K, bank, o, bm[:, ix, :], cm.ident_f[:], False, True, R=[bm, cm.ident_f])
            return tiles, q0

        def pv(blk, i, tiles, q0):
            n = len(tiles)
            p = pT[i % 2]
            na = min(n, 4)
            K.op(K.act, lambda e: e.activation(p[:, 0:na * 128], sA[i % 2][:, 0:na * 128], AF.Exp), R=[sA[i % 2]], W=[p])
            if n > 4:
                K.op(K.act, lambda e: e.activation(p[:, 512:n * 128], sB[i % 2][:, 0:(n - 4) * 128], AF.Exp), R=[sB[i % 2]], W=[p])
            pq = po[i % 2]
            for s, (t, ix) in enumerate(tiles):
                mm(K, pq, pq[:, 0:128], v[:, t, :], p[:, s * 128:(s + 1) * 128], s == 0, s == n - 1, R=[v, p])
            for s, (t, ix) in enumerate(tiles):
                mm(K, pq, pq[:, 128:256], cm.ones_b[:], p[:, s * 128:(s + 1) * 128], s == 0, s == n - 1, R=[cm.ones_b, p])
            r = rc[i % 2]
            K.op(K.dve, lambda e: e.reciprocal(r[:], pq[:, 128:256]), R=[pq], W=[r])
            K.op(K.dve, lambda e: e.tensor_tensor(osb[:, q0:q0 + 128], pq[:, 0:128], r[:], ALU.mult), R=[pq, r], W=[osb])

        prev = None
        for blk in blocks:
            cur = (blk, it) + qk(blk, it)
            it += 1
            if prev is not None:
                pv(*prev)
            prev = cur
        pv(*prev)
        K.dma(K.sp, d["OT"][h], osb[:], R=[osb])
    K.end_phase()


def phase_win(C, K, l, need_ctx):
    nc, d, cm = C.nc, C.d, C.cm
    e = l // 2
    K.begin_phase()
    QT = [K.sbuf(f"QT{i}", [128, 4, NTOK], BF16) for i in range(2)]
    KT = [K.sbuf(f"KT{i}", [128, NTOK], BF16) for i in range(2)]
    V = [K.sbuf(f"V{i}", [128, 34, 128], BF16) for i in range(2)]
    OS = K.sbuf("OS", [128, 4, NTOK], BF16, partial=True)
    wm = K.sbuf("wm", [128, 2, 128], F32)
    id4 = K.sbuf("id4", [128, 4, 128], F32)
    K.dma(K.sp, wm[:], d["c_wmask"][:, :, :], W=[wm])
    for i in range(4):
        K.dma(K.sp, id4[:, i, :], d["c_ident"][:, :], W=[id4])
    sk = K.sbuf("sk", [1, 16], F32)
    esr = K.sbuf("esr", [1, 16, 128], F32)
    K.dma(K.sp, sk[:], d["win_sink"][e:e + 1, :], W=[sk])
    K.op(K.act, lambda e_: e_.activation(sk[:], sk[:], AF.Exp), R=[sk], W=[sk])
    for h in range(16):
        K.op(K.dve, lambda e_, h=h: e_.tensor_scalar(esr[0:1, h, :], cm.ones_f[0:1, 0:128], sk[0:1, h:h + 1], None, ALU.mult), R=[sk, cm.ones_f], W=[esr])
    sb = [K.psum(f"sb{i}", [128, 512], F32) for i in range(5)]
    po = K.psum("po", [128, 512], F32)
    pm = K.psum("pm", [128, 512], F32)
    pT = [K.sbuf(f"pT{i}", [128, 5, 512], BF16) for i in range(2)]
    rc = K.sbuf("rc", [128, 512], F32)
    blocks = [("lat", j) for j in range(32)] + ([("ctx", 0), ("ctx", 1)] if need_ctx else [])
    it = 0
    for g in range(4):
        qt, kt, v = QT[g % 2], KT[g % 2], V[g % 2]
        K.dma(K.sp, qt[:], d["QbT"][4 * g:4 * g + 4].rearrange("h p t -> p h t"), W=[qt])
        K.dma(K.sp, kt[:], d["KbT"][g], W=[kt])
        K.dma(K.sp, v[:], d["Vb"].rearrange("(t p) c -> p t c", p=128)[:, :, g * 128:(g + 1) * 128], W=[v])
        for blk in blocks:
            kind, j = blk
            if kind == "lat":
                tiles = []
                if j > 0:
                    tiles.append((2 + j - 1, 0))
                tiles.append((2 + j, None))
                if j < 31:
                    tiles.append((2 + j + 1, 1))
                tiles += [(0, None), (1, None)]
                q0 = NCTX + 128 * j
            else:
                tiles = [(0, None), (1, None)]
                q0 = 128 * j
            n = len(tiles)
            p = pT[it % 2]
            it += 1
            for s, (t, mi) in enumerate(tiles):
                bank = sb[s]
                mm(K, bank, bank[:], kt[:, t * 128:(t + 1) * 128], qt[:, :, q0:q0 + 128], True, mi is None, R=[kt, qt])
                if mi is not None:
                    mm(K, bank, bank[:], wm[:, mi, :], id4[:], False, True, R=[wm, id4])
                K.op(K.act, lambda e_, s=s, bank=bank: e_.activation(p[:, s, :], bank[:], AF.Exp), R=[bank], W=[p])
            for s, (t, mi) in enumerate(tiles):
                mm(K, po, po[:], v[:, t, :], p[:, s, :], s == 0, s == n - 1, R=[v, p])
            for s, (t, mi) in enumerate(tiles):
                mm(K, pm, pm[:], cm.ones_b[:], p[:, s, :], s == 0, False, R=[cm.ones_b, p])
            mm(K, pm, pm[:], cm.ones_f[0:1, 0:128], esr[0:1, 4 * g:4 * g + 4, :], False, True, R=[cm.ones_f, esr])
            K.op(K.dve, lambda e_: e_.reciprocal(rc[:], pm[:]), R=[pm], W=[rc])
            K.op(K.dve, lambda e_: e_.tensor_tensor(OS[:, :, q0:q0 + 128], po[:].rearrange("p (h q) -> p h q", h=4), rc[:].rearrange("p (h q) -> p h q", h=4), ALU.mult),
                 R=[po, rc], W=[OS])
        for hh in range(4):
            K.dma(K.sp, d["OT"][16 + 4 * g + hh], OS[:, hh, :], R=[OS])
    K.end_phase()


def phase_outproj(C, K, l, w_out, need_ctx):
    nc, d, cm = C.nc, C.d, C.cm
    K.begin_phase()
    wbs = [K.sbuf(f"wb{i}", [128, KC, 512], BF16) for i in range(2)]
    ots = [K.sbuf(f"ot{i}", [128, KC, 512], BF16) for i in range(2)]
    gb = [K.sbuf(f"gb{i}", [128, 2, 512], F32) for i in range(2)]
    xin = [K.sbuf(f"xin{i}", [128, 512], F32) for i in range(3)]
    tt_ = [K.sbuf(f"tt{i}", [128, 512], F32) for i in range(3)]
    acc = [K.psum(f"acc{i}", [128, 512], F32) for i in range(4)]
    tiles = TOK_TILES if need_ctx else TOK_TILES[1:]
    oi = 0
    xi = 0
    for g in range(8):
        cs = slice(g * 512, (g + 1) * 512)
        wb, gbt = wbs[g % 2], gb[g % 2]
        K.dma(K.pool, wb[:], w_out[:, cs].rearrange("(c p) n -> p c n", p=128), W=[wb])
        for r in range(2):
            K.dma(K.sp, gbt[:, r, :], d["mod"][l][r:r + 1, 2 * D + g * 512:2 * D + (g + 1) * 512].broadcast_to([128, 512]), W=[gbt])
        for (tok0, ntok) in tiles:
            ot = ots[oi % 2]
            oi += 1
            row = 1 if tok0 == 0 else 0
            K.dma(K.sp, ot[:, :, 0:ntok], d["OT"][:, :, tok0:tok0 + ntok].rearrange("k p t -> p k t"), W=[ot])
            for sb in range(ntok // 128):
                ps = acc[xi % 4]
                xt, t = xin[xi % 3], tt_[xi % 3]
                xi += 1
                r0 = tok0 + sb * 128
                K.dma(K.sp, xt[:], d["xl"][r0:r0 + 128, cs], W=[xt])
                for kc in range(KC):
                    mm(K, ps, ps[:], ot[:, kc, sb * 128:(sb + 1) * 128], wb[:, kc, :], kc == 0, kc == KC - 1, R=[ot, wb])
                K.op(K.dve, lambda e_, t=t, ps=ps: e_.tensor_tensor(t[:], ps[:], gbt[:, row, :], ALU.mult), R=[ps, gbt], W=[t])
                K.op(K.pool, lambda e_, t=t, xt=xt: e_.tensor_tensor(t[:], t[:], xt[:], ALU.add), R=[t, xt], W=[t])
                K.dma(K.sp, d["xm"][r0:r0 + 128, cs], t[:], R=[t])
    K.end_phase()


def host_bm(rpb):
    q = np.arange(128)
    key = np.arange(128)
    mats = [(10, 10 + off) for off in (-2, -1, 0, 1, 2)]
    for j in (0, 1):
        mats += [(j, kp) for kp in (0, 1, 2, 3)]
    for j in (30, 31):
        mats += [(j, kp) for kp in (28, 29, 30, 31)]
    out = np.full((2, 16, 128, 21, 128), NEG, np.float32)
    for m, (j, kp) in enumerate(mats):
        rq = (2 * j + q // 64)[:, None]
        cq = (q % 64)[:, None]
        rk = (2 * kp + key // 64)[None, :]
        ck = (key % 64)[None, :]
        rs = np.clip(rq - 4, 0, 56)
        cs = np.clip(cq - 8, 0, 48)
        valid = (rk >= rs) & (rk <= rs + 7) & (ck >= cs) & (ck <= cs + 15)
        dr = np.clip(rk - rq + 7, 0, 14)
        dc = np.clip(ck - cq + 15, 0, 30)
        g = rpb[:, :, dr, dc]
        out[:, :, :, m, :] = np.where(valid[None, None], g, np.float32(NEG))
    return out


def host_wmask():
    q = np.arange(128)[:, None]
    k = np.arange(128)[None, :]
    wm = np.zeros((128, 2, 128), np.float32)
    wm[:, 0, :] = np.where(q <= k, 0.0, NEG)
    wm[:, 1, :] = np.where(k <= q, 0.0, NEG)
    return wm


def phase_router(C, K, l, need_ctx, n_iter=34):
    nc, d, cm = C.nc, C.d, C.cm
    K.begin_phase()
    nb = NormBufs(K)
    vs = K.sbuf("vs", [32, 5, 128], F32)
    tmpv = K.sbuf("tmpv", [128, 5, 32], F32)
    gsh = K.sbuf("gsh", [128, 4, 32], F32)
    acc = [K.psum(f"acc{i}", [128, 512], F32) for i in range(2)]
    build_gsh(C, K, l, 2, gsh, vs, acc[0], tmpv)
    hTs = [K.sbuf(f"hT{i}", [128, KC, 512], BF16, partial=True) for i in range(2)]
    wr = K.sbuf("wr", [128, KC, 16], BF16)
    K.dma(K.pool, wr[:], d["moe_router"][l].rearrange("(c p) e -> p c e", p=128), W=[wr])
    E = K.sbuf("E", [16, NTOK], F32, partial=True)
    A = K.sbuf("A", [16, NTOK], F32, partial=True)
    junk = K.sbuf("junk", [16, NLAT], F32)
    tiles = TOK_TILES if need_ctx else TOK_TILES[1:]
    for ti, (tok0, ntok) in enumerate(tiles):
        hT = hTs[ti % 2]
        row = 1 if tok0 == 0 else 0
        for sb in range(ntok // 128):
            norm_block(C, K, nb, d["xm"][tok0 + sb * 128: tok0 + (sb + 1) * 128, :], gsh, row, hT, sb * 128)
        ps = acc[ti % 2]
        for kc in range(KC):
            mm(K, ps, ps[0:16, 0:ntok], wr[:, kc, :], hT[:, kc, 0:ntok], kc == 0, kc == KC - 1, R=[wr, hT])
        K.op(K.act, lambda e: e.activation(E[:, tok0:tok0 + ntok], ps[0:16, 0:ntok], AF.Exp), R=[ps], W=[E])
    for ti, (tok0, ntok) in enumerate(tiles):
        ps = acc[ti % 2]
        mm(K, ps, ps[0:16, 0:ntok], cm.ones_f[0:16, 0:16], E[:, tok0:tok0 + ntok], True, True, R=[cm.ones_f, E])
        K.op(K.dve, lambda e: e.reciprocal(A[:, tok0:tok0 + ntok], ps[0:16, 0:ntok]), R=[ps], W=[A])
        K.op(K.dve, lambda e: e.tensor_tensor(A[:, tok0:tok0 + ntok], A[:, tok0:tok0 + ntok], E[:, tok0:tok0 + ntok], ALU.mult), R=[A, E], W=[A])
    sets = ([(0, NCTX, 32)] if need_ctx else []) + [(NCTX, NLAT, 512)]
    G = K.sbuf("G", [16, NTOK], F32, partial=True)
    for si, (c0, n, cap) in enumerate(sets):
        st = K.sbuf(f"bis{si}", [16, 8], F32)
        lo, hi, mid, cnt, ge, nge, t1, t2 = [st[:, i:i + 1] for i in range(8)]
        K.op(K.dve, lambda e: e.memset(st[:], 0.0), W=[st])
        K.op(K.dve, lambda e: e.memset(hi, 2.0), R=[st], W=[st])
        Av = A[:, c0:c0 + n]
        for it in range(n_iter):
            K.op(K.dve, lambda e: e.tensor_scalar(mid, lo, hi, 0.5, ALU.add, ALU.mult), R=[st], W=[st])
            K.op(K.dve, lambda e: e.tensor_scalar(junk[:, 0:n], Av, mid, 0.0, ALU.is_ge, ALU.add, accum_out=cnt), R=[A, st], W=[junk, st])
            K.op(K.dve, lambda e: e.tensor_scalar(ge, cnt, float(cap) - 0.5, None, ALU.is_ge), R=[st], W=[st])
            K.op(K.dve, lambda e: e.tensor_scalar(nge, ge, -1.0, 1.0, ALU.mult, ALU.add), R=[st], W=[st])
            K.op(K.dve, lambda e: e.tensor_tensor(t1, mid, ge, ALU.mult), R=[st], W=[st])
            K.op(K.dve, lambda e: e.tensor_tensor(t2, mid, nge, ALU.mult), R=[st], W=[st])
            K.op(K.dve, lambda e: e.scalar_tensor_tensor(lo, lo, nge, t1, ALU.mult, ALU.add), R=[st], W=[st])
            K.op(K.dve, lambda e: e.scalar_tensor_tensor(hi, hi, ge, t2, ALU.mult, ALU.add), R=[st], W=[st])
        K.op(K.dve, lambda e: e.scalar_tensor_tensor(G[:, c0:c0 + n], Av, lo, Av, ALU.is_ge, ALU.mult), R=[A, st], W=[G])
    t0 = 0 if need_ctx else NCTX
    K.dma(K.sp, d["gm"][:, t0:NTOK], G[:, t0:NTOK], R=[G])
    K.end_phase()


def phase_moe(C, K, l, need_ctx):
    nc, d, cm = C.nc, C.d, C.cm
    K.begin_phase()
    nb = NormBufs(K)
    vs = K.sbuf("vs", [32, 5, 128], F32)
    tmpv = K.sbuf("tmpv", [128, 5, 32], F32)
    gsh = K.sbuf("gsh", [128, 4, 32], F32)
    pa = [K.psum(f"pa{i}", [128, 512], F32) for i in range(2)]
    pu = [K.psum(f"pu{i}", [128, 512], F32) for i in range(2)]
    py = [K.psum(f"py{i}", [128, 256], F32) for i in range(2)]
    build_gsh(C, K, l, 2, gsh, vs, pa[0], tmpv)
    hT = K.sbuf("hT", [128, KC, 512], BF16, partial=True)
    aT = K.sbuf("aT", [128, 32, 512], BF16, partial=True)
    wp = [K.sbuf(f"wp{i}", [128, KC, 256], BF16) for i in range(4)]
    g2 = K.sbuf("g2", [128, D], F32)
    gmb = [K.sbuf(f"gmb{i}", [128, 512], F32) for i in range(2)]
    sl = [K.sbuf(f"sl{i}", [128, 512], F32) for i in range(2)]
    tl = [K.sbuf(f"tl{i}", [128, 512], F32) for i in range(2)]
    xin = [K.sbuf(f"xin{i}", [128, 256], F32) for i in range(3)]
    yo = [K.sbuf(f"yo{i}", [128, 256], F32) for i in range(3)]
    w1, w3, w2 = d["moe_w1"][l], d["moe_w3"][l], d["moe_w2"][l]
    w2v = w2.rearrange("e (fb p) n -> p (e fb) n", p=128)
    tiles = TOK_TILES if need_ctx else TOK_TILES[1:]
    wi = 0
    ai = 0
    yi = 0
    for ti, (tok0, ntok) in enumerate(tiles):
        row = 1 if tok0 == 0 else 0
        if ti == 0 or (ti == 1 and need_ctx):
            K.dma(K.sp, g2[:], d["mod"][l][row:row + 1, 5 * D:6 * D].broadcast_to([128, D]), W=[g2])
        for sb in range(ntok // 128):
            norm_block(C, K, nb, d["xm"][tok0 + sb * 128: tok0 + (sb + 1) * 128, :], gsh, row, hT, sb * 128)
        for e in range(16):
            wa, wb_ = wp[wi % 4], wp[(wi + 1) % 4]
            wi += 2
            K.dma(K.pool, wa[:], w1[e].rearrange("(c p) f -> p c f", p=128), W=[wa])
            K.dma(K.pool, wb_[:], w3[e].rearrange("(c p) f -> p c f", p=128), W=[wb_])
            gb = gmb[e % 2]
            K.dma(K.sp, gb[:, 0:ntok], d["gm"][e:e + 1, tok0:tok0 + ntok].broadcast_to([128, ntok]), W=[gb])
            for fb in range(2):
                a, u = pa[ai % 2], pu[ai % 2]
                s, t = sl[ai % 2], tl[ai % 2]
                ai += 1
                for kc in range(KC):
                    mm(K, a, a[:, 0:ntok], wa[:, kc, fb * 128:(fb + 1) * 128], hT[:, kc, 0:ntok], kc == 0, kc == KC - 1, R=[wa, hT])
                for kc in range(KC):
                    mm(K, u, u[:, 0:ntok], wb_[:, kc, fb * 128:(fb + 1) * 128], hT[:, kc, 0:ntok], kc == 0, kc == KC - 1, R=[wb_, hT])
                K.op(K.act, lambda e_: e_.activation(s[:, 0:ntok], a[:, 0:ntok], AF.Silu), R=[a], W=[s])
                K.op(K.dve, lambda e_: e_.tensor_tensor(t[:, 0:ntok], s[:, 0:ntok], u[:, 0:ntok], ALU.mult), R=[s, u], W=[t])
                K.op(K.pool, lambda e_: e_.tensor_tensor(aT[:, e * 2 + fb, 0:ntok], t[:, 0:ntok], gb[:, 0:ntok], ALU.mult), R=[t, gb], W=[aT])
        for hg in range(16):
            cs = slice(hg * 256, (hg + 1) * 256)
            w = wp[wi % 4]
            wi += 1
            K.dma(K.pool, w[:], w2v[:, :, cs], W=[w])
            for sb in range(ntok // 128):
                ps = py[yi % 2]
                xt, y = xin[yi % 3], yo[yi % 3]
                yi += 1
                r0 = tok0 + sb * 128
                K.dma(K.sp, xt[:], d["xm"][r0:r0 + 128, cs], W=[xt])
                for c in range(32):
                    mm(K, ps, ps[:], aT[:, c, sb * 128:(sb + 1) * 128], w[:, c, :], c == 0, c == 31, R=[aT, w])
                K.op(K.dve, lambda e_: e_.tensor_tensor(y[:], ps[:], g2[:, cs], ALU.mult), R=[ps, g2], W=[y])
                K.op(K.pool, lambda e_: e_.tensor_tensor(y[:], y[:], xt[:], ALU.add), R=[y, xt], W=[y])
                K.dma(K.sp, d["xl"][r0:r0 + 128, cs], y[:], R=[y])
    K.end_phase()


def phase_final(C, K):
    nc, d, cm = C.nc, C.d, C.cm
    K.begin_phase()
    fw = K.sbuf("fw", [128, D], F32)
    K.dma(K.sp, fw[:], d["final_norm_w"][0:1, :].broadcast_to([128, D]), W=[fw])
    xts = [K.sbuf(f"fx{i}", [128, D], F32) for i in range(2)]
    jk = K.sbuf("fj", [128, D], BF16)
    yts = [K.sbuf(f"fy{i}", [128, D], F32) for i in range(2)]
    sss = [K.sbuf(f"fs{i}", [128, 2], F32) for i in range(2)]
    for i in range(NLAT // 128):
        xt, yt, ss = xts[i % 2], yts[i % 2], sss[i % 2]
        K.dma(K.sp, xt[:], d["xl"][NCTX + i * 128:NCTX + (i + 1) * 128, :], W=[xt])
        K.op(K.act, lambda e: e.activation(jk[:], xt[:], AF.Square, accum_out=ss[:, 0:1]), R=[xt], W=[jk, ss])
        K.op(K.act, lambda e: e.activation(ss[:, 1:2], ss[:, 0:1], AF.Sqrt, scale=1.0 / D, bias=cm.eps[:, 0:1]), R=[ss, cm.eps], W=[ss])
        K.op(K.dve, lambda e: e.reciprocal(ss[:, 1:2], ss[:, 1:2]), R=[ss], W=[ss])
        K.op(K.dve, lambda e: e.scalar_tensor_tensor(yt[:], xt[:], ss[:, 1:2], fw[:], ALU.mult, ALU.mult), R=[xt, ss, fw], W=[yt])
        K.dma(K.sp, d["y"][i * 128:(i + 1) * 128, :], yt[:], R=[yt])
    K.end_phase()


def ev_tm_gates(dst):
    def f(S, g, sb, ps):
        K = S.K
        st = S.stgf[S.n % 3]
        S.n += 1
        K.op(K.dve, lambda e: e.tensor_tensor(st[:, 0:32], ps[:, 0:32], S.bg[:], ALU.add), R=[ps, S.bg], W=[st])
        r0 = S.tok0 + sb * 128
        K.dma(K.sp, dst[r0:r0 + 128, :], st[:, 0:32], R=[st])
    return f


def phase_inproj_mlstm(C, K, l):
    d = C.d
    o = l // 2
    w = d["ml_w_in"][o]
    groups = []
    for i in range(4):
        groups.append(dict(kind="FM", w=w[:, i * 512:(i + 1) * 512], ncols=512, b0=4 * i, evac=ev_fm_plain(d["mqT"], 256 ** -0.5)))
    for i in range(4):
        groups.append(dict(kind="FM", w=w[:, 2048 + i * 512:2048 + (i + 1) * 512], ncols=512, b0=4 * i, evac=ev_fm_plain(d["mkT"])))
    for i in range(4):
        groups.append(dict(kind="TM", w=w[:, 2048 + i * 512:2048 + (i + 1) * 512], ncols=512, c0=512 * i, evac=ev_tm_plain(d["mK"])))
    for i in range(8):
        groups.append(dict(kind="TM", w=w[:, 4096 + i * 512:4096 + (i + 1) * 512], ncols=512, c0=512 * i, evac=ev_tm_plain(d["mV"])))
    for i in range(8):
        groups.append(dict(kind="TM", w=w[:, 8192 + i * 512:8192 + (i + 1) * 512], ncols=512, c0=512 * i, evac=ev_tm_plain(d["mO"])))
    groups.append(dict(kind="TM", w=w[:, 12288:12320], ncols=32, c0=0, evac=ev_tm_gates(d["mG"])))

    def setup(S):
        S.bg = K.sbuf("bg", [128, 32], F32)
        K.dma(K.sp, S.bg[:], d["ml_b_gates"][o:o + 1, :].broadcast_to([128, 32]), W=[S.bg])
    phase_inproj(C, K, l, groups, setup)


def phase_mlstm_scan(C, K, l, dr):
    nc, d, cm = C.nc, C.d, C.cm
    K.begin_phase()
    tri = K.sbuf("tri", [64, 4, 64], F32)
    K.dma(K.sp, tri[:], d["c_tri"][:, :, :], W=[tri])
    Cst = [K.sbuf(f"Cst{h}", [128, 2, 512], F32) for h in range(8)]
    Cb = [K.sbuf(f"Cb{h}", [128, 2, 512], BF16) for h in range(8)]
    nst = [K.sbuf(f"nst{h}", [128, 2], F32) for h in range(8)]
    nbf = [K.sbuf(f"nbf{h}", [128, 2], BF16) for h in range(8)]
    for h in range(8):
        K.op(K.dve, lambda e, h=h: e.memset(Cst[h][:], 0.0), W=[Cst[h]])
        K.op(K.pool, lambda e, h=h: e.memset(Cb[h][:], 0.0), W=[Cb[h]])
        K.op(K.dve, lambda e, h=h: e.memset(nst[h][:], 0.0), W=[nst[h]])
        K.op(K.pool, lambda e, h=h: e.memset(nbf[h][:], 0.0), W=[nbf[h]])
    qT4 = [K.sbuf(f"qT4{i}", [128, 16, 256], BF16) for i in range(2)]
    kT4 = [K.sbuf(f"kT4{i}", [128, 16, 256], BF16) for i in range(2)]
    Kt = [K.sbuf(f"Kt{i}", [64, 2048], BF16) for i in range(2)]
    Vt = [K.sbuf(f"Vt{i}", [64, D], BF16) for i in range(2)]
    Gt = [K.sbuf(f"Gt{i}", [64, 32], F32) for i in range(2)]
    hch = [K.sbuf(f"hch{i}", [64, D], F32, partial=True) for i in range(2)]
    gs = [K.sbuf(f"gs{i}", [128, 6, 8], F32, partial=True) for i in range(2)]
    smalls = [K.psum(f"psm{i}", [128, 512], F32) for i in range(2)]
    pgt = [Buf(K, f"pgt{i}", smalls[i].t[:, 0:16]) for i in range(2)]
    pst = [Buf(K, f"pst{i}", smalls[i].t[0:64, 64:128]) for i in range(2)]
    pd = [Buf(K, f"pd{i}", smalls[i].t[0:64, 128:130]) for i in range(2)]
    pdn = [Buf(K, f"pdn{i}", smalls[i].t[:, 136:138]) for i in range(2)]
    pn = [K.psum(f"pn{i}", [64, 512], F32) for i in range(2)]
    pdc = [K.psum(f"pdc{i}", [128, 512], F32) for i in range(4)]
    Pt = [K.sbuf(f"Pt{i}", [64, 64], BF16) for i in range(2)]
    Kw = [K.sbuf(f"Kw{i}", [64, 256], BF16) for i in range(2)]
    dd = [K.sbuf(f"dd{i}", [64, 2], F32) for i in range(2)]
    ic, fc = (0, 8) if dr == 0 else (16, 24)
    order = list(range(68)) if dr == 0 else [3, 2, 1, 0] + list(range(67, 3, -1))
    dst = d["hf"] if dr == 0 else d["hb"]
    cur_grp = None
    gi = 0
    si = 0
    for ci, ck in enumerate(order):
        grp = ck // 4
        if grp != cur_grp:
            cur_grp = grp
            q4, k4 = qT4[gi % 2], kT4[gi % 2]
            gi += 1
            K.dma(K.sp, q4[:], d["mqT"][:, :, grp * 256:(grp + 1) * 256].rearrange("b p t -> p b t"), W=[q4])
            K.dma(K.sp, k4[:], d["mkT"][:, :, grp * 256:(grp + 1) * 256].rearrange("b p t -> p b t"), W=[k4])
        t0 = (ck % 4) * 64
        r0 = ck * 64
        kt, vt, gt, hc, g_ = Kt[ci % 2], Vt[ci % 2], Gt[ci % 2], hch[ci % 2], gs[ci % 2]
        pg = pgt[ci % 2]
        K.dma(K.sp, kt[:], d["mK"][r0:r0 + 64, :], W=[kt])
        K.dma(K.sp, vt[:], d["mV"][r0:r0 + 64, :], W=[vt])
        K.dma(K.sp, gt[:], d["mG"][r0:r0 + 64, :], W=[gt])
        lq, vq, ev, ea, eb, eg = [g_[:, i, :] for i in range(6)]
        K.op(K.act, lambda e: e.activation(lq[0:64], gt[:, fc:fc + 8], AF.Exp, scale=-1.0), R=[gt], W=[g_])
        K.op(K.act, lambda e: e.activation(lq[0:64], lq[0:64], AF.Ln, bias=cm.ones_f[0:64, 0:1]), R=[g_, cm.ones_f], W=[g_])
        mm(K, pg, pg[0:64, 0:8], tri[:, dr, :], lq[0:64], True, True, R=[tri, g_])
        mm(K, pg, pg[:, 8:16], cm.ones_f[0:64, 0:128], lq[0:64], True, True, R=[cm.ones_f, g_])
        K.op(K.dve, lambda e: e.tensor_tensor(vq[0:64], gt[:, ic:ic + 8], pg[0:64, 0:8], ALU.add), R=[gt, pg], W=[g_])
        K.op(K.act, lambda e: e.activation(ev[0:64], vq[0:64], AF.Exp), R=[g_], W=[g_])
        K.op(K.dve, lambda e: e.tensor_tensor(ea[0:64], vq[0:64], pg[0:64, 8:16], ALU.subtract), R=[g_, pg], W=[g_])
        K.op(K.act, lambda e: e.activation(ea[0:64], ea[0:64], AF.Exp), R=[g_], W=[g_])
        K.op(K.act, lambda e: e.activation(eb[0:64], pg[0:64, 0:8], AF.Exp, scale=-1.0), R=[pg], W=[g_])
        K.op(K.act, lambda e: e.activation(eg, pg[:, 8:16], AF.Exp, scale=-1.0), R=[pg], W=[g_])
        for h in range(8):
            ps_s, ps_n, ps_d, ps_dn = pst[si % 2], pn[si % 2], pd[si % 2], pdn[si % 2]
            pc0, pc1 = pdc[(2 * si) % 4], pdc[(2 * si + 1) % 4]
            pt, kw, dq = Pt[si % 2], Kw[si % 2], dd[si % 2]
            si += 1
            qa = [q4[:, 2 * h + c, t0:t0 + 64] for c in range(2)]
            ka = [k4[:, 2 * h + c, t0:t0 + 64] for c in range(2)]
            vh = vt[:, h * 512:(h + 1) * 512]
            for c in range(2):
                mm(K, ps_s, ps_s[:, :], ka[c], qa[c], c == 0, c == 1, R=[k4, q4])
            K.op(K.dve, lambda e: e.scalar_tensor_tensor(pt[:], ps_s[:, :], ev[0:64, h:h + 1], tri[:, dr, :], ALU.mult, ALU.mult), R=[ps_s, g_, tri], W=[pt])
            for c in range(2):
                mm(K, ps_n, ps_n[:, :], qa[c], Cb[h][:, c, :], c == 0, False, R=[q4, Cb[h]])
            mm(K, ps_n, ps_n[:, :], pt[:], vh, False, True, R=[pt, vt])
            for c in range(2):
                mm(K, ps_d, ps_d[:, 0:1], qa[c], nbf[h][:, c:c + 1], c == 0, False, R=[q4, nbf[h]])
            mm(K, ps_d, ps_d[:, 0:1], pt[:], cm.ones_b[0:64, 0:1], False, True, R=[pt, cm.ones_b])
            K.op(K.dve, lambda e: e.tensor_scalar(dq[:, 0:1], ps_d[:, 0:1], eb[0:64, h:h + 1], None, ALU.mult), R=[ps_d, g_], W=[dq])
            K.op(K.dve, lambda e: e.tensor_scalar(dq[:, 1:2], ps_d[:, 0:1], eb[0:64, h:h + 1], -1.0, ALU.mult, ALU.mult), R=[ps_d, g_], W=[dq])
            K.op(K.dve, lambda e: e.scalar_tensor_tensor(dq[:, 0:1], dq[:, 0:1], 1.0, dq[:, 1:2], ALU.max, ALU.max), R=[dq], W=[dq])
            K.op(K.dve, lambda e: e.reciprocal(dq[:, 0:1], dq[:, 0:1]), R=[dq], W=[dq])
            K.op(K.dve, lambda e: e.tensor_tensor(dq[:, 1:2], dq[:, 0:1], eb[0:64, h:h + 1], ALU.mult), R=[dq, g_], W=[dq])
            K.op(K.act, lambda e: e.activation(hc[:, h * 512:(h + 1) * 512], ps_n[:, :], AF.Copy, scale=dq[:, 1:2]), R=[ps_n, dq], W=[hc])
            K.op(K.pool, lambda e: e.tensor_scalar(kw[:], kt[:, h * 256:(h + 1) * 256], ea[0:64, h:h + 1], None, ALU.mult), R=[kt, g_], W=[kw])
            mm(K, pc0, pc0[:, :], kw[:, 0:128], vh, True, True, R=[kw, vt])
            mm(K, pc1, pc1[:, :], kw[:, 128:256], vh, True, True, R=[kw, vt])
            for c in range(2):
                mm(K, ps_dn, ps_dn[:, c:c + 1], kw[:, c * 128:(c + 1) * 128], cm.ones_b[0:64, 0:1], True, True, R=[kw, cm.ones_b])
            K.op(K.dve, lambda e: e.scalar_tensor_tensor(Cst[h][:, 0, :], Cst[h][:, 0, :], eg[:, h:h + 1], pc0[:, :], ALU.mult, ALU.add), R=[Cst[h], g_, pc0], W=[Cst[h]])
            K.op(K.dve, lambda e: e.scalar_tensor_tensor(Cst[h][:, 1, :], Cst[h][:, 1, :], eg[:, h:h + 1], pc1[:, :], ALU.mult, ALU.add), R=[Cst[h], g_, pc1], W=[Cst[h]])
            K.op(K.act, lambda e: e.copy(Cb[h][:], Cst[h][:]), R=[Cst[h]], W=[Cb[h]])
            K.op(K.dve, lambda e: e.scalar_tensor_tensor(nst[h][:], nst[h][:], eg[:, h:h + 1], ps_dn[:, :], ALU.mult, ALU.add), R=[nst[h], g_, ps_dn], W=[nst[h]])
            K.op(K.pool, lambda e: e.tensor_copy(nbf[h][:], nst[h][:]), R=[nst[h]], W=[nbf[h]])
        K.dma(K.sp, dst[r0:r0 + 64, :], hc[:], R=[hc])
    K.end_phase()


def phase_mlstm_post(C, K, l, need_ctx):
    nc, d, cm = C.nc, C.d, C.cm
    o = l // 2
    K.begin_phase()
    nwb = K.sbuf("nwb", [128, D], F32)
    K.dma(K.sp, nwb[:], d["ml_norm_w"][o:o + 1, :].broadcast_to([128, D]), W=[nwb])
    hf = [K.sbuf(f"hf{i}", [128, D], F32) for i in range(2)]
    hb = [K.sbuf(f"hb{i}", [128, D], F32) for i in range(2)]
    ot = [K.sbuf(f"ot{i}", [128, D], BF16) for i in range(2)]
    sg = K.sbuf("sg", [128, D], F32)
    jk = K.sbuf("jk", [128, 512], BF16)
    hnb = K.sbuf("hnb", [128, D], BF16)
    ss = K.sbuf("ss", [128, 2, 8], F32, partial=True)
    stg = [K.sbuf(f"stg{i}", [128, KC, 512], BF16, partial=True) for i in range(2)]
    tp = [K.psum(f"tp{i}", [128, 1024], BF16) for i in range(2)]
    tiles = TOK_TILES if need_ctx else TOK_TILES[1:]
    bi = 0
    ti_ = 0
    for ti, (tok0, ntok) in enumerate(tiles):
        st = stg[ti % 2]
        for sb in range(ntok // 128):
            r0 = tok0 + sb * 128
            a, b, og = hf[bi % 2], hb[bi % 2], ot[bi % 2]
            bi += 1
            K.dma(K.sp, a[:], d["hf"][r0:r0 + 128, :], W=[a])
            K.dma(K.sp, b[:], d["hb"][r0:r0 + 128, :], W=[b])
            K.dma(K.sp, og[:], d["mO"][r0:r0 + 128, :], W=[og])
            K.op(K.pool, lambda e: e.tensor_tensor(a[:], a[:], b[:], ALU.add), R=[a, b], W=[a])
            for h in range(8):
                K.op(K.act, lambda e, h=h: e.activation(jk[:], a[:, h * 512:(h + 1) * 512], AF.Square, accum_out=ss[:, 0, h:h + 1]), R=[a], W=[jk, ss])
            K.op(K.act, lambda e: e.activation(ss[:, 1, :], ss[:, 0, :], AF.Sqrt, scale=1.0 / 512, bias=cm.eps[:, 0:1]), R=[ss, cm.eps], W=[ss])
            K.op(K.dve, lambda e: e.reciprocal(ss[:, 1, :], ss[:, 1, :]), R=[ss], W=[ss])
            K.op(K.act, lambda e: e.activation(sg[:], og[:], AF.Sigmoid), R=[og], W=[sg])
            for h in range(8):
                hs = slice(h * 512, (h + 1) * 512)
                K.op(K.dve, lambda e, h=h, hs=hs: e.scalar_tensor_tensor(a[:, hs], a[:, hs], ss[:, 1, h:h + 1], nwb[:, hs], ALU.mult, ALU.mult), R=[a, ss, nwb], W=[a])
            K.op(K.pool, lambda e: e.tensor_tensor(hnb[:], a[:], sg[:], ALU.mult), R=[a, sg], W=[hnb])
            for g in range(4):
                t = tp[ti_ % 2]
                ti_ += 1
                for c in range(8):
                    ch = g * 8 + c
                    K.op(K.pe, lambda e, c=c, ch=ch, t=t: e.transpose(t[:, c * 128:(c + 1) * 128], hnb[:, ch * 128:(ch + 1) * 128], cm.ident_b[:]), R=[hnb, cm.ident_b], W=[t])
                o_ap = st[:, g * 8:(g + 1) * 8, sb * 128:(sb + 1) * 128]
                i_ap = t[:, :].rearrange("p (c q) -> p c q", c=8)
                if g % 2 == 0:
                    K.op(K.act, lambda e, o_ap=o_ap, i_ap=i_ap: e.copy(o_ap, i_ap), R=[t], W=[st])
                else:
                    K.op(K.dve, lambda e, o_ap=o_ap, i_ap=i_ap: e.tensor_copy(o_ap, i_ap), R=[t], W=[st])
        K.dma(K.sp, d["OT"][:, :, tok0:tok0 + ntok].rearrange("k p t -> p k t"), st[:, :, 0:ntok], R=[st])
    K.end_phase()


def host_tri():
    s = np.arange(64)[:, None]
    t = np.arange(64)[None, :]
    tri = np.zeros((64, 4, 64), np.float32)
    tri[:, 0, :] = (s <= t)
    tri[:, 1, :] = (s >= t)
    return tri


def build_full():
    C = Prog()
    C.declare()
    nc = C.nc
    k = K(nc)
    d = C.d
    with nc.Block():
        setup_common(C, k)
        phase_mods(C, k, [0, 1, 2, 3])
        for l in range(DEPTH):
            need_ctx = l < DEPTH - 1
            if l % 2 == 0:
                phase_inproj_attn(C, k, l)
                phase_na(C, k, l, need_ctx)
                phase_win(C, k, l, need_ctx)
                phase_outproj(C, k, l, d["ab_w_out"][l // 2], need_ctx)
            else:
                phase_inproj_mlstm(C, k, l)
                phase_mlstm_scan(C, k, l, 0)
                phase_mlstm_scan(C, k, l, 1)
                phase_mlstm_post(C, k, l, need_ctx)
                phase_outproj(C, k, l, d["ml_w_out"][l // 2], need_ctx)
            phase_router(C, k, l, need_ctx)
            phase_moe(C, k, l, need_ctx)
        phase_final(C, k)
    return C, k


def kernel(x, c, ctx, c_ctx, ada_w, ada_b, norm1_w, norm2_w, ab_w_in, ab_w_out, na_rpb, win_sink,
           ml_w_in, ml_b_gates, ml_norm_w, ml_w_out, moe_router, moe_w1, moe_w3, moe_w2, final_norm_w):
    f = lambda a: np.ascontiguousarray(np.asarray(a, dtype=np.float32))
    C, k = build_full()
    hc = host_consts()
    shared = {
        "ada_w": f(ada_w), "ada_b": f(ada_b), "norm1_w": f(norm1_w), "norm2_w": f(norm2_w),
        "ab_w_in": f(ab_w_in), "ab_w_out": f(ab_w_out), "win_sink": f(win_sink),
        "ml_w_in": f(ml_w_in), "ml_b_gates": f(ml_b_gates), "ml_norm_w": f(ml_norm_w), "ml_w_out": f(ml_w_out),
        "moe_router": f(moe_router), "moe_w1": f(moe_w1), "moe_w3": f(moe_w3), "moe_w2": f(moe_w2),
        "final_norm_w": f(final_norm_w).reshape(1, D),
        "c_ident": hc["c_ident"], "c_perm": hc["c_perm"], "c_cos": hc["c_cos"], "c_sin": hc["c_sin"],
        "c_bm": host_bm(f(na_rpb)), "c_wmask": host_wmask(), "c_tri": host_tri(),
    }
    x, c, ctx, c_ctx = f(x), f(c), f(ctx), f(c_ctx)
    B = x.shape[0]
    in_maps = []
    for s in range(B):
        m = dict(shared)
        m["x"] = x[s]
        m["ctx"] = ctx[s]
        m["cvec"] = np.stack([c[s], c_ctx]).astype(np.float32)
        in_maps.append(m)
    res = run_bass_kernel_spmd(C.nc, in_maps, core_ids=list(range(B)))
    return np.stack([res.results[s]["y"] for s in range(B)]).astype(np.float32)
```

```python
import numpy as np
import ml_dtypes
from contextlib import ExitStack
import concourse.bass as bass
import concourse.mybir as mybir
from concourse.bass_utils import run_bass_kernel_spmd

F32 = mybir.dt.float32
BF16 = mybir.dt.bfloat16
AF = mybir.ActivationFunctionType
ALU = mybir.AluOpType
AX = mybir.AxisListType

D = 4096
NTOK = 4352
NCTX = 256
NLAT = 4096
DEPTH = 4
KC = 32
EPS = 1e-6
NEG = -30000.0


class Eng:
    def __init__(self, k, name, e, sem):
        self.k, self.name, self.e, self.sem = k, name, e, sem
        self.seq = 0
        self.insts = {}
        self.tick = []
        self.count = 0
        self.known = {}

    def ticket(self, seq):
        for s, c in reversed(self.tick[-64:]):
            if s < seq:
                break
        lo = None
        for s, c in reversed(self.tick):
            if s >= seq:
                lo = c
            else:
                break
        if lo is not None:
            return lo
        if seq not in self.insts:
            seq = max(self.insts)
        ins = self.insts[seq]
        self.count += 1
        ins.then_inc(self.sem, 1)
        self.tick.append((seq, self.count))
        if len(self.tick) > 256:
            self.tick = self.tick[-128:]
        for s in [s for s in self.insts if s <= seq]:
            del self.insts[s]
        return self.count


class DmaSem:
    def __init__(self, sem):
        self.sem = sem
        self.issued = 0


class Buf:
    def __init__(self, k, name, t, partial=False):
        self.k, self.name, self.t, self.partial = k, name, t, partial
        self.w = {}
        self.r = {}
        self.dsem = None

    def __getitem__(self, idx):
        return self.t[idx]


class K:
    def __init__(self, nc):
        self.nc = nc
        self.stack = ExitStack()
        self.pe = Eng(self, "pe", nc.tensor, self._sem("s_pe"))
        self.act = Eng(self, "act", nc.scalar, self._sem("s_act"))
        self.dve = Eng(self, "dve", nc.vector, self._sem("s_dve"))
        self.pool = Eng(self, "pool", nc.gpsimd, self._sem("s_pool"))
        self.sp = Eng(self, "sp", nc.sync, self._sem("s_sp"))
        self.engs = [self.pe, self.act, self.dve, self.pool, self.sp]
        self.dsem_free = [DmaSem(self._sem(f"s_dma{i}")) for i in range(64)]
        self.bar = self._sem("s_bar")
        self.bar_n = 0
        self.phase_stack = None
        self.phase_bufs = []
        self.n_inst = 0

    def _sem(self, name):
        return self.stack.enter_context(self.nc.semaphore(name))

    def begin_phase(self):
        self.phase_stack = ExitStack()
        self.phase_bufs = []

    def sbuf(self, name, shape, dt, partial=False):
        self.uid = getattr(self, "uid", 0) + 1
        t = self.phase_stack.enter_context(self.nc.sbuf_tensor(f"{name}_u{self.uid}", list(shape), dt))
        b = Buf(self, name, t, partial)
        self.phase_bufs.append(b)
        return b

    def psum(self, name, shape, dt, partial=False):
        self.uid = getattr(self, "uid", 0) + 1
        t = self.phase_stack.enter_context(self.nc.psum_tensor(f"{name}_u{self.uid}", list(shape), dt))
        b = Buf(self, name, t, partial)
        self.phase_bufs.append(b)
        return b

    def end_phase(self):
        for b in self.phase_bufs:
            if b.dsem is not None:
                self._wait(self.sp, b.dsem.sem, 16 * b.dsem.issued)
        self.barrier()
        for b in self.phase_bufs:
            if b.dsem is not None:
                self.dsem_free.append(b.dsem)
                b.dsem = None
        self.phase_stack.close()
        self.phase_stack = None
        self.phase_bufs = []

    def barrier(self):
        for e in self.engs:
            if e.seq > 0 and e is not self.sp:
                last = e.seq
                if last in e.insts or any(s >= last for s, _ in e.tick):
                    t = e.ticket(last)
                    self._wait(e, e.sem, t)
        self.bar_n += 1
        for e in self.engs:
            e.e.sem_inc(self.bar, 1)
        for e in self.engs:
            e.e.wait_ge(self.bar, len(self.engs) * self.bar_n)

    def _wait(self, eng, sem, val):
        key = id(sem)
        if eng.known.get(key, 0) >= val:
            return
        eng.known[key] = val
        eng.e.wait_ge(sem, val)

    def _wait_dep(self, eng, key, val, same_engine_ok):
        if isinstance(key, Eng):
            if key is eng and not same_engine_ok:
                return
            if key is eng and key is self.pe:
                return
            t = key.ticket(val)
            self._wait(eng, key.sem, t)
        else:
            b = key[1]
            if b.dsem is None:
                return
            self._wait(eng, b.dsem.sem, 16 * b.dsem.issued)

    def _deps(self, eng, R, W):
        for b in R:
            for key, val in list(b.w.items()):
                self._wait_dep(eng, key, val, same_engine_ok=True)
        for b in W:
            if not b.partial:
                for key, val in list(b.w.items()):
                    self._wait_dep(eng, key, val, same_engine_ok=False)
            for key, val in list(b.r.items()):
                self._wait_dep(eng, key, val, same_engine_ok=False)

    def _record(self, key, val, R, W):
        for b in W:
            if b.partial:
                b.w[key] = val
            else:
                b.w = {key: val}
                b.r = {}
        for b in R:
            b.r[key] = val

    def op(self, eng, fn, R=(), W=()):
        self._deps(eng, R, W)
        ins = fn(eng.e)
        eng.seq += 1
        eng.insts[eng.seq] = ins
        if len(eng.insts) > 4096:
            for s in sorted(eng.insts)[:2048]:
                del eng.insts[s]
        self._record(eng, eng.seq, R, W)
        self.n_inst += 1
        return ins

    def dma(self, eng, out, in_, R=(), W=(), **kw):
        self._deps(eng, R, W)
        sb = (list(W) + list(R))[0]
        if sb.dsem is None:
            sb.dsem = self.dsem_free.pop()
        ins = eng.e.dma_start(out=out, in_=in_, **kw)
        ins.then_inc(sb.dsem.sem, 16)
        sb.dsem.issued += 1
        self._record(("dma", sb), True, R, W)
        self.n_inst += 1
        return ins


TOK_TILES = [(0, 256)] + [(256 + 512 * i, 512) for i in range(8)]


class Prog:
    def __init__(self, kinds=None, layers=(0, 1, 2, 3), only=None, shapes=None):
        self.nc = nc = bass.Bass("TRN2", target_bir_lowering=False)
        self.kinds = kinds or {}
        self.layers = layers
        self.only = only
        self.shapes = shapes or {}
        self.d = {}

    def dram(self, name, shape, dt, kind="Internal"):
        kind = self.kinds.get(name, kind)
        if kind == "ExternalInput" and self.only is not None and name not in self.only:
            return None
        shape = self.shapes.get(name, shape)
        self.d[name] = self.nc.dram_tensor(name, list(shape), dt, kind=kind).ap()
        return self.d[name]

    def declare(self):
        I = "ExternalInput"
        d = self.dram
        d("x", [NLAT, D], F32, I); d("ctx", [NCTX, D], F32, I); d("cvec", [2, D], F32, I)
        d("ada_w", [DEPTH, D, 6 * D], F32, I); d("ada_b", [DEPTH, 6 * D], F32, I)
        d("norm1_w", [DEPTH, D], F32, I); d("norm2_w", [DEPTH, D], F32, I)
        d("ab_w_in", [2, D, 9216], F32, I); d("ab_w_out", [2, D, D], F32, I)
        d("win_sink", [2, 16], F32, I)
        d("ml_w_in", [2, D, 12320], F32, I); d("ml_b_gates", [2, 32], F32, I)
        d("ml_norm_w", [2, D], F32, I); d("ml_w_out", [2, D, D], F32, I)
        d("moe_router", [DEPTH, D, 16], F32, I)
        d("moe_w1", [DEPTH, 16, D, 256], F32, I); d("moe_w3", [DEPTH, 16, D, 256], F32, I)
        d("moe_w2", [DEPTH, 16, 256, D], F32, I)
        d("final_norm_w", [1, D], F32, I)
        d("c_ident", [128, 128], F32, I)
        d("c_bm", [2, 16, 128, 21, 128], F32, I)
        d("c_cos", [128, NLAT], F32, I); d("c_sin", [128, NLAT], F32, I); d("c_perm", [128, 128], F32, I)
        d("c_wmask", [128, 2, 128], F32, I)
        d("c_tri", [64, 4, 64], F32, I)
        d("y", [NLAT, D], F32, "ExternalOutput")
        d("xl", [NTOK, D], F32); d("xm", [NTOK, D], F32); d("mod", [DEPTH, 2, 6 * D], F32)
        d("QaT", [16, 128, NTOK], BF16); d("KaT", [16, 128, NTOK], BF16); d("Va", [NTOK, 2048], BF16)
        d("QbT", [16, 128, NTOK], BF16); d("KbT", [4, 128, NTOK], BF16); d("Vb", [NTOK, 512], BF16)
        d("OT", [32, 128, NTOK], BF16)
        d("gm", [16, NTOK], F32)
        d("mqT", [16, 128, NTOK], BF16); d("mkT", [16, 128, NTOK], BF16)
        d("mK", [NTOK, 2048], BF16); d("mV", [NTOK, D], BF16); d("mO", [NTOK, D], BF16)
        d("mG", [NTOK, 32], F32); d("hf", [NTOK, D], F32); d("hb", [NTOK, D], F32)


def mm(K, ps_buf, out_ap, lhsT, rhs, start, stop, R):
    return K.op(K.pe, lambda e: e.matmul(out_ap, lhsT, rhs, start=start, stop=stop), R=R, W=[ps_buf])


class Common:
    pass


def setup_common(C, K, copy_inputs=True):
    nc = C.nc
    st = K.stack
    cm = Common()
    def gbuf(name, shape, dt):
        t = st.enter_context(nc.sbuf_tensor(name, list(shape), dt))
        return Buf(K, name, t)
    cm.ident_f = gbuf("ident_f", [128, 128], F32)
    cm.ident_b = gbuf("ident_b", [128, 128], BF16)
    cm.ones_b = gbuf("ones_b", [128, 128], BF16)
    cm.ones_f = gbuf("ones_f", [128, 128], F32)
    K.dma(K.sp, cm.ident_f[:], C.d["c_ident"][:, :], W=[cm.ident_f])
    K.dma(K.pool, cm.ident_b[:], C.d["c_ident"][:, :], W=[cm.ident_b])
    K.op(K.dve, lambda e: e.memset(cm.ones_b[:], 1.0), W=[cm.ones_b])
    K.op(K.dve, lambda e: e.memset(cm.ones_f[:], 1.0), W=[cm.ones_f])
    cm.eps = gbuf("eps_t", [128, 1], F32)
    K.op(K.dve, lambda e: e.memset(cm.eps[:], EPS), W=[cm.eps])
    C.cm = cm
    if not copy_inputs:
        return
    tmp = gbuf("cp_sem_holder", [1, 2], F32)
    K.dma(K.sp, C.d["xl"][0:NCTX, :], C.d["ctx"][:, :], W=[tmp])
    K.dma(K.sp, C.d["xl"][NCTX:NTOK, :], C.d["x"][:, :], W=[tmp])
    K._wait(K.sp, tmp.dsem.sem, 16 * tmp.dsem.issued)
    K.barrier()


def load_vecs_pp(C, K, rows, out_buf, scratch_v, ps_buf):
    cm = C.cm
    n = len(rows)
    for j, r in enumerate(rows):
        K.dma(K.sp, scratch_v[0:32, j, :], r.rearrange("(c p) -> c p", p=128), W=[scratch_v])
    for j in range(n):
        K.op(K.pe, lambda e, j=j: e.transpose(ps_buf[:, j * 32:(j + 1) * 32], scratch_v[0:32, j, :], cm.ident_f[0:32, 0:32]),
             R=[scratch_v, cm.ident_f], W=[ps_buf])
    K.op(K.dve, lambda e: e.tensor_copy(out_buf[:, 0:n, :].rearrange("p j c -> p (j c)"), ps_buf[:, 0:n * 32]), R=[ps_buf], W=[out_buf])


def phase_mods(C, K, layers):
    nc, d, cm = C.nc, C.d, C.cm
    K.begin_phase()
    vs = K.sbuf("vs", [32, 2, 128], F32)
    ps = K.psum("ps_m", [128, 512], F32)
    cpp = K.sbuf("cpp", [128, 2, 32], F32)
    scT = K.sbuf("scT", [128, 32, 2], BF16)
    load_vecs_pp(C, K, [d["cvec"][0], d["cvec"][1]], cpp, vs, ps)
    K.op(K.act, lambda e: e.activation(scT[:].rearrange("p c r -> p r c"), cpp[:], AF.Silu), R=[cpp], W=[scT])
    wbs = [K.sbuf(f"wb{i}", [128, KC, 512], BF16) for i in range(2)]
    bbs = [K.sbuf(f"bb{i}", [2, 512], F32) for i in range(2)]
    obs = [K.sbuf(f"ob{i}", [2, 512], F32) for i in range(2)]
    pss = [K.psum(f"ps_mod{i}", [128, 512], F32) for i in range(2)]
    it = 0
    for l in layers:
        for g in range(48):
            wb, bb, ob, pq = wbs[it % 2], bbs[it % 2], obs[it % 2], pss[it % 2]
            it += 1
            cs = slice(g * 512, (g + 1) * 512)
            K.dma(K.pool, wb[:], d["ada_w"][l][:, cs].rearrange("(c p) n -> p c n", p=128), W=[wb])
            K.dma(K.sp, bb[:], d["ada_b"][l:l + 1, cs].partition_broadcast(2) if False else d["ada_b"][l:l + 1, cs].broadcast_to([2, 512]), W=[bb])
            for kc in range(KC):
                mm(K, pq, pq[0:2, :], scT[:, kc, :], wb[:, kc, :], kc == 0, kc == KC - 1, R=[scT, wb])
            K.op(K.dve, lambda e, ob=ob, pq=pq, bb=bb: e.tensor_tensor(ob[:], pq[0:2, :], bb[:], ALU.add), R=[pq, bb], W=[ob])
            K.dma(K.sp, d["mod"][l][:, cs], ob[:], R=[ob])
    K.end_phase()


class NormBufs:
    def __init__(self, K, tag=""):
        self.xt = K.sbuf("nb_xt" + tag, [128, D], F32)
        self.xs = K.sbuf("nb_xs" + tag, [128, D], BF16)
        self.ss = K.sbuf("nb_ss" + tag, [128, 2], F32)
        self.tp = [K.psum(f"nb_tp{i}" + tag, [128, 1024], BF16) for i in range(2)]
        self.n = 0


def norm_block(C, K, nb, src_rows, gsh, row, hT, col0):
    cm = C.cm
    xt, xs, ss = nb.xt, nb.xs, nb.ss
    K.dma(K.sp, xt[:], src_rows, W=[xt])
    K.op(K.act, lambda e: e.activation(xs[:], xt[:], AF.Square, accum_out=ss[:, 0:1]), R=[xt], W=[xs, ss])
    K.op(K.act, lambda e: e.activation(ss[:, 1:2], ss[:, 0:1], AF.Sqrt, scale=1.0 / D, bias=cm.eps[:, 0:1]), R=[ss, cm.eps], W=[ss])
    K.op(K.dve, lambda e: e.reciprocal(ss[:, 1:2], ss[:, 1:2]), R=[ss], W=[ss])
    K.op(K.act, lambda e: e.activation(xs[:].rearrange("t (c p) -> t c p", p=128), xt[:].rearrange("t (p c) -> t c p", c=KC), AF.Copy, scale=ss[:, 1:2]), R=[xt, ss], W=[xs])
    for g in range(4):
        tp = nb.tp[nb.n % 2]
        nb.n += 1
        for c in range(8):
            ch = g * 8 + c
            K.op(K.pe, lambda e, c=c, ch=ch, tp=tp: e.transpose(tp[:, c * 128:(c + 1) * 128], xs[:, ch * 128:(ch + 1) * 128], cm.ident_b[:]),
                 R=[xs, cm.ident_b], W=[tp])
        for c in range(8):
            ch = g * 8 + c
            o = hT[:, ch, col0:col0 + 128]
            i = tp[:, c * 128:(c + 1) * 128]
            gs = gsh[:, 2 * row, ch:ch + 1]
            sh = gsh[:, 2 * row + 1, ch:ch + 1]
            if g % 2 == 0:
                K.op(K.act, lambda e, o=o, i=i, gs=gs, sh=sh: e.activation(o, i, AF.Identity, scale=gs, bias=sh), R=[tp, gsh], W=[hT])
            else:
                K.op(K.dve, lambda e, o=o, i=i, gs=gs, sh=sh: e.tensor_scalar(o, i, gs, sh, ALU.mult, ALU.add), R=[tp, gsh], W=[hT])


def build_gsh(C, K, l, which, gsh, vs, ps, tmp):
    d = C.d
    nw = d["norm1_w"] if which == 1 else d["norm2_w"]
    o = 0 if which == 1 else 3
    m = d["mod"][l]
    rows = [nw[l], m[0, (o + 1) * D:(o + 2) * D], m[0, o * D:(o + 1) * D], m[1, (o + 1) * D:(o + 2) * D], m[1, o * D:(o + 1) * D]]
    for j, rw in enumerate(rows):
        K.dma(K.sp, tmp[:, j, :], rw.rearrange("(p c) -> p c", c=KC), W=[tmp])
    for r in range(2):
        K.op(K.dve, lambda e, r=r: e.scalar_tensor_tensor(gsh[:, 2 * r, :], tmp[:, 1 + 2 * r, :], 1.0, tmp[:, 0, :], ALU.add, ALU.mult),
             R=[tmp], W=[gsh])
        K.op(K.dve, lambda e, r=r: e.tensor_copy(gsh[:, 2 * r + 1, :], tmp[:, 2 + 2 * r, :]), R=[tmp], W=[gsh])


def phase_inproj(C, K, l, groups, extra_setup=None):
    nc, d, cm = C.nc, C.d, C.cm
    K.begin_phase()
    S = type("S", (), {})()
    nb = NormBufs(K)
    vs = K.sbuf("vs", [32, 5, 128], F32)
    tmpv = K.sbuf("tmpv", [128, 5, 32], F32)
    gsh = K.sbuf("gsh", [128, 4, 32], F32)
    acc = [K.psum(f"acc{i}", [128, 512], F32) for i in range(4)]
    S.rp = [K.psum(f"rp{i}", [128, 512], F32) for i in range(2)]
    build_gsh(C, K, l, 1, gsh, vs, acc[0], tmpv)
    hTs = [K.sbuf(f"hT{i}", [128, KC, 512], BF16, partial=True) for i in range(2)]
    wbs = [K.sbuf(f"wb{i}", [128, KC, 512], BF16) for i in range(2)]
    S.stg = [K.sbuf(f"stg{i}", [128, 512], BF16) for i in range(4)]
    S.stgf = [K.sbuf(f"stgf{i}", [128, 512], F32) for i in range(3)]
    S.n = 0
    S.K, S.C = K, C
    if extra_setup:
        extra_setup(S)
    wi = 0
    ai = 0
    for ti, (tok0, ntok) in enumerate(TOK_TILES):
        hT = hTs[ti % 2]
        row = 1 if ti == 0 else 0
        nsub = ntok // 128
        for sb in range(nsub):
            norm_block(C, K, nb, d["xl"][tok0 + sb * 128: tok0 + (sb + 1) * 128, :], gsh, row, hT, sb * 128)
        S.tok0, S.ntok, S.is_ctx = tok0, ntok, ti == 0
        if hasattr(S, "tile_setup"):
            S.tile_setup(S)
        for g in groups:
            wb = wbs[wi % 2]
            wi += 1
            ncols = g["ncols"]
            K.dma(K.pool, wb[:, :, 0:ncols], g["w"].rearrange("(p c) n -> p c n", c=KC), W=[wb])
            if g["kind"] == "FM":
                for b in range(ncols // 128):
                    ps = acc[ai % 4]
                    ai += 1
                    for kc in range(KC):
                        mm(K, ps, ps[:, 0:ntok], wb[:, kc, b * 128:(b + 1) * 128], hT[:, kc, 0:ntok], kc == 0, kc == KC - 1, R=[wb, hT])
                    g["evac"](S, g, b, ps)
            else:
                for sb in range(nsub):
                    ps = acc[ai % 4]
                    ai += 1
                    for kc in range(KC):
                        mm(K, ps, ps[:, 0:ncols], hT[:, kc, sb * 128:(sb + 1) * 128], wb[:, kc, 0:ncols], kc == 0, kc == KC - 1, R=[wb, hT])
                    g["evac"](S, g, sb, ps)
    K.end_phase()


def ev_fm_plain(dst, scale=None):
    def f(S, g, b, ps):
        K = S.K
        st = S.stg[S.n % 4]
        S.n += 1
        n = S.ntok
        if scale is None:
            K.op(K.act, lambda e: e.copy(st[:, 0:n], ps[:, 0:n]), R=[ps], W=[st])
        else:
            K.op(K.act, lambda e: e.activation(st[:, 0:n], ps[:, 0:n], AF.Copy, scale=scale), R=[ps], W=[st])
        K.dma(K.sp, dst[g["b0"] + b, :, S.tok0:S.tok0 + n], st[:, 0:n], R=[st])
    return f


def ev_tm_plain(dst, dt=BF16):
    def f(S, g, sb, ps):
        K = S.K
        nco = g["ncols"]
        if dt == BF16:
            st = S.stg[S.n % 4]
        else:
            st = S.stgf[S.n % 3]
        S.n += 1
        if S.n % 2 == 0:
            K.op(K.act, lambda e: e.copy(st[:, 0:nco], ps[:, 0:nco]), R=[ps], W=[st])
        else:
            K.op(K.dve, lambda e: e.tensor_copy(st[:, 0:nco], ps[:, 0:nco]), R=[ps], W=[st])
        r0 = S.tok0 + sb * 128
        K.dma(K.sp, dst[r0:r0 + 128, g["c0"]:g["c0"] + nco], st[:, 0:nco], R=[st])
    return f


def ev_fm_rope(dst, scale):
    plain = ev_fm_plain(dst, scale)
    def f(S, g, b, ps):
        K, C = S.K, S.C
        if S.is_ctx:
            return plain(S, g, b, ps)
        n = S.ntok
        xs = S.stgf[S.n % 3]
        t1 = S.stgf[(S.n + 1) % 3]
        t2 = S.stgf[(S.n + 2) % 3]
        st = S.stg[S.n % 4]
        rp = S.rp[S.n % 2]
        S.n += 1
        K.op(K.act, lambda e: e.activation(xs[:, 0:n], ps[:, 0:n], AF.Copy, scale=(1.0 if scale is None else scale)), R=[ps], W=[xs])
        mm(K, rp, rp[:, 0:n], S.perm[:], xs[:, 0:n], True, True, R=[S.perm, xs])
        K.op(K.dve, lambda e: e.tensor_tensor(t1[:, 0:n], xs[:, 0:n], S.cos[:, 0:n], ALU.mult), R=[xs, S.cos], W=[t1])
        K.op(K.dve, lambda e: e.tensor_tensor(t2[:, 0:n], rp[:, 0:n], S.sin[:, 0:n], ALU.mult), R=[rp, S.sin], W=[t2])
        K.op(K.dve, lambda e: e.tensor_tensor(st[:, 0:n], t1[:, 0:n], t2[:, 0:n], ALU.add), R=[t1, t2], W=[st])
        K.dma(K.sp, dst[g["b0"] + b, :, S.tok0:S.tok0 + n], st[:, 0:n], R=[st])
    return f


def phase_inproj_attn(C, K, l):
    d = C.d
    e = l // 2
    w = d["ab_w_in"][e]
    qs = 128 ** -0.5
    groups = []
    for i in range(4):
        groups.append(dict(kind="FM", w=w[:, i * 512:(i + 1) * 512], ncols=512, b0=4 * i, evac=ev_fm_plain(d["QaT"], qs)))
    for i in range(4):
        groups.append(dict(kind="FM", w=w[:, 2048 + i * 512:2048 + (i + 1) * 512], ncols=512, b0=4 * i, evac=ev_fm_plain(d["KaT"])))
    for i in range(4):
        groups.append(dict(kind="TM", w=w[:, 4096 + i * 512:4096 + (i + 1) * 512], ncols=512, c0=512 * i, evac=ev_tm_plain(d["Va"])))
    for i in range(4):
        groups.append(dict(kind="FM", w=w[:, 6144 + i * 512:6144 + (i + 1) * 512], ncols=512, b0=4 * i, evac=ev_fm_rope(d["QbT"], qs)))
    groups.append(dict(kind="FM", w=w[:, 8192:8704], ncols=512, b0=0, evac=ev_fm_rope(d["KbT"], None)))
    groups.append(dict(kind="TM", w=w[:, 8704:9216], ncols=512, c0=0, evac=ev_tm_plain(d["Vb"])))

    def setup(S):
        S.perm = K.sbuf("perm", [128, 128], F32)
        K.dma(K.sp, S.perm[:], d["c_perm"][:, :], W=[S.perm])
        S.cos = K.sbuf("cos", [128, 512], F32)
        S.sin = K.sbuf("sin", [128, 512], F32)

        def tile_setup(S):
            if not S.is_ctx:
                p0 = S.tok0 - NCTX
                K.dma(K.sp, S.cos[:], d["c_cos"][:, p0:p0 + 512], W=[S.cos])
                K.dma(K.sp, S.sin[:], d["c_sin"][:, p0:p0 + 512], W=[S.sin])
        S.tile_setup = tile_setup
    phase_inproj(C, K, l, groups, setup)


def host_consts():
    c = {}
    c["c_ident"] = np.eye(128, dtype=np.float32)
    perm = np.zeros((128, 128), np.float32)
    for m in range(128):
        p = m + 32 if (m % 64) < 32 else m - 32
        perm[p, m] = 1.0
    c["c_perm"] = perm
    inv = (np.float32(10000.0) ** (-np.arange(32, dtype=np.float32) / np.float32(32))).astype(np.float32)
    t = np.arange(NLAT)
    rowp = (t // 64).astype(np.float32)
    colp = (t % 64).astype(np.float32)
    cos = np.zeros((128, NLAT), np.float32)
    sin = np.zeros((128, NLAT), np.float32)
    for dd in range(128):
        pos = rowp if dd < 64 else colp
        ang = (pos * inv[dd % 32]).astype(np.float32)
        cos[dd] = np.cos(ang)
        sin[dd] = np.sin(ang) * (-1.0 if (dd % 64) < 32 else 1.0)
    c["c_cos"], c["c_sin"] = cos, sin
    return c


def na_key_tiles(j):
    if j <= 1:
        kps = [0, 1, 2, 3]
        base = 5 + 4 * j
        idx = [base + i for i in range(4)]
    elif j >= 30:
        kps = [28, 29, 30, 31]
        base = 13 + 4 * (j - 30)
        idx = [base + i for i in range(4)]
    else:
        kps = [j - 2, j - 1, j, j + 1, j + 2]
        idx = [0, 1, 2, 3, 4]
    return kps, idx


def phase_na(C, K, l, need_ctx):
    nc, d, cm = C.nc, C.d, C.cm
    e = l // 2
    K.begin_phase()
    QT = [K.sbuf(f"QT{i}", [128, NTOK], BF16) for i in range(2)]
    KT = [K.sbuf(f"KT{i}", [128, NTOK], BF16) for i in range(2)]
    V = [K.sbuf(f"V{i}", [128, 34, 128], BF16) for i in range(2)]
    BM = [K.sbuf(f"BM{i}", [128, 21, 128], F32) for i in range(2)]
    OS = [K.sbuf(f"OS{i}", [128, NTOK], BF16, partial=True) for i in range(2)]
    sA = [K.psum(f"sA{i}", [128, 512], F32) for i in range(2)]
    sB = [K.psum(f"sB{i}", [128, 512], F32) for i in range(2)]
    po = [K.psum(f"po{i}", [128, 256], F32) for i in range(2)]
    pT = [K.sbuf(f"pT{i}", [128, 896], BF16) for i in range(2)]
    rc = [K.sbuf(f"rc{i}", [128, 128], F32) for i in range(2)]
    blocks = [("lat", j) for j in range(32)] + ([("ctx", 0), ("ctx", 1)] if need_ctx else [])
    it = 0
    for h in range(16):
        qt, kt, v, bm, osb = QT[h % 2], KT[h % 2], V[h % 2], BM[h % 2], OS[h % 2]
        K.dma(K.sp, qt[:], d["QaT"][h], W=[qt])
        K.dma(K.sp, kt[:], d["KaT"][h], W=[kt])
        K.dma(K.sp, v[:], d["Va"].rearrange("(t p) c -> p t c", p=128)[:, :, h * 128:(h + 1) * 128], W=[v])
        K.dma(K.sp, bm[:], d["c_bm"][e, h], W=[bm])

        def qk(blk, i):
            kind, j = blk
            if kind == "lat":
                kps, idx = na_key_tiles(j)
                tiles = [(2 + kp, ix) for kp, ix in zip(kps, idx)] + [(0, None), (1, None)]
                q0 = NCTX + 128 * j
            else:
                tiles = [(0, None), (1, None)]
                q0 = 128 * j
            for s, (t, ix) in enumerate(tiles):
                bank = sA[i % 2] if s < 4 else sB[i % 2]
                o = bank[:, (s % 4) * 128:(s % 4 + 1) * 128]
                mm(K, bank, o, kt[:, t * 128:(t + 1) * 128], qt[:, q0:q0 + 128], True, ix is None, R=[kt, qt])
                if ix is not None:
                    mm(K, bank, o, bm[:, ix, :], cm.ident_f[:], False, True, R=[bm, cm.ident_f])
            return tiles, q0

        def pv(blk, i, tiles, q0):
            n = len(tiles)
            p = pT[i % 2]
            na = min(n, 4)
            K.op(K.act, lambda e: e.activation(p[:, 0:na * 128], sA[i % 2][:, 0:na * 128], AF.Exp), R=[sA[i % 2]], W=[p])
            if n > 4:
                K.op(K.act, lambda e: e.activation(p[:, 512:n * 128], sB[i % 2][:, 0:(n - 4) * 128], AF.Exp), R=[sB[i % 2]], W=[p])
            pq = po[i % 2]
            for s, (t, ix) in enumerate(tiles):
                mm(K, pq, pq[:, 0:128], v[:, t, :], p[:, s * 128:(s + 1) * 128], s == 0, s == n - 1, R=[v, p])
            for s, (t, ix) in enumerate(tiles):
                mm(K, pq, pq[:, 128:256], cm.ones_b[:], p[:, s * 128:(s + 1) * 128], s == 0, s == n - 1, R=[cm.ones_b, p])
            r = rc[i % 2]
            K.op(K.dve, lambda e: e.reciprocal(r[:], pq[:, 128:256]), R=[pq], W=[r])
            K.op(K.dve, lambda e: e.tensor_tensor(osb[:, q0:q0 + 128], pq[:, 0:128], r[:], ALU.mult), R=[pq, r], W=[osb])

        prev = None
        for blk in blocks:
            cur = (blk, it) + qk(blk, it)
            it += 1
            if prev is not None:
                pv(*prev)
            prev = cur
        pv(*prev)
        K.dma(K.sp, d["OT"][h], osb[:], R=[osb])
    K.end_phase()


def phase_win(C, K, l, need_ctx):
    nc, d, cm = C.nc, C.d, C.cm
    e = l // 2
    K.begin_phase()
    QT = [K.sbuf(f"QT{i}", [128, 4, NTOK], BF16) for i in range(2)]
    KT = [K.sbuf(f"KT{i}", [128, NTOK], BF16) for i in range(2)]
    V = [K.sbuf(f"V{i}", [128, 34, 128], BF16) for i in range(2)]
    OS = K.sbuf("OS", [128, 4, NTOK], BF16, partial=True)
    wm = K.sbuf("wm", [128, 2, 128], F32)
    id4 = K.sbuf("id4", [128, 4, 128], F32)
    K.dma(K.sp, wm[:], d["c_wmask"][:, :, :], W=[wm])
    for i in range(4):
        K.dma(K.sp, id4[:, i, :], d["c_ident"][:, :], W=[id4])
    sk = K.sbuf("sk", [1, 16], F32)
    esr = K.sbuf("esr", [1, 16, 128], F32)
    K.dma(K.sp, sk[:], d["win_sink"][e:e + 1, :], W=[sk])
    K.op(K.act, lambda e_: e_.activation(sk[:], sk[:], AF.Exp), R=[sk], W=[sk])
    for h in range(16):
        K.op(K.dve, lambda e_, h=h: e_.tensor_scalar(esr[0:1, h, :], cm.ones_f[0:1, 0:128], sk[0:1, h:h + 1], None, ALU.mult), R=[sk, cm.ones_f], W=[esr])
    sb = [K.psum(f"sb{i}", [128, 512], F32) for i in range(5)]
    po = K.psum("po", [128, 512], F32)
    pm = K.psum("pm", [128, 512], F32)
    pT = [K.sbuf(f"pT{i}", [128, 5, 512], BF16) for i in range(2)]
    rc = K.sbuf("rc", [128, 512], F32)
    blocks = [("lat", j) for j in range(32)] + ([("ctx", 0), ("ctx", 1)] if need_ctx else [])
    it = 0
    for g in range(4):
        qt, kt, v = QT[g % 2], KT[g % 2], V[g % 2]
        K.dma(K.sp, qt[:], d["QbT"][4 * g:4 * g + 4].rearrange("h p t -> p h t"), W=[qt])
        K.dma(K.sp, kt[:], d["KbT"][g], W=[kt])
        K.dma(K.sp, v[:], d["Vb"].rearrange("(t p) c -> p t c", p=128)[:, :, g * 128:(g + 1) * 128], W=[v])
        for blk in blocks:
            kind, j = blk
            if kind == "lat":
                tiles = []
                if j > 0:
                    tiles.append((2 + j - 1, 0))
                tiles.append((2 + j, None))
                if j < 31:
                    tiles.append((2 + j + 1, 1))
                tiles += [(0, None), (1, None)]
                q0 = NCTX + 128 * j
            else:
                tiles = [(0, None), (1, None)]
                q0 = 128 * j
            n = len(tiles)
            p = pT[it % 2]
            it += 1
            for s, (t, mi) in enumerate(tiles):
                bank = sb[s]
                mm(K, bank, bank[:], kt[:, t * 128:(t + 1) * 128], qt[:, :, q0:q0 + 128], True, mi is None, R=[kt, qt])
                if mi is not None:
                    mm(K, bank, bank[:], wm[:, mi, :], id4[:], False, True, R=[wm, id4])
                K.op(K.act, lambda e_, s=s, bank=bank: e_.activation(p[:, s, :], bank[:], AF.Exp), R=[bank], W=[p])
            for s, (t, mi) in enumerate(tiles):
                mm(K, po, po[:], v[:, t, :], p[:, s, :], s == 0, s == n - 1, R=[v, p])
            for s, (t, mi) in enumerate(tiles):
                mm(K, pm, pm[:], cm.ones_b[:], p[:, s, :], s == 0, False, R=[cm.ones_b, p])
            mm(K, pm, pm[:], cm.ones_f[0:1, 0:128], esr[0:1, 4 * g:4 * g + 4, :], False, True, R=[cm.ones_f, esr])
            K.op(K.dve, lambda e_: e_.reciprocal(rc[:], pm[:]), R=[pm], W=[rc])
            K.op(K.dve, lambda e_: e_.tensor_tensor(OS[:, :, q0:q0 + 128], po[:].rearrange("p (h q) -> p h q", h=4), rc[:].rearrange("p (h q) -> p h q", h=4), ALU.mult),
                 R=[po, rc], W=[OS])
        for hh in range(4):
            K.dma(K.sp, d["OT"][16 + 4 * g + hh], OS[:, hh, :], R=[OS])
    K.end_phase()


def phase_outproj(C, K, l, w_out, need_ctx):
    nc, d, cm = C.nc, C.d, C.cm
    K.begin_phase()
    wbs = [K.sbuf(f"wb{i}", [128, KC, 512], BF16) for i in range(2)]
    ots = [K.sbuf(f"ot{i}", [128, KC, 512], BF16) for i in range(2)]
    gb = [K.sbuf(f"gb{i}", [128, 2, 512], F32) for i in range(2)]
    xin = [K.sbuf(f"xin{i}", [128, 512], F32) for i in range(3)]
    tt_ = [K.sbuf(f"tt{i}", [128, 512], F32) for i in range(3)]
    acc = [K.psum(f"acc{i}", [128, 512], F32) for i in range(4)]
    tiles = TOK_TILES if need_ctx else TOK_TILES[1:]
    oi = 0
    xi = 0
    for g in range(8):
        cs = slice(g * 512, (g + 1) * 512)
        wb, gbt = wbs[g % 2], gb[g % 2]
        K.dma(K.pool, wb[:], w_out[:, cs].rearrange("(c p) n -> p c n", p=128), W=[wb])
        for r in range(2):
            K.dma(K.sp, gbt[:, r, :], d["mod"][l][r:r + 1, 2 * D + g * 512:2 * D + (g + 1) * 512].broadcast_to([128, 512]), W=[gbt])
        for (tok0, ntok) in tiles:
            ot = ots[oi % 2]
            oi += 1
            row = 1 if tok0 == 0 else 0
            K.dma(K.sp, ot[:, :, 0:ntok], d["OT"][:, :, tok0:tok0 + ntok].rearrange("k p t -> p k t"), W=[ot])
            for sb in range(ntok // 128):
                ps = acc[xi % 4]
                xt, t = xin[xi % 3], tt_[xi % 3]
                xi += 1
                r0 = tok0 + sb * 128
                K.dma(K.sp, xt[:], d["xl"][r0:r0 + 128, cs], W=[xt])
                for kc in range(KC):
                    mm(K, ps, ps[:], ot[:, kc, sb * 128:(sb + 1) * 128], wb[:, kc, :], kc == 0, kc == KC - 1, R=[ot, wb])
                K.op(K.dve, lambda e_, t=t, ps=ps: e_.tensor_tensor(t[:], ps[:], gbt[:, row, :], ALU.mult), R=[ps, gbt], W=[t])
                K.op(K.dve, lambda e_, t=t, xt=xt: e_.tensor_tensor(t[:], t[:], xt[:], ALU.add), R=[t, xt], W=[t])
                K.dma(K.sp, d["xm"][r0:r0 + 128, cs], t[:], R=[t])
    K.end_phase()


def host_bm(rpb):
    q = np.arange(128)
    key = np.arange(128)
    mats = [(10, 10 + off) for off in (-2, -1, 0, 1, 2)]
    for j in (0, 1):
        mats += [(j, kp) for kp in (0, 1, 2, 3)]
    for j in (30, 31):
        mats += [(j, kp) for kp in (28, 29, 30, 31)]
    out = np.full((2, 16, 128, 21, 128), NEG, np.float32)
    for m, (j, kp) in enumerate(mats):
        rq = (2 * j + q // 64)[:, None]
        cq = (q % 64)[:, None]
        rk = (2 * kp + key // 64)[None, :]
        ck = (key % 64)[None, :]
        rs = np.clip(rq - 4, 0, 56)
        cs = np.clip(cq - 8, 0, 48)
        valid = (rk >= rs) & (rk <= rs + 7) & (ck >= cs) & (ck <= cs + 15)
        dr = np.clip(rk - rq + 7, 0, 14)
        dc = np.clip(ck - cq + 15, 0, 30)
        g = rpb[:, :, dr, dc]
        out[:, :, :, m, :] = np.where(valid[None, None], g, np.float32(NEG))
    return out


def host_wmask():
    q = np.arange(128)[:, None]
    k = np.arange(128)[None, :]
    wm = np.zeros((128, 2, 128), np.float32)
    wm[:, 0, :] = np.where(q <= k, 0.0, NEG)
    wm[:, 1, :] = np.where(k <= q, 0.0, NEG)
    return wm


def phase_router(C, K, l, need_ctx, n_iter=34):
    nc, d, cm = C.nc, C.d, C.cm
    K.begin_phase()
    nb = NormBufs(K)
    vs = K.sbuf("vs", [32, 5, 128], F32)
    tmpv = K.sbuf("tmpv", [128, 5, 32], F32)
    gsh = K.sbuf("gsh", [128, 4, 32], F32)
    acc = [K.psum(f"acc{i}", [128, 512], F32) for i in range(2)]
    build_gsh(C, K, l, 2, gsh, vs, acc[0], tmpv)
    hTs = [K.sbuf(f"hT{i}", [128, KC, 512], BF16, partial=True) for i in range(2)]
    wr = K.sbuf("wr", [128, KC, 16], BF16)
    K.dma(K.pool, wr[:], d["moe_router"][l].rearrange("(p c) e -> p c e", c=KC), W=[wr])
    E = K.sbuf("E", [16, NTOK], F32, partial=True)
    A = K.sbuf("A", [16, NTOK], F32, partial=True)
    junk = K.sbuf("junk", [16, NLAT], F32)
    tiles = TOK_TILES if need_ctx else TOK_TILES[1:]
    for ti, (tok0, ntok) in enumerate(tiles):
        hT = hTs[ti % 2]
        row = 1 if tok0 == 0 else 0
        for sb in range(ntok // 128):
            norm_block(C, K, nb, d["xm"][tok0 + sb * 128: tok0 + (sb + 1) * 128, :], gsh, row, hT, sb * 128)
        ps = acc[ti % 2]
        for kc in range(KC):
            mm(K, ps, ps[0:16, 0:ntok], wr[:, kc, :], hT[:, kc, 0:ntok], kc == 0, kc == KC - 1, R=[wr, hT])
        K.op(K.act, lambda e: e.activation(E[:, tok0:tok0 + ntok], ps[0:16, 0:ntok], AF.Exp), R=[ps], W=[E])
    for ti, (tok0, ntok) in enumerate(tiles):
        ps = acc[ti % 2]
        mm(K, ps, ps[0:16, 0:ntok], cm.ones_f[0:16, 0:16], E[:, tok0:tok0 + ntok], True, True, R=[cm.ones_f, E])
        K.op(K.dve, lambda e: e.reciprocal(A[:, tok0:tok0 + ntok], ps[0:16, 0:ntok]), R=[ps], W=[A])
        K.op(K.dve, lambda e: e.tensor_tensor(A[:, tok0:tok0 + ntok], A[:, tok0:tok0 + ntok], E[:, tok0:tok0 + ntok], ALU.mult), R=[A, E], W=[A])
    sets = ([(0, NCTX, 32)] if need_ctx else []) + [(NCTX, NLAT, 512)]
    G = K.sbuf("G", [16, NTOK], F32, partial=True)
    for si, (c0, n, cap) in enumerate(sets):
        st = K.sbuf(f"bis{si}", [16, 8], F32)
        lo, hi, mid, cnt, ge, nge, t1, t2 = [st[:, i:i + 1] for i in range(8)]
        K.op(K.dve, lambda e: e.memset(st[:], 0.0), W=[st])
        K.op(K.dve, lambda e: e.memset(hi, 2.0), R=[st], W=[st])
        Av = A[:, c0:c0 + n]
        for it in range(n_iter):
            K.op(K.dve, lambda e: e.tensor_scalar(mid, lo, hi, 0.5, ALU.add, ALU.mult), R=[st], W=[st])
            K.op(K.dve, lambda e: e.tensor_scalar(junk[:, 0:n], Av, mid, 0.0, ALU.is_ge, ALU.add, accum_out=cnt), R=[A, st], W=[junk, st])
            K.op(K.dve, lambda e: e.tensor_scalar(ge, cnt, float(cap) - 0.5, None, ALU.is_ge), R=[st], W=[st])
            K.op(K.dve, lambda e: e.tensor_scalar(nge, ge, -1.0, 1.0, ALU.mult, ALU.add), R=[st], W=[st])
            K.op(K.dve, lambda e: e.tensor_tensor(t1, mid, ge, ALU.mult), R=[st], W=[st])
            K.op(K.dve, lambda e: e.tensor_tensor(t2, mid, nge, ALU.mult), R=[st], W=[st])
            K.op(K.dve, lambda e: e.scalar_tensor_tensor(lo, lo, nge, t1, ALU.mult, ALU.add), R=[st], W=[st])
            K.op(K.dve, lambda e: e.scalar_tensor_tensor(hi, hi, ge, t2, ALU.mult, ALU.add), R=[st], W=[st])
        K.op(K.dve, lambda e: e.scalar_tensor_tensor(G[:, c0:c0 + n], Av, lo, Av, ALU.is_ge, ALU.mult), R=[A, st], W=[G])
    t0 = 0 if need_ctx else NCTX
    K.dma(K.sp, d["gm"][:, t0:NTOK], G[:, t0:NTOK], R=[G])
    K.end_phase()


def phase_moe(C, K, l, need_ctx):
    nc, d, cm = C.nc, C.d, C.cm
    K.begin_phase()
    nb = NormBufs(K)
    vs = K.sbuf("vs", [32, 5, 128], F32)
    tmpv = K.sbuf("tmpv", [128, 5, 32], F32)
    gsh = K.sbuf("gsh", [128, 4, 32], F32)
    pa = [K.psum(f"pa{i}", [128, 512], F32) for i in range(2)]
    pu = [K.psum(f"pu{i}", [128, 512], F32) for i in range(2)]
    py = [K.psum(f"py{i}", [128, 512], F32) for i in range(2)]
    build_gsh(C, K, l, 2, gsh, vs, pa[0], tmpv)
    hT = K.sbuf("hT", [128, KC, 512], BF16, partial=True)
    aT = K.sbuf("aT", [128, 32, 512], BF16, partial=True)
    wp = [K.sbuf(f"wp{i}", [128, 2, KC, 256], BF16, partial=True) for i in range(2)]
    g2 = K.sbuf("g2", [128, D], F32)
    gmb = [K.sbuf(f"gmb{i}", [128, 512], F32) for i in range(2)]
    sl = [K.sbuf(f"sl{i}", [128, 512], F32) for i in range(2)]
    tl = [K.sbuf(f"tl{i}", [128, 512], F32) for i in range(2)]
    xin = [K.sbuf(f"xin{i}", [128, 512], F32) for i in range(3)]
    yo = [K.sbuf(f"yo{i}", [128, 512], F32) for i in range(3)]
    w1, w3, w2 = d["moe_w1"][l], d["moe_w3"][l], d["moe_w2"][l]
    w2v = w2.rearrange("e (fb p) n -> p (e fb) n", p=128)
    tiles = TOK_TILES if need_ctx else TOK_TILES[1:]
    wi = 0
    ai = 0
    yi = 0
    for ti, (tok0, ntok) in enumerate(tiles):
        row = 1 if tok0 == 0 else 0
        if ti == 0 or (ti == 1 and need_ctx):
            K.dma(K.sp, g2[:], d["mod"][l][row:row + 1, 5 * D:6 * D].broadcast_to([128, D]), W=[g2])
        for sb in range(ntok // 128):
            norm_block(C, K, nb, d["xm"][tok0 + sb * 128: tok0 + (sb + 1) * 128, :], gsh, row, hT, sb * 128)
        for e in range(16):
            w = wp[wi % 2]
            wi += 1
            K.dma(K.pool, w[:, 0], w1[e].rearrange("(p c) f -> p c f", c=KC), W=[w])
            K.dma(K.pool, w[:, 1], w3[e].rearrange("(p c) f -> p c f", c=KC), W=[w])
            gb = gmb[e % 2]
            K.dma(K.sp, gb[:, 0:ntok], d["gm"][e:e + 1, tok0:tok0 + ntok].broadcast_to([128, ntok]), W=[gb])
            for fb in range(2):
                a, u = pa[ai % 2], pu[ai % 2]
                s_, t = sl[ai % 2], tl[ai % 2]
                ai += 1
                for kc in range(KC):
                    mm(K, a, a[:, 0:ntok], w[:, 0, kc, fb * 128:(fb + 1) * 128], hT[:, kc, 0:ntok], kc == 0, kc == KC - 1, R=[w, hT])
                for kc in range(KC):
                    mm(K, u, u[:, 0:ntok], w[:, 1, kc, fb * 128:(fb + 1) * 128], hT[:, kc, 0:ntok], kc == 0, kc == KC - 1, R=[w, hT])
                K.op(K.act, lambda e_: e_.activation(s_[:, 0:ntok], a[:, 0:ntok], AF.Silu), R=[a], W=[s_])
                K.op(K.dve, lambda e_: e_.tensor_tensor(t[:, 0:ntok], s_[:, 0:ntok], u[:, 0:ntok], ALU.mult), R=[s_, u], W=[t])
                K.op(K.dve, lambda e_: e_.tensor_tensor(aT[:, e * 2 + fb, 0:ntok], t[:, 0:ntok], gb[:, 0:ntok], ALU.mult), R=[t, gb], W=[aT])
        for g in range(8):
            cs = slice(g * 512, (g + 1) * 512)
            w = wp[wi % 2]
            wi += 1
            wv = w[:].rearrange("p a c f -> p (a c f)").rearrange("p (c n) -> p c n", n=512)
            K.dma(K.pool, wv, w2v[:, :, cs], W=[w])
            for sb in range(ntok // 128):
                ps = py[yi % 2]
                xt, y = xin[yi % 3], yo[yi % 3]
                yi += 1
                r0 = tok0 + sb * 128
                K.dma(K.sp, xt[:], d["xm"][r0:r0 + 128, cs], W=[xt])
                for c in range(32):
                    mm(K, ps, ps[:], aT[:, c, sb * 128:(sb + 1) * 128], wv[:, c, :], c == 0, c == 31, R=[aT, w])
                K.op(K.dve, lambda e_: e_.tensor_tensor(y[:], ps[:], g2[:, cs], ALU.mult), R=[ps, g2], W=[y])
                K.op(K.dve, lambda e_: e_.tensor_tensor(y[:], y[:], xt[:], ALU.add), R=[y, xt], W=[y])
                K.dma(K.sp, d["xl"][r0:r0 + 128, cs], y[:], R=[y])
    K.end_phase()


def phase_final(C, K):
    nc, d, cm = C.nc, C.d, C.cm
    K.begin_phase()
    fw = K.sbuf("fw", [128, D], F32)
    K.dma(K.sp, fw[:], d["final_norm_w"][0:1, :].broadcast_to([128, D]), W=[fw])
    xts = [K.sbuf(f"fx{i}", [128, D], F32) for i in range(2)]
    jk = K.sbuf("fj", [128, D], BF16)
    yts = [K.sbuf(f"fy{i}", [128, D], F32) for i in range(2)]
    sss = [K.sbuf(f"fs{i}", [128, 2], F32) for i in range(2)]
    for i in range(NLAT // 128):
        xt, yt, ss = xts[i % 2], yts[i % 2], sss[i % 2]
        K.dma(K.sp, xt[:], d["xl"][NCTX + i * 128:NCTX + (i + 1) * 128, :], W=[xt])
        K.op(K.act, lambda e: e.activation(jk[:], xt[:], AF.Square, accum_out=ss[:, 0:1]), R=[xt], W=[jk, ss])
        K.op(K.act, lambda e: e.activation(ss[:, 1:2], ss[:, 0:1], AF.Sqrt, scale=1.0 / D, bias=cm.eps[:, 0:1]), R=[ss, cm.eps], W=[ss])
        K.op(K.dve, lambda e: e.reciprocal(ss[:, 1:2], ss[:, 1:2]), R=[ss], W=[ss])
        K.op(K.dve, lambda e: e.scalar_tensor_tensor(yt[:], xt[:], ss[:, 1:2], fw[:], ALU.mult, ALU.mult), R=[xt, ss, fw], W=[yt])
        K.dma(K.sp, d["y"][i * 128:(i + 1) * 128, :], yt[:], R=[yt])
    K.end_phase()


def ev_tm_gates(dst):
    def f(S, g, sb, ps):
        K = S.K
        st = S.stgf[S.n % 3]
        S.n += 1
        K.op(K.dve, lambda e: e.tensor_tensor(st[:, 0:32], ps[:, 0:32], S.bg[:], ALU.add), R=[ps, S.bg], W=[st])
        r0 = S.tok0 + sb * 128
        K.dma(K.sp, dst[r0:r0 + 128, :], st[:, 0:32], R=[st])
    return f


def phase_inproj_mlstm(C, K, l):
    d = C.d
    o = l // 2
    w = d["ml_w_in"][o]
    groups = []
    for i in range(4):
        groups.append(dict(kind="FM", w=w[:, i * 512:(i + 1) * 512], ncols=512, b0=4 * i, evac=ev_fm_plain(d["mqT"], 256 ** -0.5)))
    for i in range(4):
        groups.append(dict(kind="FM", w=w[:, 2048 + i * 512:2048 + (i + 1) * 512], ncols=512, b0=4 * i, evac=ev_fm_plain(d["mkT"])))
    for i in range(4):
        groups.append(dict(kind="TM", w=w[:, 2048 + i * 512:2048 + (i + 1) * 512], ncols=512, c0=512 * i, evac=ev_tm_plain(d["mK"])))
    for i in range(8):
        groups.append(dict(kind="TM", w=w[:, 4096 + i * 512:4096 + (i + 1) * 512], ncols=512, c0=512 * i, evac=ev_tm_plain(d["mV"])))
    for i in range(8):
        groups.append(dict(kind="TM", w=w[:, 8192 + i * 512:8192 + (i + 1) * 512], ncols=512, c0=512 * i, evac=ev_tm_plain(d["mO"])))
    groups.append(dict(kind="TM", w=w[:, 12288:12320], ncols=32, c0=0, evac=ev_tm_gates(d["mG"])))

    def setup(S):
        S.bg = K.sbuf("bg", [128, 32], F32)
        K.dma(K.sp, S.bg[:], d["ml_b_gates"][o:o + 1, :].broadcast_to([128, 32]), W=[S.bg])
    phase_inproj(C, K, l, groups, setup)


def phase_mlstm_scan(C, K, l, dr):
    nc, d, cm = C.nc, C.d, C.cm
    K.begin_phase()
    tri = K.sbuf("tri", [64, 4, 64], F32)
    K.dma(K.sp, tri[:], d["c_tri"][:, :, :], W=[tri])
    Cst = [K.sbuf(f"Cst{h}", [128, 2, 512], F32) for h in range(8)]
    Cb = [K.sbuf(f"Cb{h}", [128, 2, 512], BF16) for h in range(8)]
    nst = [K.sbuf(f"nst{h}", [128, 2], F32) for h in range(8)]
    nbf = [K.sbuf(f"nbf{h}", [128, 2], BF16) for h in range(8)]
    for h in range(8):
        K.op(K.dve, lambda e, h=h: e.memset(Cst[h][:], 0.0), W=[Cst[h]])
        K.op(K.pool, lambda e, h=h: e.memset(Cb[h][:], 0.0), W=[Cb[h]])
        K.op(K.dve, lambda e, h=h: e.memset(nst[h][:], 0.0), W=[nst[h]])
        K.op(K.pool, lambda e, h=h: e.memset(nbf[h][:], 0.0), W=[nbf[h]])
    qT4 = [K.sbuf(f"qT4{i}", [128, 16, 256], BF16) for i in range(2)]
    kT4 = [K.sbuf(f"kT4{i}", [128, 16, 256], BF16) for i in range(2)]
    Kt = [K.sbuf(f"Kt{i}", [64, 2048], BF16) for i in range(2)]
    Vt = [K.sbuf(f"Vt{i}", [64, D], BF16) for i in range(2)]
    Gt = [K.sbuf(f"Gt{i}", [64, 32], F32) for i in range(2)]
    hch = [K.sbuf(f"hch{i}", [64, D], F32, partial=True) for i in range(2)]
    gcp = [K.sbuf(f"gcp{i}", [128, 16], F32) for i in range(2)]
    gs = [K.sbuf(f"gs{i}", [128, 6, 8], F32, partial=True) for i in range(2)]
    smalls = [K.psum(f"psm{i}", [128, 512], F32) for i in range(2)]
    pgt = [Buf(K, f"pgt{i}", smalls[i].t[:, 0:16]) for i in range(2)]
    pst = [Buf(K, f"pst{i}", smalls[i].t[0:64, 64:128]) for i in range(2)]
    pd = [Buf(K, f"pd{i}", smalls[i].t[0:64, 128:130]) for i in range(2)]
    pdn = [Buf(K, f"pdn{i}", smalls[i].t[:, 136:138]) for i in range(2)]
    pn = [K.psum(f"pn{i}", [64, 512], F32) for i in range(2)]
    pdc = [K.psum(f"pdc{i}", [128, 512], F32) for i in range(4)]
    Pt = [K.sbuf(f"Pt{i}", [64, 64], BF16) for i in range(2)]
    Kw = [K.sbuf(f"Kw{i}", [64, 256], BF16) for i in range(2)]
    dd = [K.sbuf(f"dd{i}", [64, 2], F32) for i in range(2)]
    ic, fc = (0, 8) if dr == 0 else (16, 24)
    order = list(range(68)) if dr == 0 else [3, 2, 1, 0] + list(range(67, 3, -1))
    dst = d["hf"] if dr == 0 else d["hb"]
    cur_grp = None
    gi = 0
    si = 0
    for ci, ck in enumerate(order):
        grp = ck // 4
        if grp != cur_grp:
            cur_grp = grp
            q4, k4 = qT4[gi % 2], kT4[gi % 2]
            gi += 1
            K.dma(K.sp, q4[:], d["mqT"][:, :, grp * 256:(grp + 1) * 256].rearrange("b p t -> p b t"), W=[q4])
            K.dma(K.sp, k4[:], d["mkT"][:, :, grp * 256:(grp + 1) * 256].rearrange("b p t -> p b t"), W=[k4])
        t0 = (ck % 4) * 64
        r0 = ck * 64
        kt, vt, gt, hc, g_ = Kt[ci % 2], Vt[ci % 2], Gt[ci % 2], hch[ci % 2], gs[ci % 2]
        pg = pgt[ci % 2]
        K.dma(K.sp, kt[:], d["mK"][r0:r0 + 64, :], W=[kt])
        K.dma(K.sp, vt[:], d["mV"][r0:r0 + 64, :], W=[vt])
        K.dma(K.sp, gt[:], d["mG"][r0:r0 + 64, :], W=[gt])
        lq, vq, ev, ea, eb, eg = [g_[:, i, :] for i in range(6)]
        K.op(K.act, lambda e: e.activation(lq[0:64], gt[:, fc:fc + 8], AF.Exp, scale=-1.0), R=[gt], W=[g_])
        K.op(K.act, lambda e: e.activation(lq[0:64], lq[0:64], AF.Ln, bias=cm.ones_f[0:64, 0:1]), R=[g_, cm.ones_f], W=[g_])
        mm(K, pg, pg[0:64, 0:8], tri[:, dr, :], lq[0:64], True, True, R=[tri, g_])
        mm(K, pg, pg[:, 8:16], cm.ones_f[0:64, 0:128], lq[0:64], True, True, R=[cm.ones_f, g_])
        K.op(K.dve, lambda e: e.tensor_tensor(vq[0:64], gt[:, ic:ic + 8], pg[0:64, 0:8], ALU.add), R=[gt, pg], W=[g_])
        K.op(K.act, lambda e: e.activation(ev[0:64], vq[0:64], AF.Exp), R=[g_], W=[g_])
        K.op(K.dve, lambda e: e.tensor_tensor(ea[0:64], vq[0:64], pg[0:64, 8:16], ALU.subtract), R=[g_, pg], W=[g_])
        K.op(K.act, lambda e: e.activation(ea[0:64], ea[0:64], AF.Exp), R=[g_], W=[g_])
        K.op(K.act, lambda e: e.activation(eb[0:64], pg[0:64, 0:8], AF.Exp, scale=-1.0), R=[pg], W=[g_])
        K.op(K.act, lambda e: e.activation(eg, pg[:, 8:16], AF.Exp, scale=-1.0), R=[pg], W=[g_])
        for h in range(8):
            ps_s, ps_n, ps_d, ps_dn = pst[si % 2], pn[si % 2], pd[si % 2], pdn[si % 2]
            pc0, pc1 = pdc[(2 * si) % 4], pdc[(2 * si + 1) % 4]
            pt, kw, dq = Pt[si % 2], Kw[si % 2], dd[si % 2]
            si += 1
            qa = [q4[:, 2 * h + c, t0:t0 + 64] for c in range(2)]
            ka = [k4[:, 2 * h + c, t0:t0 + 64] for c in range(2)]
            vh = vt[:, h * 512:(h + 1) * 512]
            for c in range(2):
                mm(K, ps_s, ps_s[:, :], ka[c], qa[c], c == 0, c == 1, R=[k4, q4])
            K.op(K.dve, lambda e: e.scalar_tensor_tensor(pt[:], ps_s[:, :], ev[0:64, h:h + 1], tri[:, dr, :], ALU.mult, ALU.mult), R=[ps_s, g_, tri], W=[pt])
            for c in range(2):
                mm(K, ps_n, ps_n[:, :], qa[c], Cb[h][:, c, :], c == 0, False, R=[q4, Cb[h]])
            mm(K, ps_n, ps_n[:, :], pt[:], vh, False, True, R=[pt, vt])
            for c in range(2):
                mm(K, ps_d, ps_d[:, 0:1], qa[c], nbf[h][:, c:c + 1], c == 0, False, R=[q4, nbf[h]])
            mm(K, ps_d, ps_d[:, 0:1], pt[:], cm.ones_b[0:64, 0:1], False, True, R=[pt, cm.ones_b])
            K.op(K.dve, lambda e: e.tensor_scalar(dq[:, 0:1], ps_d[:, 0:1], eb[0:64, h:h + 1], None, ALU.mult), R=[ps_d, g_], W=[dq])
            K.op(K.dve, lambda e: e.tensor_scalar(dq[:, 1:2], ps_d[:, 0:1], eb[0:64, h:h + 1], -1.0, ALU.mult, ALU.mult), R=[ps_d, g_], W=[dq])
            K.op(K.dve, lambda e: e.scalar_tensor_tensor(dq[:, 0:1], dq[:, 0:1], 1.0, dq[:, 1:2], ALU.max, ALU.max), R=[dq], W=[dq])
            K.op(K.dve, lambda e: e.reciprocal(dq[:, 0:1], dq[:, 0:1]), R=[dq], W=[dq])
            K.op(K.dve, lambda e: e.tensor_tensor(dq[:, 1:2], dq[:, 0:1], eb[0:64, h:h + 1], ALU.mult), R=[dq, g_], W=[dq])
            K.op(K.act, lambda e: e.activation(hc[:, h * 512:(h + 1) * 512], ps_n[:, :], AF.Copy, scale=dq[:, 1:2]), R=[ps_n, dq], W=[hc])
            K.op(K.pool, lambda e: e.tensor_scalar(kw[:], kt[:, h * 256:(h + 1) * 256], ea[0:64, h:h + 1], None, ALU.mult), R=[kt, g_], W=[kw])
            mm(K, pc0, pc0[:, :], kw[:, 0:128], vh, True, True, R=[kw, vt])
            mm(K, pc1, pc1[:, :], kw[:, 128:256], vh, True, True, R=[kw, vt])
            for c in range(2):
                mm(K, ps_dn, ps_dn[:, c:c + 1], kw[:, c * 128:(c + 1) * 128], cm.ones_b[0:64, 0:1], True, True, R=[kw, cm.ones_b])
            K.op(K.dve, lambda e: e.scalar_tensor_tensor(Cst[h][:, 0, :], Cst[h][:, 0, :], eg[:, h:h + 1], pc0[:, :], ALU.mult, ALU.add), R=[Cst[h], g_, pc0], W=[Cst[h]])
            K.op(K.dve, lambda e: e.scalar_tensor_tensor(Cst[h][:, 1, :], Cst[h][:, 1, :], eg[:, h:h + 1], pc1[:, :], ALU.mult, ALU.add), R=[Cst[h], g_, pc1], W=[Cst[h]])
            K.op(K.act, lambda e: e.copy(Cb[h][:], Cst[h][:]), R=[Cst[h]], W=[Cb[h]])
            K.op(K.dve, lambda e: e.scalar_tensor_tensor(nst[h][:], nst[h][:], eg[:, h:h + 1], ps_dn[:, :], ALU.mult, ALU.add), R=[nst[h], g_, ps_dn], W=[nst[h]])
            K.op(K.pool, lambda e: e.tensor_copy(nbf[h][:], nst[h][:]), R=[nst[h]], W=[nbf[h]])
        K.dma(K.sp, dst[r0:r0 + 64, :], hc[:], R=[hc])
    K.end_phase()


def phase_mlstm_post(C, K, l, need_ctx):
    nc, d, cm = C.nc, C.d, C.cm
    o = l // 2
    K.begin_phase()
    nwb = K.sbuf("nwb", [128, D], F32)
    K.dma(K.sp, nwb[:], d["ml_norm_w"][o:o + 1, :].broadcast_to([128, D]), W=[nwb])
    hf = [K.sbuf(f"hf{i}", [128, D], F32) for i in range(2)]
    hb = [K.sbuf(f"hb{i}", [128, D], F32) for i in range(2)]
    ot = [K.sbuf(f"ot{i}", [128, D], BF16) for i in range(2)]
    sg = K.sbuf("sg", [128, D], F32)
    jk = K.sbuf("jk", [128, 512], BF16)
    hnb = K.sbuf("hnb", [128, D], BF16)
    ss = K.sbuf("ss", [128, 2, 8], F32, partial=True)
    stg = [K.sbuf(f"stg{i}", [128, KC, 512], BF16, partial=True) for i in range(2)]
    tp = [K.psum(f"tp{i}", [128, 1024], BF16) for i in range(2)]
    tiles = TOK_TILES if need_ctx else TOK_TILES[1:]
    bi = 0
    ti_ = 0
    for ti, (tok0, ntok) in enumerate(tiles):
        st = stg[ti % 2]
        for sb in range(ntok // 128):
            r0 = tok0 + sb * 128
            a, b, og = hf[bi % 2], hb[bi % 2], ot[bi % 2]
            bi += 1
            K.dma(K.sp, a[:], d["hf"][r0:r0 + 128, :], W=[a])
            K.dma(K.sp, b[:], d["hb"][r0:r0 + 128, :], W=[b])
            K.dma(K.sp, og[:], d["mO"][r0:r0 + 128, :], W=[og])
            K.op(K.pool, lambda e: e.tensor_tensor(a[:], a[:], b[:], ALU.add), R=[a, b], W=[a])
            for h in range(8):
                K.op(K.act, lambda e, h=h: e.activation(jk[:], a[:, h * 512:(h + 1) * 512], AF.Square, accum_out=ss[:, 0, h:h + 1]), R=[a], W=[jk, ss])
            K.op(K.act, lambda e: e.activation(ss[:, 1, :], ss[:, 0, :], AF.Sqrt, scale=1.0 / 512, bias=cm.eps[:, 0:1]), R=[ss, cm.eps], W=[ss])
            K.op(K.dve, lambda e: e.reciprocal(ss[:, 1, :], ss[:, 1, :]), R=[ss], W=[ss])
            K.op(K.act, lambda e: e.activation(sg[:], og[:], AF.Sigmoid), R=[og], W=[sg])
            for h in range(8):
                hs = slice(h * 512, (h + 1) * 512)
                K.op(K.dve, lambda e, h=h, hs=hs: e.scalar_tensor_tensor(a[:, hs], a[:, hs], ss[:, 1, h:h + 1], nwb[:, hs], ALU.mult, ALU.mult), R=[a, ss, nwb], W=[a])
            K.op(K.pool, lambda e: e.tensor_tensor(hnb[:], a[:], sg[:], ALU.mult), R=[a, sg], W=[hnb])
            for g in range(4):
                t = tp[ti_ % 2]
                ti_ += 1
                for c in range(8):
                    ch = g * 8 + c
                    K.op(K.pe, lambda e, c=c, ch=ch, t=t: e.transpose(t[:, c * 128:(c + 1) * 128], hnb[:, ch * 128:(ch + 1) * 128], cm.ident_b[:]), R=[hnb, cm.ident_b], W=[t])
                o_ap = st[:, g * 8:(g + 1) * 8, sb * 128:(sb + 1) * 128]
                i_ap = t[:, :].rearrange("p (c q) -> p c q", c=8)
                if g % 2 == 0:
                    K.op(K.act, lambda e, o_ap=o_ap, i_ap=i_ap: e.copy(o_ap, i_ap), R=[t], W=[st])
                else:
                    K.op(K.dve, lambda e, o_ap=o_ap, i_ap=i_ap: e.tensor_copy(o_ap, i_ap), R=[t], W=[st])
        K.dma(K.sp, d["OT"][:, :, tok0:tok0 + ntok].rearrange("k p t -> p k t"), st[:, :, 0:ntok], R=[st])
    K.end_phase()


def host_tri():
    s = np.arange(64)[:, None]
    t = np.arange(64)[None, :]
    tri = np.zeros((64, 4, 64), np.float32)
    tri[:, 0, :] = (s <= t)
    tri[:, 1, :] = (s >= t)
    return tri


def build_full():
    C = Prog()
    C.declare()
    nc = C.nc
    k = K(nc)
    d = C.d
    with nc.Block():
        setup_common(C, k)
        phase_mods(C, k, [0, 1, 2, 3])
        for l in range(DEPTH):
            need_ctx = l < DEPTH - 1
            if l % 2 == 0:
                phase_inproj_attn(C, k, l)
                phase_na(C, k, l, need_ctx)
                phase_win(C, k, l, need_ctx)
                phase_outproj(C, k, l, d["ab_w_out"][l // 2], need_ctx)
            else:
                phase_inproj_mlstm(C, k, l)
                phase_mlstm_scan(C, k, l, 0)
                phase_mlstm_scan(C, k, l, 1)
                phase_mlstm_post(C, k, l, need_ctx)
                phase_outproj(C, k, l, d["ml_w_out"][l // 2], need_ctx)
            phase_router(C, k, l, need_ctx)
            phase_moe(C, k, l, need_ctx)
        phase_final(C, k)
    return C, k


def kernel(x, c, ctx, c_ctx, ada_w, ada_b, norm1_w, norm2_w, ab_w_in, ab_w_out, na_rpb, win_sink,
           ml_w_in, ml_b_gates, ml_norm_w, ml_w_out, moe_router, moe_w1, moe_w3, moe_w2, final_norm_w):
    f = lambda a: np.ascontiguousarray(np.asarray(a, dtype=np.float32))
    C, k = build_full()
    hc = host_consts()
    shared = {
        "ada_w": f(ada_w), "ada_b": f(ada_b), "norm1_w": f(norm1_w), "norm2_w": f(norm2_w),
        "ab_w_in": f(ab_w_in), "ab_w_out": f(ab_w_out), "win_sink": f(win_sink),
        "ml_w_in": f(ml_w_in), "ml_b_gates": f(ml_b_gates), "ml_norm_w": f(ml_norm_w), "ml_w_out": f(ml_w_out),
        "moe_router": f(moe_router), "moe_w1": f(moe_w1), "moe_w3": f(moe_w3), "moe_w2": f(moe_w2),
        "final_norm_w": f(final_norm_w).reshape(1, D),
        "c_ident": hc["c_ident"], "c_perm": hc["c_perm"], "c_cos": hc["c_cos"], "c_sin": hc["c_sin"],
        "c_bm": host_bm(f(na_rpb)), "c_wmask": host_wmask(), "c_tri": host_tri(),
    }
    x, c, ctx, c_ctx = f(x), f(c), f(ctx), f(c_ctx)
    B = x.shape[0]
    in_maps = []
    for s in range(B):
        m = dict(shared)
        m["x"] = x[s]
        m["ctx"] = ctx[s]
        m["cvec"] = np.stack([c[s], c_ctx]).astype(np.float32)
        in_maps.append(m)
    res = run_bass_kernel_spmd(C.nc, in_maps, core_ids=list(range(B)))
    return np.stack([res.results[s]["y"] for s in range(B)]).astype(np.float32)
```

```python
import numpy as np
import ml_dtypes
from contextlib import ExitStack
import concourse.bass as bass
import concourse.mybir as mybir
from concourse.bass_utils import run_bass_kernel_spmd

F32 = mybir.dt.float32
BF16 = mybir.dt.bfloat16
AF = mybir.ActivationFunctionType
ALU = mybir.AluOpType
AX = mybir.AxisListType

D = 4096
NTOK = 4352
NCTX = 256
NLAT = 4096
DEPTH = 4
KC = 32
EPS = 1e-6
NEG = -30000.0


class Eng:
    def __init__(self, k, name, e, sem):
        self.k, self.name, self.e, self.sem = k, name, e, sem
        self.seq = 0
        self.insts = {}
        self.tick = []
        self.count = 0
        self.known = {}

    def ticket(self, seq):
        for s, c in reversed(self.tick[-64:]):
            if s < seq:
                break
        lo = None
        for s, c in reversed(self.tick):
            if s >= seq:
                lo = c
            else:
                break
        if lo is not None:
            return lo
        if seq not in self.insts:
            seq = max(self.insts)
        ins = self.insts[seq]
        self.count += 1
        ins.then_inc(self.sem, 1)
        self.tick.append((seq, self.count))
        if len(self.tick) > 256:
            self.tick = self.tick[-128:]
        for s in [s for s in self.insts if s <= seq]:
            del self.insts[s]
        return self.count


class DmaSem:
    def __init__(self, sem):
        self.sem = sem
        self.issued = 0


class Buf:
    def __init__(self, k, name, t, partial=False):
        self.k, self.name, self.t, self.partial = k, name, t, partial
        self.w = {}
        self.r = {}
        self.dsem = None

    def __getitem__(self, idx):
        return self.t[idx]


class K:
    def __init__(self, nc):
        self.nc = nc
        self.stack = ExitStack()
        self.pe = Eng(self, "pe", nc.tensor, self._sem("s_pe"))
        self.act = Eng(self, "act", nc.scalar, self._sem("s_act"))
        self.dve = Eng(self, "dve", nc.vector, self._sem("s_dve"))
        self.pool = Eng(self, "pool", nc.gpsimd, self._sem("s_pool"))
        self.sp = Eng(self, "sp", nc.sync, self._sem("s_sp"))
        self.engs = [self.pe, self.act, self.dve, self.pool, self.sp]
        self.dsem_free = [DmaSem(self._sem(f"s_dma{i}")) for i in range(64)]
        self.bar = self._sem("s_bar")
        self.bar_n = 0
        self.phase_stack = None
        self.phase_bufs = []
        self.n_inst = 0

    def _sem(self, name):
        return self.stack.enter_context(self.nc.semaphore(name))

    def begin_phase(self):
        self.phase_stack = ExitStack()
        self.phase_bufs = []

    def sbuf(self, name, shape, dt, partial=False):
        self.uid = getattr(self, "uid", 0) + 1
        t = self.phase_stack.enter_context(self.nc.sbuf_tensor(f"{name}_u{self.uid}", list(shape), dt))
        b = Buf(self, name, t, partial)
        self.phase_bufs.append(b)
        return b

    def psum(self, name, shape, dt, partial=False):
        self.uid = getattr(self, "uid", 0) + 1
        t = self.phase_stack.enter_context(self.nc.psum_tensor(f"{name}_u{self.uid}", list(shape), dt))
        b = Buf(self, name, t, partial)
        self.phase_bufs.append(b)
        return b

    def end_phase(self):
        for b in self.phase_bufs:
            if b.dsem is not None:
                self._wait(self.sp, b.dsem.sem, 16 * b.dsem.issued)
        self.barrier()
        for b in self.phase_bufs:
            if b.dsem is not None:
                self.dsem_free.append(b.dsem)
                b.dsem = None
        self.phase_stack.close()
        self.phase_stack = None
        self.phase_bufs = []

    def barrier(self):
        for e in self.engs:
            if e.seq > 0 and e is not self.sp:
                last = e.seq
                if last in e.insts or any(s >= last for s, _ in e.tick):
                    t = e.ticket(last)
                    self._wait(e, e.sem, t)
        self.bar_n += 1
        for e in self.engs:
            e.e.sem_inc(self.bar, 1)
        for e in self.engs:
            e.e.wait_ge(self.bar, len(self.engs) * self.bar_n)

    def _wait(self, eng, sem, val):
        key = id(sem)
        if eng.known.get(key, 0) >= val:
            return
        eng.known[key] = val
        eng.e.wait_ge(sem, val)

    def _wait_dep(self, eng, key, val, same_engine_ok):
        if isinstance(key, Eng):
            if key is eng and not same_engine_ok:
                return
            if key is eng and key is self.pe:
                return
            t = key.ticket(val)
            self._wait(eng, key.sem, t)
        else:
            b = key[1]
            if b.dsem is None:
                return
            self._wait(eng, b.dsem.sem, 16 * b.dsem.issued)

    def _deps(self, eng, R, W):
        for b in R:
            for key, val in list(b.w.items()):
                self._wait_dep(eng, key, val, same_engine_ok=True)
        for b in W:
            if not b.partial:
                for key, val in list(b.w.items()):
                    self._wait_dep(eng, key, val, same_engine_ok=False)
            for key, val in list(b.r.items()):
                self._wait_dep(eng, key, val, same_engine_ok=False)

    def _record(self, key, val, R, W):
        for b in W:
            if b.partial:
                b.w[key] = val
            else:
                b.w = {key: val}
                b.r = {}
        for b in R:
            b.r[key] = val

    def op(self, eng, fn, R=(), W=()):
        self._deps(eng, R, W)
        ins = fn(eng.e)
        eng.seq += 1
        eng.insts[eng.seq] = ins
        if len(eng.insts) > 4096:
            for s in sorted(eng.insts)[:2048]:
                del eng.insts[s]
        self._record(eng, eng.seq, R, W)
        self.n_inst += 1
        return ins

    def dma(self, eng, out, in_, R=(), W=(), **kw):
        self._deps(eng, R, W)
        sb = (list(W) + list(R))[0]
        if sb.dsem is None:
            sb.dsem = self.dsem_free.pop()
        ins = eng.e.dma_start(out=out, in_=in_, **kw)
        ins.then_inc(sb.dsem.sem, 16)
        sb.dsem.issued += 1
        self._record(("dma", sb), True, R, W)
        self.n_inst += 1
        return ins


TOK_TILES = [(0, 256)] + [(256 + 512 * i, 512) for i in range(8)]


class Prog:
    def __init__(self, kinds=None, layers=(0, 1, 2, 3), only=None, shapes=None):
        self.nc = nc = bass.Bass("TRN2", target_bir_lowering=False)
        self.kinds = kinds or {}
        self.layers = layers
        self.only = only
        self.shapes = shapes or {}
        self.d = {}

    def dram(self, name, shape, dt, kind="Internal"):
        kind = self.kinds.get(name, kind)
        if kind == "ExternalInput" and self.only is not None and name not in self.only:
            return None
        shape = self.shapes.get(name, shape)
        self.d[name] = self.nc.dram_tensor(name, list(shape), dt, kind=kind).ap()
        return self.d[name]

    def declare(self):
        I = "ExternalInput"
        d = self.dram
        d("x", [NLAT, D], F32, I); d("ctx", [NCTX, D], F32, I); d("cvec", [2, D], F32, I)
        d("ada_w", [DEPTH, D, 6 * D], F32, I); d("ada_b", [DEPTH, 6 * D], F32, I)
        d("norm1_w", [DEPTH, D], F32, I); d("norm2_w", [DEPTH, D], F32, I)
        d("ab_w_in", [2, D, 9216], F32, I); d("ab_w_out", [2, D, D], F32, I)
        d("win_sink", [2, 16], F32, I)
        d("ml_w_in", [2, D, 12320], F32, I); d("ml_b_gates", [2, 32], F32, I)
        d("ml_norm_w", [2, D], F32, I); d("ml_w_out", [2, D, D], F32, I)
        d("moe_router", [DEPTH, D, 16], F32, I)
        d("moe_w1", [DEPTH, 16, D, 256], F32, I); d("moe_w3", [DEPTH, 16, D, 256], F32, I)
        d("moe_w2", [DEPTH, 16, 256, D], F32, I)
        d("final_norm_w", [1, D], F32, I)
        d("c_ident", [128, 128], F32, I)
        d("c_bm", [2, 16, 128, 21, 128], F32, I)
        d("c_cos", [128, NLAT], F32, I); d("c_sin", [128, NLAT], F32, I); d("c_perm", [128, 128], F32, I)
        d("c_wmask", [128, 2, 128], F32, I)
        d("c_tri", [64, 4, 64], F32, I)
        d("y", [NLAT, D], F32, "ExternalOutput")
        d("xl", [NTOK, D], F32); d("xm", [NTOK, D], F32); d("mod", [DEPTH, 2, 6 * D], F32)
        d("QaT", [16, 128, NTOK], BF16); d("KaT", [16, 128, NTOK], BF16); d("Va", [NTOK, 2048], BF16)
        d("QbT", [16, 128, NTOK], BF16); d("KbT", [4, 128, NTOK], BF16); d("Vb", [NTOK, 512], BF16)
        d("OT", [32, 128, NTOK], BF16)
        d("gm", [16, NTOK], F32)
        d("mqT", [16, 128, NTOK], BF16); d("mkT", [16, 128, NTOK], BF16)
        d("mK", [NTOK, 2048], BF16); d("mV", [NTOK, D], BF16); d("mO", [NTOK, D], BF16)
        d("mG", [NTOK, 32], F32); d("hf", [NTOK, D], F32); d("hb", [NTOK, D], F32)


def mm(K, ps_buf, out_ap, lhsT, rhs, start, stop, R):
    return K.op(K.pe, lambda e: e.matmul(out_ap, lhsT, rhs, start=start, stop=stop), R=R, W=[ps_buf])


class Common:
    pass


def setup_common(C, K, copy_inputs=True):
    nc = C.nc
    st = K.stack
    cm = Common()
    def gbuf(name, shape, dt):
        t = st.enter_context(nc.sbuf_tensor(name, list(shape), dt))
        return Buf(K, name, t)
    cm.ident_f = gbuf("ident_f", [128, 128], F32)
    cm.ident_b = gbuf("ident_b", [128, 128], BF16)
    cm.ones_b = gbuf("ones_b", [128, 128], BF16)
    cm.ones_f = gbuf("ones_f", [128, 128], F32)
    K.dma(K.sp, cm.ident_f[:], C.d["c_ident"][:, :], W=[cm.ident_f])
    K.dma(K.pool, cm.ident_b[:], C.d["c_ident"][:, :], W=[cm.ident_b])
    K.op(K.dve, lambda e: e.memset(cm.ones_b[:], 1.0), W=[cm.ones_b])
    K.op(K.dve, lambda e: e.memset(cm.ones_f[:], 1.0), W=[cm.ones_f])
    cm.eps = gbuf("eps_t", [128, 1], F32)
    K.op(K.dve, lambda e: e.memset(cm.eps[:], EPS), W=[cm.eps])
    C.cm = cm
    if not copy_inputs:
        return
    tmp = gbuf("cp_sem_holder", [1, 2], F32)
    K.dma(K.sp, C.d["xl"][0:NCTX, :], C.d["ctx"][:, :], W=[tmp])
    K.dma(K.sp, C.d["xl"][NCTX:NTOK, :], C.d["x"][:, :], W=[tmp])
    K._wait(K.sp, tmp.dsem.sem, 16 * tmp.dsem.issued)
    K.barrier()


def load_vecs_pp(C, K, rows, out_buf, scratch_v, ps_buf):
    cm = C.cm
    n = len(rows)
    for j, r in enumerate(rows):
        K.dma(K.sp, scratch_v[0:32, j, :], r.rearrange("(c p) -> c p", p=128), W=[scratch_v])
    for j in range(n):
        K.op(K.pe, lambda e, j=j: e.transpose(ps_buf[:, j * 32:(j + 1) * 32], scratch_v[0:32, j, :], cm.ident_f[0:32, 0:32]),
             R=[scratch_v, cm.ident_f], W=[ps_buf])
    K.op(K.dve, lambda e: e.tensor_copy(out_buf[:, 0:n, :].rearrange("p j c -> p (j c)"), ps_buf[:, 0:n * 32]), R=[ps_buf], W=[out_buf])


def phase_mods(C, K, layers):
    nc, d, cm = C.nc, C.d, C.cm
    K.begin_phase()
    vs = K.sbuf("vs", [32, 2, 128], F32)
    ps = K.psum("ps_m", [128, 512], F32)
    cpp = K.sbuf("cpp", [128, 2, 32], F32)
    scT = K.sbuf("scT", [128, 32, 2], BF16)
    load_vecs_pp(C, K, [d["cvec"][0], d["cvec"][1]], cpp, vs, ps)
    K.op(K.act, lambda e: e.activation(scT[:].rearrange("p c r -> p r c"), cpp[:], AF.Silu), R=[cpp], W=[scT])
    wbs = [K.sbuf(f"wb{i}", [128, KC, 512], BF16) for i in range(2)]
    bbs = [K.sbuf(f"bb{i}", [2, 512], F32) for i in range(2)]
    obs = [K.sbuf(f"ob{i}", [2, 512], F32) for i in range(2)]
    pss = [K.psum(f"ps_mod{i}", [128, 512], F32) for i in range(2)]
    it = 0
    for l in layers:
        for g in range(48):
            wb, bb, ob, pq = wbs[it % 2], bbs[it % 2], obs[it % 2], pss[it % 2]
            it += 1
            cs = slice(g * 512, (g + 1) * 512)
            K.dma(K.pool, wb[:], d["ada_w"][l][:, cs].rearrange("(c p) n -> p c n", p=128), W=[wb])
            K.dma(K.sp, bb[:], d["ada_b"][l:l + 1, cs].partition_broadcast(2) if False else d["ada_b"][l:l + 1, cs].broadcast_to([2, 512]), W=[bb])
            for kc in range(KC):
                mm(K, pq, pq[0:2, :], scT[:, kc, :], wb[:, kc, :], kc == 0, kc == KC - 1, R=[scT, wb])
            K.op(K.dve, lambda e, ob=ob, pq=pq, bb=bb: e.tensor_tensor(ob[:], pq[0:2, :], bb[:], ALU.add), R=[pq, bb], W=[ob])
            K.dma(K.sp, d["mod"][l][:, cs], ob[:], R=[ob])
    K.end_phase()


class NormBufs:
    def __init__(self, K, tag=""):
        self.xt = K.sbuf("nb_xt" + tag, [128, D], F32)
        self.xs = K.sbuf("nb_xs" + tag, [128, D], BF16)
        self.ss = K.sbuf("nb_ss" + tag, [128, 2], F32)
        self.tp = [K.psum(f"nb_tp{i}" + tag, [128, 1024], BF16) for i in range(2)]
        self.n = 0


def norm_block(C, K, nb, src_rows, gsh, row, hT, col0):
    cm = C.cm
    xt, xs, ss = nb.xt, nb.xs, nb.ss
    K.dma(K.sp, xt[:], src_rows, W=[xt])
    K.op(K.act, lambda e: e.activation(xs[:], xt[:], AF.Square, accum_out=ss[:, 0:1]), R=[xt], W=[xs, ss])
    K.op(K.act, lambda e: e.activation(ss[:, 1:2], ss[:, 0:1], AF.Sqrt, scale=1.0 / D, bias=cm.eps[:, 0:1]), R=[ss, cm.eps], W=[ss])
    K.op(K.dve, lambda e: e.reciprocal(ss[:, 1:2], ss[:, 1:2]), R=[ss], W=[ss])
    K.op(K.act, lambda e: e.activation(xs[:].rearrange("t (c p) -> t c p", p=128), xt[:].rearrange("t (p c) -> t c p", c=KC), AF.Copy, scale=ss[:, 1:2]), R=[xt, ss], W=[xs])
    for g in range(4):
        tp = nb.tp[nb.n % 2]
        nb.n += 1
        for c in range(8):
            ch = g * 8 + c
            K.op(K.pe, lambda e, c=c, ch=ch, tp=tp: e.transpose(tp[:, c * 128:(c + 1) * 128], xs[:, ch * 128:(ch + 1) * 128], cm.ident_b[:]),
                 R=[xs, cm.ident_b], W=[tp])
        for c in range(8):
            ch = g * 8 + c
            o = hT[:, ch, col0:col0 + 128]
            i = tp[:, c * 128:(c + 1) * 128]
            gs = gsh[:, 2 * row, ch:ch + 1]
            sh = gsh[:, 2 * row + 1, ch:ch + 1]
            if g % 2 == 0:
                K.op(K.act, lambda e, o=o, i=i, gs=gs, sh=sh: e.activation(o, i, AF.Identity, scale=gs, bias=sh), R=[tp, gsh], W=[hT])
            else:
                K.op(K.dve, lambda e, o=o, i=i, gs=gs, sh=sh: e.tensor_scalar(o, i, gs, sh, ALU.mult, ALU.add), R=[tp, gsh], W=[hT])


def build_gsh(C, K, l, which, gsh, vs, ps, tmp):
    d = C.d
    nw = d["norm1_w"] if which == 1 else d["norm2_w"]
    o = 0 if which == 1 else 3
    m = d["mod"][l]
    rows = [nw[l], m[0, (o + 1) * D:(o + 2) * D], m[0, o * D:(o + 1) * D], m[1, (o + 1) * D:(o + 2) * D], m[1, o * D:(o + 1) * D]]
    for j, rw in enumerate(rows):
        K.dma(K.sp, tmp[:, j, :], rw.rearrange("(p c) -> p c", c=KC), W=[tmp])
    for r in range(2):
        K.op(K.dve, lambda e, r=r: e.scalar_tensor_tensor(gsh[:, 2 * r, :], tmp[:, 1 + 2 * r, :], 1.0, tmp[:, 0, :], ALU.add, ALU.mult),
             R=[tmp], W=[gsh])
        K.op(K.dve, lambda e, r=r: e.tensor_copy(gsh[:, 2 * r + 1, :], tmp[:, 2 + 2 * r, :]), R=[tmp], W=[gsh])


def phase_inproj(C, K, l, groups, extra_setup=None):
    nc, d, cm = C.nc, C.d, C.cm
    K.begin_phase()
    S = type("S", (), {})()
    nb = NormBufs(K)
    vs = K.sbuf("vs", [32, 5, 128], F32)
    tmpv = K.sbuf("tmpv", [128, 5, 32], F32)
    gsh = K.sbuf("gsh", [128, 4, 32], F32)
    acc = [K.psum(f"acc{i}", [128, 512], F32) for i in range(4)]
    S.rp = [K.psum(f"rp{i}", [128, 512], F32) for i in range(2)]
    build_gsh(C, K, l, 1, gsh, vs, acc[0], tmpv)
    hTs = [K.sbuf(f"hT{i}", [128, KC, 512], BF16, partial=True) for i in range(2)]
    wbs = [K.sbuf(f"wb{i}", [128, KC, 512], BF16) for i in range(2)]
    S.stg = [K.sbuf(f"stg{i}", [128, 512], BF16) for i in range(4)]
    S.stgf = [K.sbuf(f"stgf{i}", [128, 512], F32) for i in range(3)]
    S.n = 0
    S.K, S.C = K, C
    if extra_setup:
        extra_setup(S)
    wi = 0
    ai = 0
    for ti, (tok0, ntok) in enumerate(TOK_TILES):
        hT = hTs[ti % 2]
        row = 1 if ti == 0 else 0
        nsub = ntok // 128
        for sb in range(nsub):
            norm_block(C, K, nb, d["xl"][tok0 + sb * 128: tok0 + (sb + 1) * 128, :], gsh, row, hT, sb * 128)
        S.tok0, S.ntok, S.is_ctx = tok0, ntok, ti == 0
        if hasattr(S, "tile_setup"):
            S.tile_setup(S)
        for g in groups:
            wb = wbs[wi % 2]
            wi += 1
            ncols = g["ncols"]
            K.dma(K.pool, wb[:, :, 0:ncols], g["w"].rearrange("(p c) n -> p c n", c=KC), W=[wb])
            if g["kind"] == "FM":
                for b in range(ncols // 128):
                    ps = acc[ai % 4]
                    ai += 1
                    for kc in range(KC):
                        mm(K, ps, ps[:, 0:ntok], wb[:, kc, b * 128:(b + 1) * 128], hT[:, kc, 0:ntok], kc == 0, kc == KC - 1, R=[wb, hT])
                    g["evac"](S, g, b, ps)
            else:
                for sb in range(nsub):
                    ps = acc[ai % 4]
                    ai += 1
                    for kc in range(KC):
                        mm(K, ps, ps[:, 0:ncols], hT[:, kc, sb * 128:(sb + 1) * 128], wb[:, kc, 0:ncols], kc == 0, kc == KC - 1, R=[wb, hT])
                    g["evac"](S, g, sb, ps)
    K.end_phase()


def ev_fm_plain(dst, scale=None):
    def f(S, g, b, ps):
        K = S.K
        st = S.stg[S.n % 4]
        S.n += 1
        n = S.ntok
        if scale is None:
            K.op(K.act, lambda e: e.copy(st[:, 0:n], ps[:, 0:n]), R=[ps], W=[st])
        else:
            K.op(K.act, lambda e: e.activation(st[:, 0:n], ps[:, 0:n], AF.Copy, scale=scale), R=[ps], W=[st])
        K.dma(K.sp, dst[g["b0"] + b, :, S.tok0:S.tok0 + n], st[:, 0:n], R=[st])
    return f


def ev_tm_plain(dst, dt=BF16):
    def f(S, g, sb, ps):
        K = S.K
        nco = g["ncols"]
        if dt == BF16:
            st = S.stg[S.n % 4]
        else:
            st = S.stgf[S.n % 3]
        S.n += 1
        if S.n % 2 == 0:
            K.op(K.act, lambda e: e.copy(st[:, 0:nco], ps[:, 0:nco]), R=[ps], W=[st])
        else:
            K.op(K.dve, lambda e: e.tensor_copy(st[:, 0:nco], ps[:, 0:nco]), R=[ps], W=[st])
        r0 = S.tok0 + sb * 128
        K.dma(K.sp, dst[r0:r0 + 128, g["c0"]:g["c0"] + nco], st[:, 0:nco], R=[st])
    return f


def ev_fm_rope(dst, scale):
    plain = ev_fm_plain(dst, scale)
    def f(S, g, b, ps):
        K, C = S.K, S.C
        if S.is_ctx:
            return plain(S, g, b, ps)
        n = S.ntok
        xs = S.stgf[S.n % 3]
        t1 = S.stgf[(S.n + 1) % 3]
        t2 = S.stgf[(S.n + 2) % 3]
        st = S.stg[S.n % 4]
        rp = S.rp[S.n % 2]
        S.n += 1
        K.op(K.act, lambda e: e.activation(xs[:, 0:n], ps[:, 0:n], AF.Copy, scale=(1.0 if scale is None else scale)), R=[ps], W=[xs])
        mm(K, rp, rp[:, 0:n], S.perm[:], xs[:, 0:n], True, True, R=[S.perm, xs])
        K.op(K.dve, lambda e: e.tensor_tensor(t1[:, 0:n], xs[:, 0:n], S.cos[:, 0:n], ALU.mult), R=[xs, S.cos], W=[t1])
        K.op(K.dve, lambda e: e.tensor_tensor(t2[:, 0:n], rp[:, 0:n], S.sin[:, 0:n], ALU.mult), R=[rp, S.sin], W=[t2])
        K.op(K.dve, lambda e: e.tensor_tensor(st[:, 0:n], t1[:, 0:n], t2[:, 0:n], ALU.add), R=[t1, t2], W=[st])
        K.dma(K.sp, dst[g["b0"] + b, :, S.tok0:S.tok0 + n], st[:, 0:n], R=[st])
    return f


def phase_inproj_attn(C, K, l):
    d = C.d
    e = l // 2
    w = d["ab_w_in"][e]
    qs = 128 ** -0.5
    groups = []
    for i in range(4):
        groups.append(dict(kind="FM", w=w[:, i * 512:(i + 1) * 512], ncols=512, b0=4 * i, evac=ev_fm_plain(d["QaT"], qs)))
    for i in range(4):
        groups.append(dict(kind="FM", w=w[:, 2048 + i * 512:2048 + (i + 1) * 512], ncols=512, b0=4 * i, evac=ev_fm_plain(d["KaT"])))
    for i in range(4):
        groups.append(dict(kind="TM", w=w[:, 4096 + i * 512:4096 + (i + 1) * 512], ncols=512, c0=512 * i, evac=ev_tm_plain(d["Va"])))
    for i in range(4):
        groups.append(dict(kind="FM", w=w[:, 6144 + i * 512:6144 + (i + 1) * 512], ncols=512, b0=4 * i, evac=ev_fm_rope(d["QbT"], qs)))
    groups.append(dict(kind="FM", w=w[:, 8192:8704], ncols=512, b0=0, evac=ev_fm_rope(d["KbT"], None)))
    groups.append(dict(kind="TM", w=w[:, 8704:9216], ncols=512, c0=0, evac=ev_tm_plain(d["Vb"])))

    def setup(S):
        S.perm = K.sbuf("perm", [128, 128], F32)
        K.dma(K.sp, S.perm[:], d["c_perm"][:, :], W=[S.perm])
        S.cos = K.sbuf("cos", [128, 512], F32)
        S.sin = K.sbuf("sin", [128, 512], F32)

        def tile_setup(S):
            if not S.is_ctx:
                p0 = S.tok0 - NCTX
                K.dma(K.sp, S.cos[:], d["c_cos"][:, p0:p0 + 512], W=[S.cos])
                K.dma(K.sp, S.sin[:], d["c_sin"][:, p0:p0 + 512], W=[S.sin])
        S.tile_setup = tile_setup
    phase_inproj(C, K, l, groups, setup)


def host_consts():
    c = {}
    c["c_ident"] = np.eye(128, dtype=np.float32)
    perm = np.zeros((128, 128), np.float32)
    for m in range(128):
        p = m + 32 if (m % 64) < 32 else m - 32
        perm[p, m] = 1.0
    c["c_perm"] = perm
    inv = (np.float32(10000.0) ** (-np.arange(32, dtype=np.float32) / np.float32(32))).astype(np.float32)
    t = np.arange(NLAT)
    rowp = (t // 64).astype(np.float32)
    colp = (t % 64).astype(np.float32)
    cos = np.zeros((128, NLAT), np.float32)
    sin = np.zeros((128, NLAT), np.float32)
    for dd in range(128):
        pos = rowp if dd < 64 else colp
        ang = (pos * inv[dd % 32]).astype(np.float32)
        cos[dd] = np.cos(ang)
        sin[dd] = np.sin(ang) * (-1.0 if (dd % 64) < 32 else 1.0)
    c["c_cos"], c["c_sin"] = cos, sin
    return c


def na_key_tiles(j):
    if j <= 1:
        kps = [0, 1, 2, 3]
        base = 5 + 4 * j
        idx = [base + i for i in range(4)]
    elif j >= 30:
        kps = [28, 29, 30, 31]
        base = 13 + 4 * (j - 30)
        idx = [base + i for i in range(4)]
    else:
        kps = [j - 2, j - 1, j, j + 1, j + 2]
        idx = [0, 1, 2, 3, 4]
    return kps, idx


def phase_na(C, K, l, need_ctx):
    nc, d, cm = C.nc, C.d, C.cm
    e = l // 2
    K.begin_phase()
    QT = [K.sbuf(f"QT{i}", [128, NTOK], BF16) for i in range(2)]
    KT = [K.sbuf(f"KT{i}", [128, NTOK], BF16) for i in range(2)]
    V = [K.sbuf(f"V{i}", [128, 34, 128], BF16) for i in range(2)]
    BM = [K.sbuf(f"BM{i}", [128, 21, 128], F32) for i in range(2)]
    OS = [K.sbuf(f"OS{i}", [128, NTOK], BF16, partial=True) for i in range(2)]
    sA = [K.psum(f"sA{i}", [128, 512], F32) for i in range(2)]
    sB = [K.psum(f"sB{i}", [128, 512], F32) for i in range(2)]
    po = [K.psum(f"po{i}", [128, 256], F32) for i in range(2)]
    pT = [K.sbuf(f"pT{i}", [128, 896], BF16) for i in range(2)]
    rc = [K.sbuf(f"rc{i}", [128, 128], F32) for i in range(2)]
    blocks = [("lat", j) for j in range(32)] + ([("ctx", 0), ("ctx", 1)] if need_ctx else [])
    it = 0
    for h in range(16):
        qt, kt, v, bm, osb = QT[h % 2], KT[h % 2], V[h % 2], BM[h % 2], OS[h % 2]
        K.dma(K.sp, qt[:], d["QaT"][h], W=[qt])
        K.dma(K.sp, kt[:], d["KaT"][h], W=[kt])
        K.dma(K.sp, v[:], d["Va"].rearrange("(t p) c -> p t c", p=128)[:, :, h * 128:(h + 1) * 128], W=[v])
        K.dma(K.sp, bm[:], d["c_bm"][e, h], W=[bm])

        def qk(blk, i):
            kind, j = blk
            if kind == "lat":
                kps, idx = na_key_tiles(j)
                tiles = [(2 + kp, ix) for kp, ix in zip(kps, idx)] + [(0, None), (1, None)]
                q0 = NCTX + 128 * j
            else:
                tiles = [(0, None), (1, None)]
                q0 = 128 * j
            for s, (t, ix) in enumerate(tiles):
                bank = sA[i % 2] if s < 4 else sB[i % 2]
                o = bank[:, (s % 4) * 128:(s % 4 + 1) * 128]
                mm(K, bank, o, kt[:, t * 128:(t + 1) * 128], qt[:, q0:q0 + 128], True, ix is None, R=[kt, qt])
                if ix is not None:
                    mm(K, bank, o, bm[:, ix, :], cm.ident_f[:], False, True, R=[bm, cm.ident_f])
            return tiles, q0

        def pv(blk, i, tiles, q0):
            n = len(tiles)
            p = pT[i % 2]
            na = min(n, 4)
            K.op(K.act, lambda e: e.activation(p[:, 0:na * 128], sA[i % 2][:, 0:na * 128], AF.Exp), R=[sA[i % 2]], W=[p])
            if n > 4:
                K.op(K.act, lambda e: e.activation(p[:, 512:n * 128], sB[i % 2][:, 0:(n - 4) * 128], AF.Exp), R=[sB[i % 2]], W=[p])
            pq = po[i % 2]
            for s, (t, ix) in enumerate(tiles):
                mm(K, pq, pq[:, 0:128], v[:, t, :], p[:, s * 128:(s + 1) * 128], s == 0, s == n - 1, R=[v, p])
            for s, (t, ix) in enumerate(tiles):
                mm(K, pq, pq[:, 128:256], cm.ones_b[:], p[:, s * 128:(s + 1) * 128], s == 0, s == n - 1, R=[cm.ones_b, p])
            r = rc[i % 2]
            K.op(K.dve, lambda e: e.reciprocal(r[:], pq[:, 128:256]), R=[pq], W=[r])
            K.op(K.dve, lambda e: e.tensor_tensor(osb[:, q0:q0 + 128], pq[:, 0:128], r[:], ALU.mult), R=[pq, r], W=[osb])

        prev = None
        for blk in blocks:
            cur = (blk, it) + qk(blk, it)
            it += 1
            if prev is not None:
                pv(*prev)
            prev = cur
        pv(*prev)
        K.dma(K.sp, d["OT"][h], osb[:], R=[osb])
    K.end_phase()


def phase_win(C, K, l, need_ctx):
    nc, d, cm = C.nc, C.d, C.cm
    e = l // 2
    K.begin_phase()
    QT = [K.sbuf(f"QT{i}", [128, 4, NTOK], BF16) for i in range(2)]
    KT = [K.sbuf(f"KT{i}", [128, NTOK], BF16) for i in range(2)]
    V = [K.sbuf(f"V{i}", [128, 34, 128], BF16) for i in range(2)]
    OS = K.sbuf("OS", [128, 4, NTOK], BF16, partial=True)
    wm = K.sbuf("wm", [128, 2, 128], F32)
    id4 = K.sbuf("id4", [128, 4, 128], F32)
    K.dma(K.sp, wm[:], d["c_wmask"][:, :, :], W=[wm])
    for i in range(4):
        K.dma(K.sp, id4[:, i, :], d["c_ident"][:, :], W=[id4])
    sk = K.sbuf("sk", [1, 16], F32)
    esr = K.sbuf("esr", [1, 16, 128], F32)
    K.dma(K.sp, sk[:], d["win_sink"][e:e + 1, :], W=[sk])
    K.op(K.act, lambda e_: e_.activation(sk[:], sk[:], AF.Exp), R=[sk], W=[sk])
    for h in range(16):
        K.op(K.dve, lambda e_, h=h: e_.tensor_scalar(esr[0:1, h, :], cm.ones_f[0:1, 0:128], sk[0:1, h:h + 1], None, ALU.mult), R=[sk, cm.ones_f], W=[esr])
    sb = [K.psum(f"sb{i}", [128, 512], F32) for i in range(5)]
    po = K.psum("po", [128, 512], F32)
    pm = K.psum("pm", [128, 512], F32)
    pT = [K.sbuf(f"pT{i}", [128, 5, 512], BF16) for i in range(2)]
    rc = K.sbuf("rc", [128, 512], F32)
    blocks = [("lat", j) for j in range(32)] + ([("ctx", 0), ("ctx", 1)] if need_ctx else [])
    it = 0
    for g in range(4):
        qt, kt, v = QT[g % 2], KT[g % 2], V[g % 2]
        K.dma(K.sp, qt[:], d["QbT"][4 * g:4 * g + 4].rearrange("h p t -> p h t"), W=[qt])
        K.dma(K.sp, kt[:], d["KbT"][g], W=[kt])
        K.dma(K.sp, v[:], d["Vb"].rearrange("(t p) c -> p t c", p=128)[:, :, g * 128:(g + 1) * 128], W=[v])
        for blk in blocks:
            kind, j = blk
            if kind == "lat":
                tiles = []
                if j > 0:
                    tiles.append((2 + j - 1, 0))
                tiles.append((2 + j, None))
                if j < 31:
                    tiles.append((2 + j + 1, 1))
                tiles += [(0, None), (1, None)]
                q0 = NCTX + 128 * j
            else:
                tiles = [(0, None), (1, None)]
                q0 = 128 * j
            n = len(tiles)
            p = pT[it % 2]
            it += 1
            for s, (t, mi) in enumerate(tiles):
                bank = sb[s]
                mm(K, bank, bank[:], kt[:, t * 128:(t + 1) * 128], qt[:, :, q0:q0 + 128], True, mi is None, R=[kt, qt])
                if mi is not None:
                    mm(K, bank, bank[:], wm[:, mi, :], id4[:], False, True, R=[wm, id4])
                K.op(K.act, lambda e_, s=s, bank=bank: e_.activation(p[:, s, :], bank[:], AF.Exp), R=[bank], W=[p])
            for s, (t, mi) in enumerate(tiles):
                mm(K, po, po[:], v[:, t, :], p[:, s, :], s == 0, s == n - 1, R=[v, p])
            for s, (t, mi) in enumerate(tiles):
                mm(K, pm, pm[:], cm.ones_b[:], p[:, s, :], s == 0, False, R=[cm.ones_b, p])
            mm(K, pm, pm[:], cm.ones_f[0:1, 0:128], esr[0:1, 4 * g:4 * g + 4, :], False, True, R=[cm.ones_f, esr])
            K.op(K.dve, lambda e_: e_.reciprocal(rc[:], pm[:]), R=[pm], W=[rc])
            K.op(K.dve, lambda e_: e_.tensor_tensor(OS[:, :, q0:q0 + 128], po[:].rearrange("p (h q) -> p h q", h=4), rc[:].rearrange("p (h q) -> p h q", h=4), ALU.mult),
                 R=[po, rc], W=[OS])
        for hh in range(4):
            K.dma(K.sp, d["OT"][16 + 4 * g + hh], OS[:, hh, :], R=[OS])
    K.end_phase()


def phase_outproj(C, K, l, w_out, need_ctx):
    nc, d, cm = C.nc, C.d, C.cm
    K.begin_phase()
    wbs = [K.sbuf(f"wb{i}", [128, KC, 512], BF16) for i in range(2)]
    ots = [K.sbuf(f"ot{i}", [128, KC, 512], BF16) for i in range(2)]
    gb = [K.sbuf(f"gb{i}", [128, 2, 512], F32) for i in range(2)]
    xin = [K.sbuf(f"xin{i}", [128, 512], F32) for i in range(3)]
    tt_ = [K.sbuf(f"tt{i}", [128, 512], F32) for i in range(3)]
    acc = [K.psum(f"acc{i}", [128, 512], F32) for i in range(4)]
    tiles = TOK_TILES if need_ctx else TOK_TILES[1:]
    oi = 0
    xi = 0
    for g in range(8):
        cs = slice(g * 512, (g + 1) * 512)
        wb, gbt = wbs[g % 2], gb[g % 2]
        K.dma(K.pool, wb[:], w_out[:, cs].rearrange("(c p) n -> p c n", p=128), W=[wb])
        for r in range(2):
            K.dma(K.sp, gbt[:, r, :], d["mod"][l][r:r + 1, 2 * D + g * 512:2 * D + (g + 1) * 512].broadcast_to([128, 512]), W=[gbt])
        for (tok0, ntok) in tiles:
            ot = ots[oi % 2]
            oi += 1
            row = 1 if tok0 == 0 else 0
            K.dma(K.sp, ot[:, :, 0:ntok], d["OT"][:, :, tok0:tok0 + ntok].rearrange("k p t -> p k t"), W=[ot])
            for sb in range(ntok // 128):
                ps = acc[xi % 4]
                xt, t = xin[xi % 3], tt_[xi % 3]
                xi += 1
                r0 = tok0 + sb * 128
                K.dma(K.sp, xt[:], d["xl"][r0:r0 + 128, cs], W=[xt])
                for kc in range(KC):
                    mm(K, ps, ps[:], ot[:, kc, sb * 128:(sb + 1) * 128], wb[:, kc, :], kc == 0, kc == KC - 1, R=[ot, wb])
                K.op(K.dve, lambda e_, t=t, ps=ps: e_.tensor_tensor(t[:], ps[:], gbt[:, row, :], ALU.mult), R=[ps, gbt], W=[t])
                K.op(K.dve, lambda e_, t=t, xt=xt: e_.tensor_tensor(t[:], t[:], xt[:], ALU.add), R=[t, xt], W=[t])
                K.dma(K.sp, d["xm"][r0:r0 + 128, cs], t[:], R=[t])
    K.end_phase()


def host_bm(rpb):
    q = np.arange(128)
    key = np.arange(128)
    mats = [(10, 10 + off) for off in (-2, -1, 0, 1, 2)]
    for j in (0, 1):
        mats += [(j, kp) for kp in (0, 1, 2, 3)]
    for j in (30, 31):
        mats += [(j, kp) for kp in (28, 29, 30, 31)]
    out = np.full((2, 16, 128, 21, 128), NEG, np.float32)
    for m, (j, kp) in enumerate(mats):
        rq = (2 * j + q // 64)[:, None]
        cq = (q % 64)[:, None]
        rk = (2 * kp + key // 64)[None, :]
        ck = (key % 64)[None, :]
        rs = np.clip(rq - 4, 0, 56)
        cs = np.clip(cq - 8, 0, 48)
        valid = (rk >= rs) & (rk <= rs + 7) & (ck >= cs) & (ck <= cs + 15)
        dr = np.clip(rk - rq + 7, 0, 14)
        dc = np.clip(ck - cq + 15, 0, 30)
        g = rpb[:, :, dr, dc]
        out[:, :, :, m, :] = np.where(valid[None, None], g, np.float32(NEG))
    return out


def host_wmask():
    q = np.arange(128)[:, None]
    k = np.arange(128)[None, :]
    wm = np.zeros((128, 2, 128), np.float32)
    wm[:, 0, :] = np.where(q <= k, 0.0, NEG)
    wm[:, 1, :] = np.where(k <= q, 0.0, NEG)
    return wm


def phase_router(C, K, l, need_ctx, n_iter=34):
    nc, d, cm = C.nc, C.d, C.cm
    K.begin_phase()
    nb = NormBufs(K)
    vs = K.sbuf("vs", [32, 5, 128], F32)
    tmpv = K.sbuf("tmpv", [128, 5, 32], F32)
    gsh = K.sbuf("gsh", [128, 4, 32], F32)
    acc = [K.psum(f"acc{i}", [128, 512], F32) for i in range(2)]
    build_gsh(C, K, l, 2, gsh, vs, acc[0], tmpv)
    hTs = [K.sbuf(f"hT{i}", [128, KC, 512], BF16, partial=True) for i in range(2)]
    wr = K.sbuf("wr", [128, KC, 16], BF16)
    K.dma(K.pool, wr[:], d["moe_router"][l].rearrange("(p c) e -> p c e", c=KC), W=[wr])
    E = K.sbuf("E", [16, NTOK], F32, partial=True)
    A = K.sbuf("A", [16, NTOK], F32, partial=True)
    junk = K.sbuf("junk", [16, NLAT], F32)
    tiles = TOK_TILES if need_ctx else TOK_TILES[1:]
    for ti, (tok0, ntok) in enumerate(tiles):
        hT = hTs[ti % 2]
        row = 1 if tok0 == 0 else 0
        for sb in range(ntok // 128):
            norm_block(C, K, nb, d["xm"][tok0 + sb * 128: tok0 + (sb + 1) * 128, :], gsh, row, hT, sb * 128)
        ps = acc[ti % 2]
        for kc in range(KC):
            mm(K, ps, ps[0:16, 0:ntok], wr[:, kc, :], hT[:, kc, 0:ntok], kc == 0, kc == KC - 1, R=[wr, hT])
        K.op(K.act, lambda e: e.activation(E[:, tok0:tok0 + ntok], ps[0:16, 0:ntok], AF.Exp), R=[ps], W=[E])
    for ti, (tok0, ntok) in enumerate(tiles):
        ps = acc[ti % 2]
        mm(K, ps, ps[0:16, 0:ntok], cm.ones_f[0:16, 0:16], E[:, tok0:tok0 + ntok], True, True, R=[cm.ones_f, E])
        K.op(K.dve, lambda e: e.reciprocal(A[:, tok0:tok0 + ntok], ps[0:16, 0:ntok]), R=[ps], W=[A])
        K.op(K.dve, lambda e: e.tensor_tensor(A[:, tok0:tok0 + ntok], A[:, tok0:tok0 + ntok], E[:, tok0:tok0 + ntok], ALU.mult), R=[A, E], W=[A])
    sets = ([(0, NCTX, 32)] if need_ctx else []) + [(NCTX, NLAT, 512)]
    G = K.sbuf("G", [16, NTOK], F32, partial=True)
    for si, (c0, n, cap) in enumerate(sets):
        st = K.sbuf(f"bis{si}", [16, 8], F32)
        lo, hi, mid, cnt, ge, nge, t1, t2 = [st[:, i:i + 1] for i in range(8)]
        K.op(K.dve, lambda e: e.memset(st[:], 0.0), W=[st])
        K.op(K.dve, lambda e: e.memset(hi, 2.0), R=[st], W=[st])
        Av = A[:, c0:c0 + n]
        for it in range(n_iter):
            K.op(K.dve, lambda e: e.tensor_scalar(mid, lo, hi, 0.5, ALU.add, ALU.mult), R=[st], W=[st])
            K.op(K.dve, lambda e: e.tensor_scalar(junk[:, 0:n], Av, mid, 0.0, ALU.is_ge, ALU.add, accum_out=cnt), R=[A, st], W=[junk, st])
            K.op(K.dve, lambda e: e.tensor_scalar(ge, cnt, float(cap) - 0.5, None, ALU.is_ge), R=[st], W=[st])
            K.op(K.dve, lambda e: e.tensor_scalar(nge, ge, -1.0, 1.0, ALU.mult, ALU.add), R=[st], W=[st])
            K.op(K.dve, lambda e: e.tensor_tensor(t1, mid, ge, ALU.mult), R=[st], W=[st])
            K.op(K.dve, lambda e: e.tensor_tensor(t2, mid, nge, ALU.mult), R=[st], W=[st])
            K.op(K.dve, lambda e: e.scalar_tensor_tensor(lo, lo, nge, t1, ALU.mult, ALU.add), R=[st], W=[st])
            K.op(K.dve, lambda e: e.scalar_tensor_tensor(hi, hi, ge, t2, ALU.mult, ALU.add), R=[st], W=[st])
        K.op(K.dve, lambda e: e.scalar_tensor_tensor(G[:, c0:c0 + n], Av, lo, Av, ALU.is_ge, ALU.mult), R=[A, st], W=[G])
    t0 = 0 if need_ctx else NCTX
    K.dma(K.sp, d["gm"][:, t0:NTOK], G[:, t0:NTOK], R=[G])
    K.end_phase()


def phase_moe(C, K, l, need_ctx):
    nc, d, cm = C.nc, C.d, C.cm
    K.begin_phase()
    nb = NormBufs(K)
    vs = K.sbuf("vs", [32, 5, 128], F32)
    tmpv = K.sbuf("tmpv", [128, 5, 32], F32)
    gsh = K.sbuf("gsh", [128, 4, 32], F32)
    pa = [K.psum(f"pa{i}", [128, 512], F32) for i in range(2)]
    pu = [K.psum(f"pu{i}", [128, 512], F32) for i in range(2)]
    py = [K.psum(f"py{i}", [128, 512], F32) for i in range(2)]
    build_gsh(C, K, l, 2, gsh, vs, pa[0], tmpv)
    hT = K.sbuf("hT", [128, KC, 512], BF16, partial=True)
    aT = K.sbuf("aT", [128, 32, 512], BF16, partial=True)
    wp = [K.sbuf(f"wp{i}", [128, 2, KC, 256], BF16, partial=True) for i in range(2)]
    g2 = K.sbuf("g2", [128, D], F32)
    gmb = [K.sbuf(f"gmb{i}", [128, 512], F32) for i in range(2)]
    sl = [K.sbuf(f"sl{i}", [128, 512], F32) for i in range(2)]
    tl = [K.sbuf(f"tl{i}", [128, 512], F32) for i in range(2)]
    xin = [K.sbuf(f"xin{i}", [128, 512], F32) for i in range(3)]
    yo = [K.sbuf(f"yo{i}", [128, 512], F32) for i in range(3)]
    w1, w3, w2 = d["moe_w1"][l], d["moe_w3"][l], d["moe_w2"][l]
    w2v = w2.rearrange("e (fb p) n -> p (e fb) n", p=128)
    tiles = TOK_TILES if need_ctx else TOK_TILES[1:]
    wi = 0
    ai = 0
    yi = 0
    for ti, (tok0, ntok) in enumerate(tiles):
        row = 1 if tok0 == 0 else 0
        if ti == 0 or (ti == 1 and need_ctx):
            K.dma(K.sp, g2[:], d["mod"][l][row:row + 1, 5 * D:6 * D].broadcast_to([128, D]), W=[g2])
        for sb in range(ntok // 128):
            norm_block(C, K, nb, d["xm"][tok0 + sb * 128: tok0 + (sb + 1) * 128, :], gsh, row, hT, sb * 128)
        for e in range(16):
            w = wp[wi % 2]
            wi += 1
            K.dma(K.pool, w[:, 0], w1[e].rearrange("(p c) f -> p c f", c=KC), W=[w])
            K.dma(K.pool, w[:, 1], w3[e].rearrange("(p c) f -> p c f", c=KC), W=[w])
            gb = gmb[e % 2]
            K.dma(K.sp, gb[:, 0:ntok], d["gm"][e:e + 1, tok0:tok0 + ntok].broadcast_to([128, ntok]), W=[gb])
            for fb in range(2):
                a, u = pa[ai % 2], pu[ai % 2]
                s_, t = sl[ai % 2], tl[ai % 2]
                ai += 1
                for kc in range(KC):
                    mm(K, a, a[:, 0:ntok], w[:, 0, kc, fb * 128:(fb + 1) * 128], hT[:, kc, 0:ntok], kc == 0, kc == KC - 1, R=[w, hT])
                for kc in range(KC):
                    mm(K, u, u[:, 0:ntok], w[:, 1, kc, fb * 128:(fb + 1) * 128], hT[:, kc, 0:ntok], kc == 0, kc == KC - 1, R=[w, hT])
                K.op(K.act, lambda e_: e_.activation(s_[:, 0:ntok], a[:, 0:ntok], AF.Silu), R=[a], W=[s_])
                K.op(K.dve, lambda e_: e_.tensor_tensor(t[:, 0:ntok], s_[:, 0:ntok], u[:, 0:ntok], ALU.mult), R=[s_, u], W=[t])
                K.op(K.dve, lambda e_: e_.tensor_tensor(aT[:, e * 2 + fb, 0:ntok], t[:, 0:ntok], gb[:, 0:ntok], ALU.mult), R=[t, gb], W=[aT])
        for g in range(8):
            cs = slice(g * 512, (g + 1) * 512)
            w = wp[wi % 2]
            wi += 1
            wv = w[:].rearrange("p a c f -> p (a c f)").rearrange("p (c n) -> p c n", n=512)
            K.dma(K.pool, wv, w2v[:, :, cs], W=[w])
            for sb in range(ntok // 128):
                ps = py[yi % 2]
                xt, y = xin[yi % 3], yo[yi % 3]
                yi += 1
                r0 = tok0 + sb * 128
                K.dma(K.sp, xt[:], d["xm"][r0:r0 + 128, cs], W=[xt])
                for c in range(32):
                    mm(K, ps, ps[:], aT[:, c, sb * 128:(sb + 1) * 128], wv[:, c, :], c == 0, c == 31, R=[aT, w])
                K.op(K.dve, lambda e_: e_.tensor_tensor(y[:], ps[:], g2[:, cs], ALU.mult), R=[ps, g2], W=[y])
                K.op(K.dve, lambda e_: e_.tensor_tensor(y[:], y[:], xt[:], ALU.add), R=[y, xt], W=[y])
                K.dma(K.sp, d["xl"][r0:r0 + 128, cs], y[:], R=[y])
    K.end_phase()


def phase_final(C, K):
    nc, d, cm = C.nc, C.d, C.cm
    K.begin_phase()
    fw = K.sbuf("fw", [128, D], F32)
    K.dma(K.sp, fw[:], d["final_norm_w"][0:1, :].broadcast_to([128, D]), W=[fw])
    xts = [K.sbuf(f"fx{i}", [128, D], F32) for i in range(2)]
    jk = K.sbuf("fj", [128, D], BF16)
    yts = [K.sbuf(f"fy{i}", [128, D], F32) for i in range(2)]
    sss = [K.sbuf(f"fs{i}", [128, 2], F32) for i in range(2)]
    for i in range(NLAT // 128):
        xt, yt, ss = xts[i % 2], yts[i % 2], sss[i % 2]
        K.dma(K.sp, xt[:], d["xl"][NCTX + i * 128:NCTX + (i + 1) * 128, :], W=[xt])
        K.op(K.act, lambda e: e.activation(jk[:], xt[:], AF.Square, accum_out=ss[:, 0:1]), R=[xt], W=[jk, ss])
        K.op(K.act, lambda e: e.activation(ss[:, 1:2], ss[:, 0:1], AF.Sqrt, scale=1.0 / D, bias=cm.eps[:, 0:1]), R=[ss, cm.eps], W=[ss])
        K.op(K.dve, lambda e: e.reciprocal(ss[:, 1:2], ss[:, 1:2]), R=[ss], W=[ss])
        K.op(K.dve, lambda e: e.scalar_tensor_tensor(yt[:], xt[:], ss[:, 1:2], fw[:], ALU.mult, ALU.mult), R=[xt, ss, fw], W=[yt])
        K.dma(K.sp, d["y"][i * 128:(i + 1) * 128, :], yt[:], R=[yt])
    K.end_phase()


def ev_tm_gates(dst):
    def f(S, g, sb, ps):
        K = S.K
        st = S.stgf[S.n % 3]
        S.n += 1
        K.op(K.dve, lambda e: e.tensor_tensor(st[:, 0:32], ps[:, 0:32], S.bg[:], ALU.add), R=[ps, S.bg], W=[st])
        r0 = S.tok0 + sb * 128
        K.dma(K.sp, dst[r0:r0 + 128, :], st[:, 0:32], R=[st])
    return f


def phase_inproj_mlstm(C, K, l):
    d = C.d
    o = l // 2
    w = d["ml_w_in"][o]
    groups = []
    for i in range(4):
        groups.append(dict(kind="FM", w=w[:, i * 512:(i + 1) * 512], ncols=512, b0=4 * i, evac=ev_fm_plain(d["mqT"], 256 ** -0.5)))
    for i in range(4):
        groups.append(dict(kind="FM", w=w[:, 2048 + i * 512:2048 + (i + 1) * 512], ncols=512, b0=4 * i, evac=ev_fm_plain(d["mkT"])))
    for i in range(4):
        groups.append(dict(kind="TM", w=w[:, 2048 + i * 512:2048 + (i + 1) * 512], ncols=512, c0=512 * i, evac=ev_tm_plain(d["mK"])))
    for i in range(8):
        groups.append(dict(kind="TM", w=w[:, 4096 + i * 512:4096 + (i + 1) * 512], ncols=512, c0=512 * i, evac=ev_tm_plain(d["mV"])))
    for i in range(8):
        groups.append(dict(kind="TM", w=w[:, 8192 + i * 512:8192 + (i + 1) * 512], ncols=512, c0=512 * i, evac=ev_tm_plain(d["mO"])))
    groups.append(dict(kind="TM", w=w[:, 12288:12320], ncols=32, c0=0, evac=ev_tm_gates(d["mG"])))

    def setup(S):
        S.bg = K.sbuf("bg", [128, 32], F32)
        K.dma(K.sp, S.bg[:], d["ml_b_gates"][o:o + 1, :].broadcast_to([128, 32]), W=[S.bg])
    phase_inproj(C, K, l, groups, setup)


def phase_mlstm_scan(C, K, l, dr):
    nc, d, cm = C.nc, C.d, C.cm
    K.begin_phase()
    tri = K.sbuf("tri", [64, 4, 64], F32)
    K.dma(K.sp, tri[:], d["c_tri"][:, :, :], W=[tri])
    Cst = [K.sbuf(f"Cst{h}", [128, 2, 512], F32) for h in range(8)]
    Cb = [K.sbuf(f"Cb{h}", [128, 2, 512], BF16) for h in range(8)]
    nst = [K.sbuf(f"nst{h}", [128, 2], F32) for h in range(8)]
    nbf = [K.sbuf(f"nbf{h}", [128, 2], BF16) for h in range(8)]
    for h in range(8):
        K.op(K.dve, lambda e, h=h: e.memset(Cst[h][:], 0.0), W=[Cst[h]])
        K.op(K.pool, lambda e, h=h: e.memset(Cb[h][:], 0.0), W=[Cb[h]])
        K.op(K.dve, lambda e, h=h: e.memset(nst[h][:], 0.0), W=[nst[h]])
        K.op(K.pool, lambda e, h=h: e.memset(nbf[h][:], 0.0), W=[nbf[h]])
    qT4 = [K.sbuf(f"qT4{i}", [128, 16, 256], BF16) for i in range(2)]
    kT4 = [K.sbuf(f"kT4{i}", [128, 16, 256], BF16) for i in range(2)]
    Kt = [K.sbuf(f"Kt{i}", [64, 2048], BF16) for i in range(2)]
    Vt = [K.sbuf(f"Vt{i}", [64, D], BF16) for i in range(2)]
    Gt = [K.sbuf(f"Gt{i}", [64, 32], F32) for i in range(2)]
    hch = [K.sbuf(f"hch{i}", [64, D], F32, partial=True) for i in range(2)]
    gcp = [K.sbuf(f"gcp{i}", [128, 16], F32) for i in range(2)]
    gs = [K.sbuf(f"gs{i}", [128, 6, 8], F32, partial=True) for i in range(2)]
    smalls = [K.psum(f"psm{i}", [128, 512], F32) for i in range(2)]
    pgt = [Buf(K, f"pgt{i}", smalls[i].t[:, 0:16]) for i in range(2)]
    pst = [Buf(K, f"pst{i}", smalls[i].t[0:64, 64:128]) for i in range(2)]
    pd = [Buf(K, f"pd{i}", smalls[i].t[0:64, 128:130]) for i in range(2)]
    pdn = [Buf(K, f"pdn{i}", smalls[i].t[:, 136:138]) for i in range(2)]
    pn = [K.psum(f"pn{i}", [64, 512], F32) for i in range(2)]
    pdc = [K.psum(f"pdc{i}", [128, 512], F32) for i in range(4)]
    Pt = [K.sbuf(f"Pt{i}", [64, 64], BF16) for i in range(2)]
    Kw = [K.sbuf(f"Kw{i}", [64, 256], BF16) for i in range(2)]
    dd = [K.sbuf(f"dd{i}", [64, 2], F32) for i in range(2)]
    ic, fc = (0, 8) if dr == 0 else (16, 24)
    order = list(range(68)) if dr == 0 else [3, 2, 1, 0] + list(range(67, 3, -1))
    dst = d["hf"] if dr == 0 else d["hb"]
    plan = []
    cur_grp = None
    gi = -1
    for ci, ck in enumerate(order):
        grp = ck // 4
        newg = grp != cur_grp
        if newg:
            cur_grp = grp
            gi += 1
        plan.append((ck, grp, newg, qT4[gi % 2], kT4[gi % 2]))

    def issue_loads(ci):
        ck, grp, newg, q4, k4 = plan[ci]
        if newg:
            K.dma(K.sp, q4[:], d["mqT"][:, :, grp * 256:(grp + 1) * 256].rearrange("b p t -> p b t"), W=[q4])
            K.dma(K.sp, k4[:], d["mkT"][:, :, grp * 256:(grp + 1) * 256].rearrange("b p t -> p b t"), W=[k4])
        r0 = ck * 64
        K.dma(K.sp, Kt[ci % 2][:], d["mK"][r0:r0 + 64, :], W=[Kt[ci % 2]])
        K.dma(K.sp, Vt[ci % 2][:], d["mV"][r0:r0 + 64, :], W=[Vt[ci % 2]])
        K.dma(K.sp, Gt[ci % 2][:], d["mG"][r0:r0 + 64, :], W=[Gt[ci % 2]])

    si = 0
    issue_loads(0)
    for ci, ck in enumerate(order):
        _, grp, _, q4, k4 = plan[ci]
        t0 = (ck % 4) * 64
        r0 = ck * 64
        kt, vt, gt, hc, g_ = Kt[ci % 2], Vt[ci % 2], Gt[ci % 2], hch[ci % 2], gs[ci % 2]
        pg = pgt[ci % 2]
        lq, vq, ev, ea, eb, eg = [g_[:, i, :] for i in range(6)]
        K.op(K.act, lambda e: e.activation(lq[0:64], gt[:, fc:fc + 8], AF.Exp, scale=-1.0), R=[gt], W=[g_])
        K.op(K.act, lambda e: e.activation(lq[0:64], lq[0:64], AF.Ln, bias=cm.ones_f[0:64, 0:1]), R=[g_, cm.ones_f], W=[g_])
        mm(K, pg, pg[0:64, 0:8], tri[:, dr, :], lq[0:64], True, True, R=[tri, g_])
        mm(K, pg, pg[:, 8:16], cm.ones_f[0:64, 0:128], lq[0:64], True, True, R=[cm.ones_f, g_])
        K.op(K.dve, lambda e: e.tensor_tensor(vq[0:64], gt[:, ic:ic + 8], pg[0:64, 0:8], ALU.add), R=[gt, pg], W=[g_])
        K.op(K.act, lambda e: e.activation(ev[0:64], vq[0:64], AF.Exp), R=[g_], W=[g_])
        K.op(K.dve, lambda e: e.tensor_tensor(ea[0:64], vq[0:64], pg[0:64, 8:16], ALU.subtract), R=[g_, pg], W=[g_])
        K.op(K.act, lambda e: e.activation(ea[0:64], ea[0:64], AF.Exp), R=[g_], W=[g_])
        K.op(K.act, lambda e: e.activation(eb[0:64], pg[0:64, 0:8], AF.Exp, scale=-1.0), R=[pg], W=[g_])
        K.op(K.act, lambda e: e.activation(eg, pg[:, 8:16], AF.Exp, scale=-1.0), R=[pg], W=[g_])
        if ci + 1 < len(order):
            issue_loads(ci + 1)
        for h in range(8):
            ps_s, ps_n, ps_d, ps_dn = pst[si % 2], pn[si % 2], pd[si % 2], pdn[si % 2]
            pc0, pc1 = pdc[(2 * si) % 4], pdc[(2 * si + 1) % 4]
            pt, kw, dq = Pt[si % 2], Kw[si % 2], dd[si % 2]
            si += 1
            qa = [q4[:, 2 * h + c, t0:t0 + 64] for c in range(2)]
            ka = [k4[:, 2 * h + c, t0:t0 + 64] for c in range(2)]
            vh = vt[:, h * 512:(h + 1) * 512]
            for c in range(2):
                mm(K, ps_s, ps_s[:, :], ka[c], qa[c], c == 0, c == 1, R=[k4, q4])
            K.op(K.dve, lambda e: e.scalar_tensor_tensor(pt[:], ps_s[:, :], ev[0:64, h:h + 1], tri[:, dr, :], ALU.mult, ALU.mult), R=[ps_s, g_, tri], W=[pt])
            for c in range(2):
                mm(K, ps_n, ps_n[:, :], qa[c], Cb[h][:, c, :], c == 0, False, R=[q4, Cb[h]])
            mm(K, ps_n, ps_n[:, :], pt[:], vh, False, True, R=[pt, vt])
            for c in range(2):
                mm(K, ps_d, ps_d[:, 0:1], qa[c], nbf[h][:, c:c + 1], c == 0, False, R=[q4, nbf[h]])
            mm(K, ps_d, ps_d[:, 0:1], pt[:], cm.ones_b[0:64, 0:1], False, True, R=[pt, cm.ones_b])
            K.op(K.dve, lambda e: e.tensor_scalar(dq[:, 0:1], ps_d[:, 0:1], eb[0:64, h:h + 1], None, ALU.mult), R=[ps_d, g_], W=[dq])
            K.op(K.dve, lambda e: e.tensor_scalar(dq[:, 1:2], ps_d[:, 0:1], eb[0:64, h:h + 1], -1.0, ALU.mult, ALU.mult), R=[ps_d, g_], W=[dq])
            K.op(K.dve, lambda e: e.scalar_tensor_tensor(dq[:, 0:1], dq[:, 0:1], 1.0, dq[:, 1:2], ALU.max, ALU.max), R=[dq], W=[dq])
            K.op(K.dve, lambda e: e.reciprocal(dq[:, 0:1], dq[:, 0:1]), R=[dq], W=[dq])
            K.op(K.dve, lambda e: e.tensor_tensor(dq[:, 1:2], dq[:, 0:1], eb[0:64, h:h + 1], ALU.mult), R=[dq, g_], W=[dq])
            K.op(K.act, lambda e: e.activation(hc[:, h * 512:(h + 1) * 512], ps_n[:, :], AF.Copy, scale=dq[:, 1:2]), R=[ps_n, dq], W=[hc])
            K.op(K.pool, lambda e: e.tensor_scalar(kw[:], kt[:, h * 256:(h + 1) * 256], ea[0:64, h:h + 1], None, ALU.mult), R=[kt, g_], W=[kw])
            mm(K, pc0, pc0[:, :], kw[:, 0:128], vh, True, True, R=[kw, vt])
            mm(K, pc1, pc1[:, :], kw[:, 128:256], vh, True, True, R=[kw, vt])
            for c in range(2):
                mm(K, ps_dn, ps_dn[:, c:c + 1], kw[:, c * 128:(c + 1) * 128], cm.ones_b[0:64, 0:1], True, True, R=[kw, cm.ones_b])
            K.op(K.dve, lambda e: e.scalar_tensor_tensor(Cst[h][:, 0, :], Cst[h][:, 0, :], eg[:, h:h + 1], pc0[:, :], ALU.mult, ALU.add), R=[Cst[h], g_, pc0], W=[Cst[h]])
            K.op(K.dve, lambda e: e.scalar_tensor_tensor(Cst[h][:, 1, :], Cst[h][:, 1, :], eg[:, h:h + 1], pc1[:, :], ALU.mult, ALU.add), R=[Cst[h], g_, pc1], W=[Cst[h]])
            K.op(K.act, lambda e: e.copy(Cb[h][:], Cst[h][:]), R=[Cst[h]], W=[Cb[h]])
            K.op(K.dve, lambda e: e.scalar_tensor_tensor(nst[h][:], nst[h][:], eg[:, h:h + 1], ps_dn[:, :], ALU.mult, ALU.add), R=[nst[h], g_, ps_dn], W=[nst[h]])
            K.op(K.pool, lambda e: e.tensor_copy(nbf[h][:], nst[h][:]), R=[nst[h]], W=[nbf[h]])
        K.dma(K.sp, dst[r0:r0 + 64, :], hc[:], R=[hc])
    K.end_phase()


def phase_mlstm_post(C, K, l, need_ctx):
    nc, d, cm = C.nc, C.d, C.cm
    o = l // 2
    K.begin_phase()
    nwb = K.sbuf("nwb", [128, D], F32)
    K.dma(K.sp, nwb[:], d["ml_norm_w"][o:o + 1, :].broadcast_to([128, D]), W=[nwb])
    hf = [K.sbuf(f"hf{i}", [128, D], F32) for i in range(2)]
    hb = [K.sbuf(f"hb{i}", [128, D], F32) for i in range(2)]
    ot = [K.sbuf(f"ot{i}", [128, D], BF16) for i in range(2)]
    sg = K.sbuf("sg", [128, D], F32)
    jk = K.sbuf("jk", [128, 512], BF16)
    hnb = K.sbuf("hnb", [128, D], BF16)
    ss = K.sbuf("ss", [128, 2, 8], F32, partial=True)
    stg = [K.sbuf(f"stg{i}", [128, KC, 512], BF16, partial=True) for i in range(2)]
    tp = [K.psum(f"tp{i}", [128, 1024], BF16) for i in range(2)]
    tiles = TOK_TILES if need_ctx else TOK_TILES[1:]
    bi = 0
    ti_ = 0
    for ti, (tok0, ntok) in enumerate(tiles):
        st = stg[ti % 2]
        for sb in range(ntok // 128):
            r0 = tok0 + sb * 128
            a, b, og = hf[bi % 2], hb[bi % 2], ot[bi % 2]
            bi += 1
            K.dma(K.sp, a[:], d["hf"][r0:r0 + 128, :], W=[a])
            K.dma(K.sp, b[:], d["hb"][r0:r0 + 128, :], W=[b])
            K.dma(K.sp, og[:], d["mO"][r0:r0 + 128, :], W=[og])
            K.op(K.pool, lambda e: e.tensor_tensor(a[:], a[:], b[:], ALU.add), R=[a, b], W=[a])
            for h in range(8):
                K.op(K.act, lambda e, h=h: e.activation(jk[:], a[:, h * 512:(h + 1) * 512], AF.Square, accum_out=ss[:, 0, h:h + 1]), R=[a], W=[jk, ss])
            K.op(K.act, lambda e: e.activation(ss[:, 1, :], ss[:, 0, :], AF.Sqrt, scale=1.0 / 512, bias=cm.eps[:, 0:1]), R=[ss, cm.eps], W=[ss])
            K.op(K.dve, lambda e: e.reciprocal(ss[:, 1, :], ss[:, 1, :]), R=[ss], W=[ss])
            K.op(K.act, lambda e: e.activation(sg[:], og[:], AF.Sigmoid), R=[og], W=[sg])
            for h in range(8):
                hs = slice(h * 512, (h + 1) * 512)
                K.op(K.dve, lambda e, h=h, hs=hs: e.scalar_tensor_tensor(a[:, hs], a[:, hs], ss[:, 1, h:h + 1], nwb[:, hs], ALU.mult, ALU.mult), R=[a, ss, nwb], W=[a])
            K.op(K.pool, lambda e: e.tensor_tensor(hnb[:], a[:], sg[:], ALU.mult), R=[a, sg], W=[hnb])
            for g in range(4):
                t = tp[ti_ % 2]
                ti_ += 1
                for c in range(8):
                    ch = g * 8 + c
                    K.op(K.pe, lambda e, c=c, ch=ch, t=t: e.transpose(t[:, c * 128:(c + 1) * 128], hnb[:, ch * 128:(ch + 1) * 128], cm.ident_b[:]), R=[hnb, cm.ident_b], W=[t])
                o_ap = st[:, g * 8:(g + 1) * 8, sb * 128:(sb + 1) * 128]
                i_ap = t[:, :].rearrange("p (c q) -> p c q", c=8)
                if g % 2 == 0:
                    K.op(K.act, lambda e, o_ap=o_ap, i_ap=i_ap: e.copy(o_ap, i_ap), R=[t], W=[st])
                else:
                    K.op(K.dve, lambda e, o_ap=o_ap, i_ap=i_ap: e.tensor_copy(o_ap, i_ap), R=[t], W=[st])
        K.dma(K.sp, d["OT"][:, :, tok0:tok0 + ntok].rearrange("k p t -> p k t"), st[:, :, 0:ntok], R=[st])
    K.end_phase()


def host_tri():
    s = np.arange(64)[:, None]
    t = np.arange(64)[None, :]
    tri = np.zeros((64, 4, 64), np.float32)
    tri[:, 0, :] = (s <= t)
    tri[:, 1, :] = (s >= t)
    return tri


def build_full():
    C = Prog()
    C.declare()
    nc = C.nc
    k = K(nc)
    d = C.d
    with nc.Block():
        setup_common(C, k)
        phase_mods(C, k, [0, 1, 2, 3])
        for l in range(DEPTH):
            need_ctx = l < DEPTH - 1
            if l % 2 == 0:
                phase_inproj_attn(C, k, l)
                phase_na(C, k, l, need_ctx)
                phase_win(C, k, l, need_ctx)
                phase_outproj(C, k, l, d["ab_w_out"][l // 2], need_ctx)
            else:
                phase_inproj_mlstm(C, k, l)
                phase_mlstm_scan(C, k, l, 0)
                phase_mlstm_scan(C, k, l, 1)
                phase_mlstm_post(C, k, l, need_ctx)
                phase_outproj(C, k, l, d["ml_w_out"][l // 2], need_ctx)
            phase_router(C, k, l, need_ctx)
            phase_moe(C, k, l, need_ctx)
        phase_final(C, k)
    return C, k


def kernel(x, c, ctx, c_ctx, ada_w, ada_b, norm1_w, norm2_w, ab_w_in, ab_w_out, na_rpb, win_sink,
           ml_w_in, ml_b_gates, ml_norm_w, ml_w_out, moe_router, moe_w1, moe_w3, moe_w2, final_norm_w):
    f = lambda a: np.ascontiguousarray(np.asarray(a, dtype=np.float32))
    C, k = build_full()
    hc = host_consts()
    shared = {
        "ada_w": f(ada_w), "ada_b": f(ada_b), "norm1_w": f(norm1_w), "norm2_w": f(norm2_w),
        "ab_w_in": f(ab_w_in), "ab_w_out": f(ab_w_out), "win_sink": f(win_sink),
        "ml_w_in": f(ml_w_in), "ml_b_gates": f(ml_b_gates), "ml_norm_w": f(ml_norm_w), "ml_w_out": f(ml_w_out),
        "moe_router": f(moe_router), "moe_w1": f(moe_w1), "moe_w3": f(moe_w3), "moe_w2": f(moe_w2),
        "final_norm_w": f(final_norm_w).reshape(1, D),
        "c_ident": hc["c_ident"], "c_perm": hc["c_perm"], "c_cos": hc["c_cos"], "c_sin": hc["c_sin"],
        "c_bm": host_bm(f(na_rpb)), "c_wmask": host_wmask(), "c_tri": host_tri(),
    }
    x, c, ctx, c_ctx = f(x), f(c), f(ctx), f(c_ctx)
    B = x.shape[0]
    in_maps = []
    for s in range(B):
        m = dict(shared)
        m["x"] = x[s]
        m["ctx"] = ctx[s]
        m["cvec"] = np.stack([c[s], c_ctx]).astype(np.float32)
        in_maps.append(m)
    res = run_bass_kernel_spmd(C.nc, in_maps, core_ids=list(range(B)))
    return np.stack([res.results[s]["y"] for s in range(B)]).astype(np.float32)
```

```python
import numpy as np
import ml_dtypes
from contextlib import ExitStack
import concourse.bass as bass
import concourse.mybir as mybir
from concourse.bass_utils import run_bass_kernel_spmd

F32 = mybir.dt.float32
BF16 = mybir.dt.bfloat16
AF = mybir.ActivationFunctionType
ALU = mybir.AluOpType
AX = mybir.AxisListType

D = 4096
NTOK = 4352
NCTX = 256
NLAT = 4096
DEPTH = 4
KC = 32
EPS = 1e-6
NEG = -30000.0


class Eng:
    def __init__(self, k, name, e, sem):
        self.k, self.name, self.e, self.sem = k, name, e, sem
        self.seq = 0
        self.insts = {}
        self.tick = []
        self.count = 0
        self.known = {}

    def ticket(self, seq):
        for s, c in reversed(self.tick[-64:]):
            if s < seq:
                break
        lo = None
        for s, c in reversed(self.tick):
            if s >= seq:
                lo = c
            else:
                break
        if lo is not None:
            return lo
        if seq not in self.insts:
            seq = max(self.insts)
        ins = self.insts[seq]
        self.count += 1
        ins.then_inc(self.sem, 1)
        self.tick.append((seq, self.count))
        if len(self.tick) > 256:
            self.tick = self.tick[-128:]
        for s in [s for s in self.insts if s <= seq]:
            del self.insts[s]
        return self.count


class DmaSem:
    def __init__(self, sem):
        self.sem = sem
        self.issued = 0


class Buf:
    def __init__(self, k, name, t, partial=False):
        self.k, self.name, self.t, self.partial = k, name, t, partial
        self.w = {}
        self.r = {}
        self.dsem = None

    def __getitem__(self, idx):
        return self.t[idx]


class K:
    def __init__(self, nc):
        self.nc = nc
        self.stack = ExitStack()
        self.pe = Eng(self, "pe", nc.tensor, self._sem("s_pe"))
        self.act = Eng(self, "act", nc.scalar, self._sem("s_act"))
        self.dve = Eng(self, "dve", nc.vector, self._sem("s_dve"))
        self.pool = Eng(self, "pool", nc.gpsimd, self._sem("s_pool"))
        self.sp = Eng(self, "sp", nc.sync, self._sem("s_sp"))
        self.engs = [self.pe, self.act, self.dve, self.pool, self.sp]
        self.dsem_free = [DmaSem(self._sem(f"s_dma{i}")) for i in range(64)]
        self.bar = self._sem("s_bar")
        self.bar_n = 0
        self.phase_stack = None
        self.phase_bufs = []
        self.n_inst = 0

    def _sem(self, name):
        return self.stack.enter_context(self.nc.semaphore(name))

    def begin_phase(self):
        self.phase_stack = ExitStack()
        self.phase_bufs = []

    def sbuf(self, name, shape, dt, partial=False):
        self.uid = getattr(self, "uid", 0) + 1
        t = self.phase_stack.enter_context(self.nc.sbuf_tensor(f"{name}_u{self.uid}", list(shape), dt))
        b = Buf(self, name, t, partial)
        self.phase_bufs.append(b)
        return b

    def psum(self, name, shape, dt, partial=False):
        self.uid = getattr(self, "uid", 0) + 1
        t = self.phase_stack.enter_context(self.nc.psum_tensor(f"{name}_u{self.uid}", list(shape), dt))
        b = Buf(self, name, t, partial)
        self.phase_bufs.append(b)
        return b

    def end_phase(self):
        for b in self.phase_bufs:
            if b.dsem is not None:
                self._wait(self.sp, b.dsem.sem, 16 * b.dsem.issued)
        self.barrier()
        for b in self.phase_bufs:
            if b.dsem is not None:
                self.dsem_free.append(b.dsem)
                b.dsem = None
        self.phase_stack.close()
        self.phase_stack = None
        self.phase_bufs = []

    def barrier(self):
        for e in self.engs:
            if e.seq > 0 and e is not self.sp:
                last = e.seq
                if last in e.insts or any(s >= last for s, _ in e.tick):
                    t = e.ticket(last)
                    self._wait(e, e.sem, t)
        self.bar_n += 1
        for e in self.engs:
            e.e.sem_inc(self.bar, 1)
        for e in self.engs:
            e.e.wait_ge(self.bar, len(self.engs) * self.bar_n)

    def _wait(self, eng, sem, val):
        key = id(sem)
        if eng.known.get(key, 0) >= val:
            return
        eng.known[key] = val
        eng.e.wait_ge(sem, val)

    def _wait_dep(self, eng, key, val, same_engine_ok):
        if isinstance(key, Eng):
            if key is eng and not same_engine_ok:
                return
            if key is eng and key is self.pe:
                return
            t = key.ticket(val)
            self._wait(eng, key.sem, t)
        else:
            b = key[1]
            if b.dsem is None:
                return
            self._wait(eng, b.dsem.sem, 16 * b.dsem.issued)

    def _deps(self, eng, R, W):
        for b in R:
            for key, val in list(b.w.items()):
                self._wait_dep(eng, key, val, same_engine_ok=True)
        for b in W:
            if not b.partial:
                for key, val in list(b.w.items()):
                    self._wait_dep(eng, key, val, same_engine_ok=False)
            for key, val in list(b.r.items()):
                self._wait_dep(eng, key, val, same_engine_ok=False)

    def _record(self, key, val, R, W):
        for b in W:
            if b.partial:
                b.w[key] = val
            else:
                b.w = {key: val}
                b.r = {}
        for b in R:
            b.r[key] = val

    def op(self, eng, fn, R=(), W=()):
        self._deps(eng, R, W)
        ins = fn(eng.e)
        eng.seq += 1
        eng.insts[eng.seq] = ins
        if len(eng.insts) > 4096:
            for s in sorted(eng.insts)[:2048]:
                del eng.insts[s]
        self._record(eng, eng.seq, R, W)
        self.n_inst += 1
        return ins

    def dma(self, eng, out, in_, R=(), W=(), **kw):
        self._deps(eng, R, W)
        sb = (list(W) + list(R))[0]
        if sb.dsem is None:
            sb.dsem = self.dsem_free.pop()
        ins = eng.e.dma_start(out=out, in_=in_, **kw)
        ins.then_inc(sb.dsem.sem, 16)
        sb.dsem.issued += 1
        self._record(("dma", sb), True, R, W)
        self.n_inst += 1
        return ins


TOK_TILES = [(0, 256)] + [(256 + 512 * i, 512) for i in range(8)]


class Prog:
    def __init__(self, kinds=None, layers=(0, 1, 2, 3), only=None, shapes=None):
        self.nc = nc = bass.Bass("TRN2", target_bir_lowering=False)
        self.kinds = kinds or {}
        self.layers = layers
        self.only = only
        self.shapes = shapes or {}
        self.d = {}

    def dram(self, name, shape, dt, kind="Internal"):
        kind = self.kinds.get(name, kind)
        if kind == "ExternalInput" and self.only is not None and name not in self.only:
            return None
        shape = self.shapes.get(name, shape)
        self.d[name] = self.nc.dram_tensor(name, list(shape), dt, kind=kind).ap()
        return self.d[name]

    def declare(self):
        I = "ExternalInput"
        d = self.dram
        d("x", [NLAT, D], F32, I); d("ctx", [NCTX, D], F32, I); d("cvec", [2, D], F32, I)
        d("ada_w", [DEPTH, D, 6 * D], F32, I); d("ada_b", [DEPTH, 6 * D], F32, I)
        d("norm1_w", [DEPTH, D], F32, I); d("norm2_w", [DEPTH, D], F32, I)
        d("ab_w_in", [2, D, 9216], F32, I); d("ab_w_out", [2, D, D], F32, I)
        d("win_sink", [2, 16], F32, I)
        d("ml_w_in", [2, D, 12320], F32, I); d("ml_b_gates", [2, 32], F32, I)
        d("ml_norm_w", [2, D], F32, I); d("ml_w_out", [2, D, D], F32, I)
        d("moe_router", [DEPTH, D, 16], F32, I)
        d("moe_w1", [DEPTH, 16, D, 256], F32, I); d("moe_w3", [DEPTH, 16, D, 256], F32, I)
        d("moe_w2", [DEPTH, 16, 256, D], F32, I)
        d("final_norm_w", [1, D], F32, I)
        d("c_ident", [128, 128], F32, I)
        d("c_bm", [2, 16, 128, 21, 128], F32, I)
        d("c_cos", [128, NLAT], F32, I); d("c_sin", [128, NLAT], F32, I); d("c_perm", [128, 128], F32, I)
        d("c_wmask", [128, 2, 128], F32, I)
        d("c_tri", [64, 4, 64], F32, I)
        d("y", [NLAT, D], F32, "ExternalOutput")
        d("xl", [NTOK, D], F32); d("xm", [NTOK, D], F32); d("mod", [DEPTH, 2, 6 * D], F32)
        d("QaT", [16, 128, NTOK], BF16); d("KaT", [16, 128, NTOK], BF16); d("Va", [NTOK, 2048], BF16)
        d("QbT", [16, 128, NTOK], BF16); d("KbT", [4, 128, NTOK], BF16); d("Vb", [NTOK, 512], BF16)
        d("OT", [32, 128, NTOK], BF16)
        d("gm", [16, NTOK], F32)
        d("mqT", [16, 128, NTOK], BF16); d("mkT", [16, 128, NTOK], BF16)
        d("mK", [NTOK, 2048], BF16); d("mV", [NTOK, D], BF16); d("mO", [NTOK, D], BF16)
        d("mG", [NTOK, 32], F32); d("hf", [NTOK, D], F32); d("hb", [NTOK, D], F32)


def mm(K, ps_buf, out_ap, lhsT, rhs, start, stop, R):
    return K.op(K.pe, lambda e: e.matmul(out_ap, lhsT, rhs, start=start, stop=stop), R=R, W=[ps_buf])


class Common:
    pass


def setup_common(C, K, copy_inputs=True):
    nc = C.nc
    st = K.stack
    cm = Common()
    def gbuf(name, shape, dt):
        t = st.enter_context(nc.sbuf_tensor(name, list(shape), dt))
        return Buf(K, name, t)
    cm.ident_f = gbuf("ident_f", [128, 128], F32)
    cm.ident_b = gbuf("ident_b", [128, 128], BF16)
    cm.ones_b = gbuf("ones_b", [128, 128], BF16)
    cm.ones_f = gbuf("ones_f", [128, 128], F32)
    K.dma(K.sp, cm.ident_f[:], C.d["c_ident"][:, :], W=[cm.ident_f])
    K.dma(K.pool, cm.ident_b[:], C.d["c_ident"][:, :], W=[cm.ident_b])
    K.op(K.dve, lambda e: e.memset(cm.ones_b[:], 1.0), W=[cm.ones_b])
    K.op(K.dve, lambda e: e.memset(cm.ones_f[:], 1.0), W=[cm.ones_f])
    cm.eps = gbuf("eps_t", [128, 1], F32)
    K.op(K.dve, lambda e: e.memset(cm.eps[:], EPS), W=[cm.eps])
    C.cm = cm
    if not copy_inputs:
        return
    tmp = gbuf("cp_sem_holder", [1, 2], F32)
    K.dma(K.sp, C.d["xl"][0:NCTX, :], C.d["ctx"][:, :], W=[tmp])
    K.dma(K.sp, C.d["xl"][NCTX:NTOK, :], C.d["x"][:, :], W=[tmp])
    K._wait(K.sp, tmp.dsem.sem, 16 * tmp.dsem.issued)
    K.barrier()


def load_vecs_pp(C, K, rows, out_buf, scratch_v, ps_buf):
    cm = C.cm
    n = len(rows)
    for j, r in enumerate(rows):
        K.dma(K.sp, scratch_v[0:32, j, :], r.rearrange("(c p) -> c p", p=128), W=[scratch_v])
    for j in range(n):
        K.op(K.pe, lambda e, j=j: e.transpose(ps_buf[:, j * 32:(j + 1) * 32], scratch_v[0:32, j, :], cm.ident_f[0:32, 0:32]),
             R=[scratch_v, cm.ident_f], W=[ps_buf])
    K.op(K.dve, lambda e: e.tensor_copy(out_buf[:, 0:n, :].rearrange("p j c -> p (j c)"), ps_buf[:, 0:n * 32]), R=[ps_buf], W=[out_buf])


def phase_mods(C, K, layers):
    nc, d, cm = C.nc, C.d, C.cm
    K.begin_phase()
    vs = K.sbuf("vs", [32, 2, 128], F32)
    ps = K.psum("ps_m", [128, 512], F32)
    cpp = K.sbuf("cpp", [128, 2, 32], F32)
    scT = K.sbuf("scT", [128, 32, 2], BF16)
    load_vecs_pp(C, K, [d["cvec"][0], d["cvec"][1]], cpp, vs, ps)
    K.op(K.act, lambda e: e.activation(scT[:].rearrange("p c r -> p r c"), cpp[:], AF.Silu), R=[cpp], W=[scT])
    wbs = [K.sbuf(f"wb{i}", [128, KC, 512], BF16) for i in range(2)]
    bbs = [K.sbuf(f"bb{i}", [2, 512], F32) for i in range(2)]
    obs = [K.sbuf(f"ob{i}", [2, 512], F32) for i in range(2)]
    pss = [K.psum(f"ps_mod{i}", [128, 512], F32) for i in range(2)]
    it = 0
    for l in layers:
        for g in range(48):
            wb, bb, ob, pq = wbs[it % 2], bbs[it % 2], obs[it % 2], pss[it % 2]
            it += 1
            cs = slice(g * 512, (g + 1) * 512)
            K.dma(K.pool, wb[:], d["ada_w"][l][:, cs].rearrange("(c p) n -> p c n", p=128), W=[wb])
            K.dma(K.sp, bb[:], d["ada_b"][l:l + 1, cs].partition_broadcast(2) if False else d["ada_b"][l:l + 1, cs].broadcast_to([2, 512]), W=[bb])
            for kc in range(KC):
                mm(K, pq, pq[0:2, :], scT[:, kc, :], wb[:, kc, :], kc == 0, kc == KC - 1, R=[scT, wb])
            K.op(K.dve, lambda e, ob=ob, pq=pq, bb=bb: e.tensor_tensor(ob[:], pq[0:2, :], bb[:], ALU.add), R=[pq, bb], W=[ob])
            K.dma(K.sp, d["mod"][l][:, cs], ob[:], R=[ob])
    K.end_phase()


class NormBufs:
    def __init__(self, K, tag=""):
        self.xt = K.sbuf("nb_xt" + tag, [128, D], F32)
        self.xs = K.sbuf("nb_xs" + tag, [128, D], BF16)
        self.ss = K.sbuf("nb_ss" + tag, [128, 2], F32)
        self.tp = [K.psum(f"nb_tp{i}" + tag, [128, 1024], BF16) for i in range(2)]
        self.n = 0


def norm_block(C, K, nb, src_rows, gsh, row, hT, col0):
    cm = C.cm
    xt, xs, ss = nb.xt, nb.xs, nb.ss
    K.dma(K.sp, xt[:], src_rows, W=[xt])
    K.op(K.act, lambda e: e.activation(xs[:], xt[:], AF.Square, accum_out=ss[:, 0:1]), R=[xt], W=[xs, ss])
    K.op(K.act, lambda e: e.activation(ss[:, 1:2], ss[:, 0:1], AF.Sqrt, scale=1.0 / D, bias=cm.eps[:, 0:1]), R=[ss, cm.eps], W=[ss])
    K.op(K.dve, lambda e: e.reciprocal(ss[:, 1:2], ss[:, 1:2]), R=[ss], W=[ss])
    K.op(K.act, lambda e: e.activation(xs[:].rearrange("t (c p) -> t c p", p=128), xt[:].rearrange("t (p c) -> t c p", c=KC), AF.Copy, scale=ss[:, 1:2]), R=[xt, ss], W=[xs])
    for g in range(4):
        tp = nb.tp[nb.n % 2]
        nb.n += 1
        for c in range(8):
            ch = g * 8 + c
            K.op(K.pe, lambda e, c=c, ch=ch, tp=tp: e.transpose(tp[:, c * 128:(c + 1) * 128], xs[:, ch * 128:(ch + 1) * 128], cm.ident_b[:]),
                 R=[xs, cm.ident_b], W=[tp])
        for c in range(8):
            ch = g * 8 + c
            o = hT[:, ch, col0:col0 + 128]
            i = tp[:, c * 128:(c + 1) * 128]
            gs = gsh[:, 2 * row, ch:ch + 1]
            sh = gsh[:, 2 * row + 1, ch:ch + 1]
            if g % 2 == 0:
                K.op(K.act, lambda e, o=o, i=i, gs=gs, sh=sh: e.activation(o, i, AF.Identity, scale=gs, bias=sh), R=[tp, gsh], W=[hT])
            else:
                K.op(K.dve, lambda e, o=o, i=i, gs=gs, sh=sh: e.tensor_scalar(o, i, gs, sh, ALU.mult, ALU.add), R=[tp, gsh], W=[hT])


def build_gsh(C, K, l, which, gsh, vs, ps, tmp):
    d = C.d
    nw = d["norm1_w"] if which == 1 else d["norm2_w"]
    o = 0 if which == 1 else 3
    m = d["mod"][l]
    rows = [nw[l], m[0, (o + 1) * D:(o + 2) * D], m[0, o * D:(o + 1) * D], m[1, (o + 1) * D:(o + 2) * D], m[1, o * D:(o + 1) * D]]
    for j, rw in enumerate(rows):
        K.dma(K.sp, tmp[:, j, :], rw.rearrange("(p c) -> p c", c=KC), W=[tmp])
    for r in range(2):
        K.op(K.dve, lambda e, r=r: e.scalar_tensor_tensor(gsh[:, 2 * r, :], tmp[:, 1 + 2 * r, :], 1.0, tmp[:, 0, :], ALU.add, ALU.mult),
             R=[tmp], W=[gsh])
        K.op(K.dve, lambda e, r=r: e.tensor_copy(gsh[:, 2 * r + 1, :], tmp[:, 2 + 2 * r, :]), R=[tmp], W=[gsh])


def phase_inproj(C, K, l, groups, extra_setup=None):
    nc, d, cm = C.nc, C.d, C.cm
    K.begin_phase()
    S = type("S", (), {})()
    nb = NormBufs(K)
    vs = K.sbuf("vs", [32, 5, 128], F32)
    tmpv = K.sbuf("tmpv", [128, 5, 32], F32)
    gsh = K.sbuf("gsh", [128, 4, 32], F32)
    acc = [K.psum(f"acc{i}", [128, 512], F32) for i in range(4)]
    S.rp = [K.psum(f"rp{i}", [128, 512], F32) for i in range(2)]
    build_gsh(C, K, l, 1, gsh, vs, acc[0], tmpv)
    hTs = [K.sbuf(f"hT{i}", [128, KC, 512], BF16, partial=True) for i in range(2)]
    wbs = [K.sbuf(f"wb{i}", [128, KC, 512], BF16) for i in range(2)]
    S.stg = [K.sbuf(f"stg{i}", [128, 512], BF16) for i in range(4)]
    S.stgf = [K.sbuf(f"stgf{i}", [128, 512], F32) for i in range(3)]
    S.n = 0
    S.K, S.C = K, C
    if extra_setup:
        extra_setup(S)
    wi = 0
    ai = 0
    for ti, (tok0, ntok) in enumerate(TOK_TILES):
        hT = hTs[ti % 2]
        row = 1 if ti == 0 else 0
        nsub = ntok // 128
        for sb in range(nsub):
            norm_block(C, K, nb, d["xl"][tok0 + sb * 128: tok0 + (sb + 1) * 128, :], gsh, row, hT, sb * 128)
        S.tok0, S.ntok, S.is_ctx = tok0, ntok, ti == 0
        if hasattr(S, "tile_setup"):
            S.tile_setup(S)
        for g in groups:
            wb = wbs[wi % 2]
            wi += 1
            ncols = g["ncols"]
            K.dma(K.pool, wb[:, :, 0:ncols], g["w"].rearrange("(p c) n -> p c n", c=KC), W=[wb])
            if g["kind"] == "FM":
                for b in range(ncols // 128):
                    ps = acc[ai % 4]
                    ai += 1
                    for kc in range(KC):
                        mm(K, ps, ps[:, 0:ntok], wb[:, kc, b * 128:(b + 1) * 128], hT[:, kc, 0:ntok], kc == 0, kc == KC - 1, R=[wb, hT])
                    g["evac"](S, g, b, ps)
            else:
                for sb in range(nsub):
                    ps = acc[ai % 4]
                    ai += 1
                    for kc in range(KC):
                        mm(K, ps, ps[:, 0:ncols], hT[:, kc, sb * 128:(sb + 1) * 128], wb[:, kc, 0:ncols], kc == 0, kc == KC - 1, R=[wb, hT])
                    g["evac"](S, g, sb, ps)
    K.end_phase()


def ev_fm_plain(dst, scale=None):
    def f(S, g, b, ps):
        K = S.K
        st = S.stg[S.n % 4]
        S.n += 1
        n = S.ntok
        if scale is None:
            K.op(K.act, lambda e: e.copy(st[:, 0:n], ps[:, 0:n]), R=[ps], W=[st])
        else:
            K.op(K.act, lambda e: e.activation(st[:, 0:n], ps[:, 0:n], AF.Copy, scale=scale), R=[ps], W=[st])
        K.dma(K.sp, dst[g["b0"] + b, :, S.tok0:S.tok0 + n], st[:, 0:n], R=[st])
    return f


def ev_tm_plain(dst, dt=BF16):
    def f(S, g, sb, ps):
        K = S.K
        nco = g["ncols"]
        if dt == BF16:
            st = S.stg[S.n % 4]
        else:
            st = S.stgf[S.n % 3]
        S.n += 1
        if S.n % 2 == 0:
            K.op(K.act, lambda e: e.copy(st[:, 0:nco], ps[:, 0:nco]), R=[ps], W=[st])
        else:
            K.op(K.dve, lambda e: e.tensor_copy(st[:, 0:nco], ps[:, 0:nco]), R=[ps], W=[st])
        r0 = S.tok0 + sb * 128
        K.dma(K.sp, dst[r0:r0 + 128, g["c0"]:g["c0"] + nco], st[:, 0:nco], R=[st])
    return f


def ev_fm_rope(dst, scale):
    plain = ev_fm_plain(dst, scale)
    def f(S, g, b, ps):
        K, C = S.K, S.C
        if S.is_ctx:
            return plain(S, g, b, ps)
        n = S.ntok
        xs = S.stgf[S.n % 3]
        t1 = S.stgf[(S.n + 1) % 3]
        t2 = S.stgf[(S.n + 2) % 3]
        st = S.stg[S.n % 4]
        rp = S.rp[S.n % 2]
        S.n += 1
        K.op(K.act, lambda e: e.activation(xs[:, 0:n], ps[:, 0:n], AF.Copy, scale=(1.0 if scale is None else scale)), R=[ps], W=[xs])
        mm(K, rp, rp[:, 0:n], S.perm[:], xs[:, 0:n], True, True, R=[S.perm, xs])
        K.op(K.dve, lambda e: e.tensor_tensor(t1[:, 0:n], xs[:, 0:n], S.cos[:, 0:n], ALU.mult), R=[xs, S.cos], W=[t1])
        K.op(K.dve, lambda e: e.tensor_tensor(t2[:, 0:n], rp[:, 0:n], S.sin[:, 0:n], ALU.mult), R=[rp, S.sin], W=[t2])
        K.op(K.dve, lambda e: e.tensor_tensor(st[:, 0:n], t1[:, 0:n], t2[:, 0:n], ALU.add), R=[t1, t2], W=[st])
        K.dma(K.sp, dst[g["b0"] + b, :, S.tok0:S.tok0 + n], st[:, 0:n], R=[st])
    return f


def phase_inproj_attn(C, K, l):
    d = C.d
    e = l // 2
    w = d["ab_w_in"][e]
    qs = 128 ** -0.5
    groups = []
    for i in range(4):
        groups.append(dict(kind="FM", w=w[:, i * 512:(i + 1) * 512], ncols=512, b0=4 * i, evac=ev_fm_plain(d["QaT"], qs)))
    for i in range(4):
        groups.append(dict(kind="FM", w=w[:, 2048 + i * 512:2048 + (i + 1) * 512], ncols=512, b0=4 * i, evac=ev_fm_plain(d["KaT"])))
    for i in range(4):
        groups.append(dict(kind="TM", w=w[:, 4096 + i * 512:4096 + (i + 1) * 512], ncols=512, c0=512 * i, evac=ev_tm_plain(d["Va"])))
    for i in range(4):
        groups.append(dict(kind="FM", w=w[:, 6144 + i * 512:6144 + (i + 1) * 512], ncols=512, b0=4 * i, evac=ev_fm_rope(d["QbT"], qs)))
    groups.append(dict(kind="FM", w=w[:, 8192:8704], ncols=512, b0=0, evac=ev_fm_rope(d["KbT"], None)))
    groups.append(dict(kind="TM", w=w[:, 8704:9216], ncols=512, c0=0, evac=ev_tm_plain(d["Vb"])))

    def setup(S):
        S.perm = K.sbuf("perm", [128, 128], F32)
        K.dma(K.sp, S.perm[:], d["c_perm"][:, :], W=[S.perm])
        S.cos = K.sbuf("cos", [128, 512], F32)
        S.sin = K.sbuf("sin", [128, 512], F32)

        def tile_setup(S):
            if not S.is_ctx:
                p0 = S.tok0 - NCTX
                K.dma(K.sp, S.cos[:], d["c_cos"][:, p0:p0 + 512], W=[S.cos])
                K.dma(K.sp, S.sin[:], d["c_sin"][:, p0:p0 + 512], W=[S.sin])
        S.tile_setup = tile_setup
    phase_inproj(C, K, l, groups, setup)


def host_consts():
    c = {}
    c["c_ident"] = np.eye(128, dtype=np.float32)
    perm = np.zeros((128, 128), np.float32)
    for m in range(128):
        p = m + 32 if (m % 64) < 32 else m - 32
        perm[p, m] = 1.0
    c["c_perm"] = perm
    inv = (np.float32(10000.0) ** (-np.arange(32, dtype=np.float32) / np.float32(32))).astype(np.float32)
    t = np.arange(NLAT)
    rowp = (t // 64).astype(np.float32)
    colp = (t % 64).astype(np.float32)
    cos = np.zeros((128, NLAT), np.float32)
    sin = np.zeros((128, NLAT), np.float32)
    for dd in range(128):
        pos = rowp if dd < 64 else colp
        ang = (pos * inv[dd % 32]).astype(np.float32)
        cos[dd] = np.cos(ang)
        sin[dd] = np.sin(ang) * (-1.0 if (dd % 64) < 32 else 1.0)
    c["c_cos"], c["c_sin"] = cos, sin
    return c


def na_key_tiles(j):
    if j <= 1:
        kps = [0, 1, 2, 3]
        base = 5 + 4 * j
        idx = [base + i for i in range(4)]
    elif j >= 30:
        kps = [28, 29, 30, 31]
        base = 13 + 4 * (j - 30)
        idx = [base + i for i in range(4)]
    else:
        kps = [j - 2, j - 1, j, j + 1, j + 2]
        idx = [0, 1, 2, 3, 4]
    return kps, idx


def phase_na(C, K, l, need_ctx):
    nc, d, cm = C.nc, C.d, C.cm
    e = l // 2
    K.begin_phase()
    QT = [K.sbuf(f"QT{i}", [128, NTOK], BF16) for i in range(2)]
    KT = [K.sbuf(f"KT{i}", [128, NTOK], BF16) for i in range(2)]
    V = [K.sbuf(f"V{i}", [128, 34, 128], BF16) for i in range(2)]
    BM = [K.sbuf(f"BM{i}", [128, 21, 128], F32) for i in range(2)]
    OS = [K.sbuf(f"OS{i}", [128, NTOK], BF16, partial=True) for i in range(2)]
    sA = [K.psum(f"sA{i}", [128, 512], F32) for i in range(2)]
    sB = [K.psum(f"sB{i}", [128, 512], F32) for i in range(2)]
    po = [K.psum(f"po{i}", [128, 256], F32) for i in range(2)]
    pT = [K.sbuf(f"pT{i}", [128, 896], BF16) for i in range(2)]
    rc = [K.sbuf(f"rc{i}", [128, 128], F32) for i in range(2)]
    blocks = [("lat", j) for j in range(32)] + ([("ctx", 0), ("ctx", 1)] if need_ctx else [])
    it = 0
    for h in range(16):
        qt, kt, v, bm, osb = QT[h % 2], KT[h % 2], V[h % 2], BM[h % 2], OS[h % 2]
        K.dma(K.sp, qt[:], d["QaT"][h], W=[qt])
        K.dma(K.sp, kt[:], d["KaT"][h], W=[kt])
        K.dma(K.sp, v[:], d["Va"].rearrange("(t p) c -> p t c", p=128)[:, :, h * 128:(h + 1) * 128], W=[v])
        K.dma(K.sp, bm[:], d["c_bm"][e, h], W=[bm])

        def qk(blk, i):
            kind, j = blk
            if kind == "lat":
                kps, idx = na_key_tiles(j)
                tiles = [(2 + kp, ix) for kp, ix in zip(kps, idx)] + [(0, None), (1, None)]
                q0 = NCTX + 128 * j
            else:
                tiles = [(0, None), (1, None)]
                q0 = 128 * j
            for s, (t, ix) in enumerate(tiles):
                bank = sA[i % 2] if s < 4 else sB[i % 2]
                o = bank[:, (s % 4) * 128:(s % 4 + 1) * 128]
                mm(K, bank, o, kt[:, t * 128:(t + 1) * 128], qt[:, q0:q0 + 128], True, ix is None, R=[kt, qt])
                if ix is not None:
                    mm(K, bank, o, bm[:, ix, :], cm.ident_f[:], False, True, R=[bm, cm.ident_f])
            return tiles, q0

        def pv(blk, i, tiles, q0):
            n = len(tiles)
            p = pT[i % 2]
            na = min(n, 4)
            K.op(K.act, lambda e: e.activation(p[:, 0:na * 128], sA[i % 2][:, 0:na * 128], AF.Exp), R=[sA[i % 2]], W=[p])
            if n > 4:
                K.op(K.act, lambda e: e.activation(p[:, 512:n * 128], sB[i % 2][:, 0:(n - 4) * 128], AF.Exp), R=[sB[i % 2]], W=[p])
            pq = po[i % 2]
            for s, (t, ix) in enumerate(tiles):
                mm(K, pq, pq[:, 0:128], v[:, t, :], p[:, s * 128:(s + 1) * 128], s == 0, s == n - 1, R=[v, p])
            for s, (t, ix) in enumerate(tiles):
                mm(K, pq, pq[:, 128:256], cm.ones_b[:], p[:, s * 128:(s + 1) * 128], s == 0, s == n - 1, R=[cm.ones_b, p])
            r = rc[i % 2]
            K.op(K.dve, lambda e: e.reciprocal(r[:], pq[:, 128:256]), R=[pq], W=[r])
            K.op(K.dve, lambda e: e.tensor_tensor(osb[:, q0:q0 + 128], pq[:, 0:128], r[:], ALU.mult), R=[pq, r], W=[osb])

        prev = None
        for blk in blocks:
            cur = (blk, it) + qk(blk, it)
            it += 1
            if prev is not None:
                pv(*prev)
            prev = cur
        pv(*prev)
        K.dma(K.sp, d["OT"][h], osb[:], R=[osb])
    K.end_phase()


def phase_win(C, K, l, need_ctx):
    nc, d, cm = C.nc, C.d, C.cm
    e = l // 2
    K.begin_phase()
    QT = [K.sbuf(f"QT{i}", [128, 4, NTOK], BF16) for i in range(2)]
    KT = [K.sbuf(f"KT{i}", [128, NTOK], BF16) for i in range(2)]
    V = [K.sbuf(f"V{i}", [128, 34, 128], BF16) for i in range(2)]
    OS = K.sbuf("OS", [128, 4, NTOK], BF16, partial=True)
    wm = K.sbuf("wm", [128, 2, 128], F32)
    id4 = K.sbuf("id4", [128, 4, 128], F32)
    K.dma(K.sp, wm[:], d["c_wmask"][:, :, :], W=[wm])
    for i in range(4):
        K.dma(K.sp, id4[:, i, :], d["c_ident"][:, :], W=[id4])
    sk = K.sbuf("sk", [1, 16], F32)
    esr = K.sbuf("esr", [1, 16, 128], F32)
    K.dma(K.sp, sk[:], d["win_sink"][e:e + 1, :], W=[sk])
    K.op(K.act, lambda e_: e_.activation(sk[:], sk[:], AF.Exp), R=[sk], W=[sk])
    for h in range(16):
        K.op(K.dve, lambda e_, h=h: e_.tensor_scalar(esr[0:1, h, :], cm.ones_f[0:1, 0:128], sk[0:1, h:h + 1], None, ALU.mult), R=[sk, cm.ones_f], W=[esr])
    sb = [K.psum(f"sb{i}", [128, 512], F32) for i in range(5)]
    po = K.psum("po", [128, 512], F32)
    pm = K.psum("pm", [128, 512], F32)
    pT = [K.sbuf(f"pT{i}", [128, 5, 512], BF16) for i in range(2)]
    rc = K.sbuf("rc", [128, 512], F32)
    blocks = [("lat", j) for j in range(32)] + ([("ctx", 0), ("ctx", 1)] if need_ctx else [])
    it = 0
    for g in range(4):
        qt, kt, v = QT[g % 2], KT[g % 2], V[g % 2]
        K.dma(K.sp, qt[:], d["QbT"][4 * g:4 * g + 4].rearrange("h p t -> p h t"), W=[qt])
        K.dma(K.sp, kt[:], d["KbT"][g], W=[kt])
        K.dma(K.sp, v[:], d["Vb"].rearrange("(t p) c -> p t c", p=128)[:, :, g * 128:(g + 1) * 128], W=[v])
        for blk in blocks:
            kind, j = blk
            if kind == "lat":
                tiles = []
                if j > 0:
                    tiles.append((2 + j - 1, 0))
                tiles.append((2 + j, None))
                if j < 31:
                    tiles.append((2 + j + 1, 1))
                tiles += [(0, None), (1, None)]
                q0 = NCTX + 128 * j
            else:
                tiles = [(0, None), (1, None)]
                q0 = 128 * j
            n = len(tiles)
            p = pT[it % 2]
            it += 1
            for s, (t, mi) in enumerate(tiles):
                bank = sb[s]
                mm(K, bank, bank[:], kt[:, t * 128:(t + 1) * 128], qt[:, :, q0:q0 + 128], True, mi is None, R=[kt, qt])
                if mi is not None:
                    mm(K, bank, bank[:], wm[:, mi, :], id4[:], False, True, R=[wm, id4])
                K.op(K.act, lambda e_, s=s, bank=bank: e_.activation(p[:, s, :], bank[:], AF.Exp), R=[bank], W=[p])
            for s, (t, mi) in enumerate(tiles):
                mm(K, po, po[:], v[:, t, :], p[:, s, :], s == 0, s == n - 1, R=[v, p])
            for s, (t, mi) in enumerate(tiles):
                mm(K, pm, pm[:], cm.ones_b[:], p[:, s, :], s == 0, False, R=[cm.ones_b, p])
            mm(K, pm, pm[:], cm.ones_f[0:1, 0:128], esr[0:1, 4 * g:4 * g + 4, :], False, True, R=[cm.ones_f, esr])
            K.op(K.dve, lambda e_: e_.reciprocal(rc[:], pm[:]), R=[pm], W=[rc])
            K.op(K.dve, lambda e_: e_.tensor_tensor(OS[:, :, q0:q0 + 128], po[:].rearrange("p (h q) -> p h q", h=4), rc[:].rearrange("p (h q) -> p h q", h=4), ALU.mult),
                 R=[po, rc], W=[OS])
        for hh in range(4):
            K.dma(K.sp, d["OT"][16 + 4 * g + hh], OS[:, hh, :], R=[OS])
    K.end_phase()


def phase_outproj(C, K, l, w_out, need_ctx):
    nc, d, cm = C.nc, C.d, C.cm
    K.begin_phase()
    wbs = [K.sbuf(f"wb{i}", [128, KC, 512], BF16) for i in range(2)]
    ots = [K.sbuf(f"ot{i}", [128, KC, 512], BF16) for i in range(2)]
    gb = [K.sbuf(f"gb{i}", [128, 2, 512], F32) for i in range(2)]
    xin = [K.sbuf(f"xin{i}", [128, 512], F32) for i in range(3)]
    tt_ = [K.sbuf(f"tt{i}", [128, 512], F32) for i in range(3)]
    acc = [K.psum(f"acc{i}", [128, 512], F32) for i in range(4)]
    tiles = TOK_TILES if need_ctx else TOK_TILES[1:]
    oi = 0
    xi = 0
    for g in range(8):
        cs = slice(g * 512, (g + 1) * 512)
        wb, gbt = wbs[g % 2], gb[g % 2]
        K.dma(K.pool, wb[:], w_out[:, cs].rearrange("(c p) n -> p c n", p=128), W=[wb])
        for r in range(2):
            K.dma(K.sp, gbt[:, r, :], d["mod"][l][r:r + 1, 2 * D + g * 512:2 * D + (g + 1) * 512].broadcast_to([128, 512]), W=[gbt])
        for (tok0, ntok) in tiles:
            ot = ots[oi % 2]
            oi += 1
            row = 1 if tok0 == 0 else 0
            K.dma(K.sp, ot[:, :, 0:ntok], d["OT"][:, :, tok0:tok0 + ntok].rearrange("k p t -> p k t"), W=[ot])
            for sb in range(ntok // 128):
                ps = acc[xi % 4]
                xt, t = xin[xi % 3], tt_[xi % 3]
                xi += 1
                r0 = tok0 + sb * 128
                K.dma(K.sp, xt[:], d["xl"][r0:r0 + 128, cs], W=[xt])
                for kc in range(KC):
                    mm(K, ps, ps[:], ot[:, kc, sb * 128:(sb + 1) * 128], wb[:, kc, :], kc == 0, kc == KC - 1, R=[ot, wb])
                K.op(K.dve, lambda e_, t=t, ps=ps: e_.tensor_tensor(t[:], ps[:], gbt[:, row, :], ALU.mult), R=[ps, gbt], W=[t])
                K.op(K.dve, lambda e_, t=t, xt=xt: e_.tensor_tensor(t[:], t[:], xt[:], ALU.add), R=[t, xt], W=[t])
                K.dma(K.act, d["xm"][r0:r0 + 128, cs], t[:], R=[t])
    K.end_phase()


def host_bm(rpb):
    q = np.arange(128)
    key = np.arange(128)
    mats = [(10, 10 + off) for off in (-2, -1, 0, 1, 2)]
    for j in (0, 1):
        mats += [(j, kp) for kp in (0, 1, 2, 3)]
    for j in (30, 31):
        mats += [(j, kp) for kp in (28, 29, 30, 31)]
    out = np.full((2, 16, 128, 21, 128), NEG, np.float32)
    for m, (j, kp) in enumerate(mats):
        rq = (2 * j + q // 64)[:, None]
        cq = (q % 64)[:, None]
        rk = (2 * kp + key // 64)[None, :]
        ck = (key % 64)[None, :]
        rs = np.clip(rq - 4, 0, 56)
        cs = np.clip(cq - 8, 0, 48)
        valid = (rk >= rs) & (rk <= rs + 7) & (ck >= cs) & (ck <= cs + 15)
        dr = np.clip(rk - rq + 7, 0, 14)
        dc = np.clip(ck - cq + 15, 0, 30)
        g = rpb[:, :, dr, dc]
        out[:, :, :, m, :] = np.where(valid[None, None], g, np.float32(NEG))
    return out


def host_wmask():
    q = np.arange(128)[:, None]
    k = np.arange(128)[None, :]
    wm = np.zeros((128, 2, 128), np.float32)
    wm[:, 0, :] = np.where(q <= k, 0.0, NEG)
    wm[:, 1, :] = np.where(k <= q, 0.0, NEG)
    return wm


def phase_router(C, K, l, need_ctx, n_iter=34):
    nc, d, cm = C.nc, C.d, C.cm
    K.begin_phase()
    nb = NormBufs(K)
    vs = K.sbuf("vs", [32, 5, 128], F32)
    tmpv = K.sbuf("tmpv", [128, 5, 32], F32)
    gsh = K.sbuf("gsh", [128, 4, 32], F32)
    acc = [K.psum(f"acc{i}", [128, 512], F32) for i in range(2)]
    build_gsh(C, K, l, 2, gsh, vs, acc[0], tmpv)
    hTs = [K.sbuf(f"hT{i}", [128, KC, 512], BF16, partial=True) for i in range(2)]
    wr = K.sbuf("wr", [128, KC, 16], BF16)
    K.dma(K.pool, wr[:], d["moe_router"][l].rearrange("(p c) e -> p c e", c=KC), W=[wr])
    E = K.sbuf("E", [16, NTOK], F32, partial=True)
    A = K.sbuf("A", [16, NTOK], F32, partial=True)
    junk = K.sbuf("junk", [16, NLAT], F32)
    tiles = TOK_TILES if need_ctx else TOK_TILES[1:]
    for ti, (tok0, ntok) in enumerate(tiles):
        hT = hTs[ti % 2]
        row = 1 if tok0 == 0 else 0
        for sb in range(ntok // 128):
            norm_block(C, K, nb, d["xm"][tok0 + sb * 128: tok0 + (sb + 1) * 128, :], gsh, row, hT, sb * 128)
        ps = acc[ti % 2]
        for kc in range(KC):
            mm(K, ps, ps[0:16, 0:ntok], wr[:, kc, :], hT[:, kc, 0:ntok], kc == 0, kc == KC - 1, R=[wr, hT])
        K.op(K.act, lambda e: e.activation(E[:, tok0:tok0 + ntok], ps[0:16, 0:ntok], AF.Exp), R=[ps], W=[E])
    for ti, (tok0, ntok) in enumerate(tiles):
        ps = acc[ti % 2]
        mm(K, ps, ps[0:16, 0:ntok], cm.ones_f[0:16, 0:16], E[:, tok0:tok0 + ntok], True, True, R=[cm.ones_f, E])
        K.op(K.dve, lambda e: e.reciprocal(A[:, tok0:tok0 + ntok], ps[0:16, 0:ntok]), R=[ps], W=[A])
        K.op(K.dve, lambda e: e.tensor_tensor(A[:, tok0:tok0 + ntok], A[:, tok0:tok0 + ntok], E[:, tok0:tok0 + ntok], ALU.mult), R=[A, E], W=[A])
    sets = ([(0, NCTX, 32)] if need_ctx else []) + [(NCTX, NLAT, 512)]
    G = K.sbuf("G", [16, NTOK], F32, partial=True)
    for si, (c0, n, cap) in enumerate(sets):
        st = K.sbuf(f"bis{si}", [16, 8], F32)
        lo, hi, mid, cnt, ge, nge, t1, t2 = [st[:, i:i + 1] for i in range(8)]
        K.op(K.dve, lambda e: e.memset(st[:], 0.0), W=[st])
        K.op(K.dve, lambda e: e.memset(hi, 2.0), R=[st], W=[st])
        Av = A[:, c0:c0 + n]
        for it in range(n_iter):
            K.op(K.dve, lambda e: e.tensor_scalar(mid, lo, hi, 0.5, ALU.add, ALU.mult), R=[st], W=[st])
            K.op(K.dve, lambda e: e.tensor_scalar(junk[:, 0:n], Av, mid, 0.0, ALU.is_ge, ALU.add, accum_out=cnt), R=[A, st], W=[junk, st])
            K.op(K.dve, lambda e: e.tensor_scalar(ge, cnt, float(cap) - 0.5, None, ALU.is_ge), R=[st], W=[st])
            K.op(K.dve, lambda e: e.tensor_scalar(nge, ge, -1.0, 1.0, ALU.mult, ALU.add), R=[st], W=[st])
            K.op(K.dve, lambda e: e.tensor_tensor(t1, mid, ge, ALU.mult), R=[st], W=[st])
            K.op(K.dve, lambda e: e.tensor_tensor(t2, mid, nge, ALU.mult), R=[st], W=[st])
            K.op(K.dve, lambda e: e.scalar_tensor_tensor(lo, lo, nge, t1, ALU.mult, ALU.add), R=[st], W=[st])
            K.op(K.dve, lambda e: e.scalar_tensor_tensor(hi, hi, ge, t2, ALU.mult, ALU.add), R=[st], W=[st])
        K.op(K.dve, lambda e: e.scalar_tensor_tensor(G[:, c0:c0 + n], Av, lo, Av, ALU.is_ge, ALU.mult), R=[A, st], W=[G])
    t0 = 0 if need_ctx else NCTX
    K.dma(K.sp, d["gm"][:, t0:NTOK], G[:, t0:NTOK], R=[G])
    K.end_phase()


def phase_moe(C, K, l, need_ctx):
    nc, d, cm = C.nc, C.d, C.cm
    K.begin_phase()
    nb = NormBufs(K)
    vs = K.sbuf("vs", [32, 5, 128], F32)
    tmpv = K.sbuf("tmpv", [128, 5, 32], F32)
    gsh = K.sbuf("gsh", [128, 4, 32], F32)
    pa = [K.psum(f"pa{i}", [128, 512], F32) for i in range(2)]
    pu = [K.psum(f"pu{i}", [128, 512], F32) for i in range(2)]
    py = [K.psum(f"py{i}", [128, 512], F32) for i in range(2)]
    build_gsh(C, K, l, 2, gsh, vs, pa[0], tmpv)
    hT = K.sbuf("hT", [128, KC, 512], BF16, partial=True)
    aT = K.sbuf("aT", [128, 32, 512], BF16, partial=True)
    wp = [K.sbuf(f"wp{i}", [128, 2, KC, 256], BF16, partial=True) for i in range(2)]
    g2 = K.sbuf("g2", [128, D], F32)
    gmb = [K.sbuf(f"gmb{i}", [128, 512], F32) for i in range(2)]
    sl = [K.sbuf(f"sl{i}", [128, 512], F32) for i in range(2)]
    tl = [K.sbuf(f"tl{i}", [128, 512], F32) for i in range(2)]
    xin = [K.sbuf(f"xin{i}", [128, 512], F32) for i in range(3)]
    yo = [K.sbuf(f"yo{i}", [128, 512], F32) for i in range(3)]
    w1, w3, w2 = d["moe_w1"][l], d["moe_w3"][l], d["moe_w2"][l]
    w2v = w2.rearrange("e (fb p) n -> p (e fb) n", p=128)
    tiles = TOK_TILES if need_ctx else TOK_TILES[1:]
    wi = 0
    ai = 0
    yi = 0
    for ti, (tok0, ntok) in enumerate(tiles):
        row = 1 if tok0 == 0 else 0
        if ti == 0 or (ti == 1 and need_ctx):
            K.dma(K.sp, g2[:], d["mod"][l][row:row + 1, 5 * D:6 * D].broadcast_to([128, D]), W=[g2])
        for sb in range(ntok // 128):
            norm_block(C, K, nb, d["xm"][tok0 + sb * 128: tok0 + (sb + 1) * 128, :], gsh, row, hT, sb * 128)
        for e in range(16):
            w = wp[wi % 2]
            wi += 1
            K.dma(K.pool, w[:, 0], w1[e].rearrange("(p c) f -> p c f", c=KC), W=[w])
            K.dma(K.pool, w[:, 1], w3[e].rearrange("(p c) f -> p c f", c=KC), W=[w])
            gb = gmb[e % 2]
            K.dma(K.sp, gb[:, 0:ntok], d["gm"][e:e + 1, tok0:tok0 + ntok].broadcast_to([128, ntok]), W=[gb])
            for fb in range(2):
                a, u = pa[ai % 2], pu[ai % 2]
                s_, t = sl[ai % 2], tl[ai % 2]
                ai += 1
                for kc in range(KC):
                    mm(K, a, a[:, 0:ntok], w[:, 0, kc, fb * 128:(fb + 1) * 128], hT[:, kc, 0:ntok], kc == 0, kc == KC - 1, R=[w, hT])
                for kc in range(KC):
                    mm(K, u, u[:, 0:ntok], w[:, 1, kc, fb * 128:(fb + 1) * 128], hT[:, kc, 0:ntok], kc == 0, kc == KC - 1, R=[w, hT])
                K.op(K.act, lambda e_: e_.activation(s_[:, 0:ntok], a[:, 0:ntok], AF.Silu), R=[a], W=[s_])
                K.op(K.dve, lambda e_: e_.tensor_tensor(t[:, 0:ntok], s_[:, 0:ntok], u[:, 0:ntok], ALU.mult), R=[s_, u], W=[t])
                K.op(K.dve, lambda e_: e_.tensor_tensor(aT[:, e * 2 + fb, 0:ntok], t[:, 0:ntok], gb[:, 0:ntok], ALU.mult), R=[t, gb], W=[aT])
        for g in range(8):
            cs = slice(g * 512, (g + 1) * 512)
            w = wp[wi % 2]
            wi += 1
            wv = w[:].rearrange("p a c f -> p (a c f)").rearrange("p (c n) -> p c n", n=512)
            K.dma(K.pool, wv, w2v[:, :, cs], W=[w])
            for sb in range(ntok // 128):
                ps = py[yi % 2]
                xt, y = xin[yi % 3], yo[yi % 3]
                yi += 1
                r0 = tok0 + sb * 128
                K.dma(K.sp, xt[:], d["xm"][r0:r0 + 128, cs], W=[xt])
                for c in range(32):
                    mm(K, ps, ps[:], aT[:, c, sb * 128:(sb + 1) * 128], wv[:, c, :], c == 0, c == 31, R=[aT, w])
                K.op(K.dve, lambda e_: e_.tensor_tensor(y[:], ps[:], g2[:, cs], ALU.mult), R=[ps, g2], W=[y])
                K.op(K.dve, lambda e_: e_.tensor_tensor(y[:], y[:], xt[:], ALU.add), R=[y, xt], W=[y])
                K.dma(K.sp, d["xl"][r0:r0 + 128, cs], y[:], R=[y])
    K.end_phase()


def phase_final(C, K):
    nc, d, cm = C.nc, C.d, C.cm
    K.begin_phase()
    fw = K.sbuf("fw", [128, D], F32)
    K.dma(K.sp, fw[:], d["final_norm_w"][0:1, :].broadcast_to([128, D]), W=[fw])
    xts = [K.sbuf(f"fx{i}", [128, D], F32) for i in range(2)]
    jk = K.sbuf("fj", [128, D], BF16)
    yts = [K.sbuf(f"fy{i}", [128, D], F32) for i in range(2)]
    sss = [K.sbuf(f"fs{i}", [128, 2], F32) for i in range(2)]
    for i in range(NLAT // 128):
        xt, yt, ss = xts[i % 2], yts[i % 2], sss[i % 2]
        K.dma(K.sp, xt[:], d["xl"][NCTX + i * 128:NCTX + (i + 1) * 128, :], W=[xt])
        K.op(K.act, lambda e: e.activation(jk[:], xt[:], AF.Square, accum_out=ss[:, 0:1]), R=[xt], W=[jk, ss])
        K.op(K.act, lambda e: e.activation(ss[:, 1:2], ss[:, 0:1], AF.Sqrt, scale=1.0 / D, bias=cm.eps[:, 0:1]), R=[ss, cm.eps], W=[ss])
        K.op(K.dve, lambda e: e.reciprocal(ss[:, 1:2], ss[:, 1:2]), R=[ss], W=[ss])
        K.op(K.dve, lambda e: e.scalar_tensor_tensor(yt[:], xt[:], ss[:, 1:2], fw[:], ALU.mult, ALU.mult), R=[xt, ss, fw], W=[yt])
        K.dma(K.pool, d["y"][i * 128:(i + 1) * 128, :], yt[:], R=[yt])
    K.end_phase()


def ev_tm_gates(dst):
    def f(S, g, sb, ps):
        K = S.K
        st = S.stgf[S.n % 3]
        S.n += 1
        K.op(K.dve, lambda e: e.tensor_tensor(st[:, 0:32], ps[:, 0:32], S.bg[:], ALU.add), R=[ps, S.bg], W=[st])
        r0 = S.tok0 + sb * 128
        K.dma(K.sp, dst[r0:r0 + 128, :], st[:, 0:32], R=[st])
    return f


def phase_inproj_mlstm(C, K, l):
    d = C.d
    o = l // 2
    w = d["ml_w_in"][o]
    groups = []
    for i in range(4):
        groups.append(dict(kind="FM", w=w[:, i * 512:(i + 1) * 512], ncols=512, b0=4 * i, evac=ev_fm_plain(d["mqT"], 256 ** -0.5)))
    for i in range(4):
        groups.append(dict(kind="FM", w=w[:, 2048 + i * 512:2048 + (i + 1) * 512], ncols=512, b0=4 * i, evac=ev_fm_plain(d["mkT"])))
    for i in range(4):
        groups.append(dict(kind="TM", w=w[:, 2048 + i * 512:2048 + (i + 1) * 512], ncols=512, c0=512 * i, evac=ev_tm_plain(d["mK"])))
    for i in range(8):
        groups.append(dict(kind="TM", w=w[:, 4096 + i * 512:4096 + (i + 1) * 512], ncols=512, c0=512 * i, evac=ev_tm_plain(d["mV"])))
    for i in range(8):
        groups.append(dict(kind="TM", w=w[:, 8192 + i * 512:8192 + (i + 1) * 512], ncols=512, c0=512 * i, evac=ev_tm_plain(d["mO"])))
    groups.append(dict(kind="TM", w=w[:, 12288:12320], ncols=32, c0=0, evac=ev_tm_gates(d["mG"])))

    def setup(S):
        S.bg = K.sbuf("bg", [128, 32], F32)
        K.dma(K.sp, S.bg[:], d["ml_b_gates"][o:o + 1, :].broadcast_to([128, 32]), W=[S.bg])
    phase_inproj(C, K, l, groups, setup)


def phase_mlstm_scan(C, K, l, dr):
    nc, d, cm = C.nc, C.d, C.cm
    K.begin_phase()
    tri = K.sbuf("tri", [64, 4, 64], F32)
    K.dma(K.sp, tri[:], d["c_tri"][:, :, :], W=[tri])
    Cst = [K.sbuf(f"Cst{h}", [128, 2, 512], F32) for h in range(8)]
    Cb = [K.sbuf(f"Cb{h}", [128, 2, 512], BF16) for h in range(8)]
    nst = [K.sbuf(f"nst{h}", [128, 2], F32) for h in range(8)]
    nbf = [K.sbuf(f"nbf{h}", [128, 2], BF16) for h in range(8)]
    for h in range(8):
        K.op(K.dve, lambda e, h=h: e.memset(Cst[h][:], 0.0), W=[Cst[h]])
        K.op(K.pool, lambda e, h=h: e.memset(Cb[h][:], 0.0), W=[Cb[h]])
        K.op(K.dve, lambda e, h=h: e.memset(nst[h][:], 0.0), W=[nst[h]])
        K.op(K.pool, lambda e, h=h: e.memset(nbf[h][:], 0.0), W=[nbf[h]])
    qT4 = [K.sbuf(f"qT4{i}", [128, 16, 256], BF16) for i in range(2)]
    kT4 = [K.sbuf(f"kT4{i}", [128, 16, 256], BF16) for i in range(2)]
    Kt = [K.sbuf(f"Kt{i}", [64, 2048], BF16) for i in range(2)]
    Vt = [K.sbuf(f"Vt{i}", [64, D], BF16) for i in range(2)]
    Gt = [K.sbuf(f"Gt{i}", [64, 32], F32) for i in range(2)]
    hch = [K.sbuf(f"hch{i}", [64, D], F32, partial=True) for i in range(2)]
    gcp = [K.sbuf(f"gcp{i}", [128, 16], F32) for i in range(2)]
    gs = [K.sbuf(f"gs{i}", [128, 6, 8], F32, partial=True) for i in range(2)]
    smalls = [K.psum(f"psm{i}", [128, 512], F32) for i in range(2)]
    pgt = [Buf(K, f"pgt{i}", smalls[i].t[:, 0:16]) for i in range(2)]
    pst = [Buf(K, f"pst{i}", smalls[i].t[0:64, 64:128]) for i in range(2)]
    pd = [Buf(K, f"pd{i}", smalls[i].t[0:64, 128:130]) for i in range(2)]
    pdn = [Buf(K, f"pdn{i}", smalls[i].t[:, 136:138]) for i in range(2)]
    pn = [K.psum(f"pn{i}", [64, 512], F32) for i in range(2)]
    pdc = [K.psum(f"pdc{i}", [128, 512], F32) for i in range(4)]
    Pt = [K.sbuf(f"Pt{i}", [64, 64], BF16) for i in range(2)]
    Kw = [K.sbuf(f"Kw{i}", [64, 256], BF16) for i in range(2)]
    dd = [K.sbuf(f"dd{i}", [64, 2], F32) for i in range(2)]
    ic, fc = (0, 8) if dr == 0 else (16, 24)
    order = list(range(68)) if dr == 0 else [3, 2, 1, 0] + list(range(67, 3, -1))
    dst = d["hf"] if dr == 0 else d["hb"]
    plan = []
    cur_grp = None
    gi = -1
    for ci, ck in enumerate(order):
        grp = ck // 4
        newg = grp != cur_grp
        if newg:
            cur_grp = grp
            gi += 1
        plan.append((ck, grp, newg, qT4[gi % 2], kT4[gi % 2]))

    def issue_loads(ci):
        ck, grp, newg, q4, k4 = plan[ci]
        if newg:
            K.dma(K.sp, q4[:], d["mqT"][:, :, grp * 256:(grp + 1) * 256].rearrange("b p t -> p b t"), W=[q4])
            K.dma(K.sp, k4[:], d["mkT"][:, :, grp * 256:(grp + 1) * 256].rearrange("b p t -> p b t"), W=[k4])
        r0 = ck * 64
        K.dma(K.sp, Kt[ci % 2][:], d["mK"][r0:r0 + 64, :], W=[Kt[ci % 2]])
        K.dma(K.sp, Vt[ci % 2][:], d["mV"][r0:r0 + 64, :], W=[Vt[ci % 2]])
        K.dma(K.sp, Gt[ci % 2][:], d["mG"][r0:r0 + 64, :], W=[Gt[ci % 2]])

    si = 0
    issue_loads(0)
    for ci, ck in enumerate(order):
        _, grp, _, q4, k4 = plan[ci]
        t0 = (ck % 4) * 64
        r0 = ck * 64
        kt, vt, gt, hc, g_ = Kt[ci % 2], Vt[ci % 2], Gt[ci % 2], hch[ci % 2], gs[ci % 2]
        pg = pgt[ci % 2]
        lq, vq, ev, ea, eb, eg = [g_[:, i, :] for i in range(6)]
        K.op(K.act, lambda e: e.activation(lq[0:64], gt[:, fc:fc + 8], AF.Exp, scale=-1.0), R=[gt], W=[g_])
        K.op(K.act, lambda e: e.activation(lq[0:64], lq[0:64], AF.Ln, bias=cm.ones_f[0:64, 0:1]), R=[g_, cm.ones_f], W=[g_])
        mm(K, pg, pg[0:64, 0:8], tri[:, dr, :], lq[0:64], True, True, R=[tri, g_])
        mm(K, pg, pg[:, 8:16], cm.ones_f[0:64, 0:128], lq[0:64], True, True, R=[cm.ones_f, g_])
        K.op(K.dve, lambda e: e.tensor_tensor(vq[0:64], gt[:, ic:ic + 8], pg[0:64, 0:8], ALU.add), R=[gt, pg], W=[g_])
        K.op(K.act, lambda e: e.activation(ev[0:64], vq[0:64], AF.Exp), R=[g_], W=[g_])
        K.op(K.dve, lambda e: e.tensor_tensor(ea[0:64], vq[0:64], pg[0:64, 8:16], ALU.subtract), R=[g_, pg], W=[g_])
        K.op(K.act, lambda e: e.activation(ea[0:64], ea[0:64], AF.Exp), R=[g_], W=[g_])
        K.op(K.act, lambda e: e.activation(eb[0:64], pg[0:64, 0:8], AF.Exp, scale=-1.0), R=[pg], W=[g_])
        K.op(K.act, lambda e: e.activation(eg, pg[:, 8:16], AF.Exp, scale=-1.0), R=[pg], W=[g_])
        if ci + 1 < len(order):
            issue_loads(ci + 1)
        for h in range(8):
            ps_s, ps_n, ps_d, ps_dn = pst[si % 2], pn[si % 2], pd[si % 2], pdn[si % 2]
            pc0, pc1 = pdc[(2 * si) % 4], pdc[(2 * si + 1) % 4]
            pt, kw, dq = Pt[si % 2], Kw[si % 2], dd[si % 2]
            si += 1
            qa = [q4[:, 2 * h + c, t0:t0 + 64] for c in range(2)]
            ka = [k4[:, 2 * h + c, t0:t0 + 64] for c in range(2)]
            vh = vt[:, h * 512:(h + 1) * 512]
            for c in range(2):
                mm(K, ps_s, ps_s[:, :], ka[c], qa[c], c == 0, c == 1, R=[k4, q4])
            K.op(K.dve, lambda e: e.scalar_tensor_tensor(pt[:], ps_s[:, :], ev[0:64, h:h + 1], tri[:, dr, :], ALU.mult, ALU.mult), R=[ps_s, g_, tri], W=[pt])
            for c in range(2):
                mm(K, ps_n, ps_n[:, :], qa[c], Cb[h][:, c, :], c == 0, False, R=[q4, Cb[h]])
            mm(K, ps_n, ps_n[:, :], pt[:], vh, False, True, R=[pt, vt])
            for c in range(2):
                mm(K, ps_d, ps_d[:, 0:1], qa[c], nbf[h][:, c:c + 1], c == 0, False, R=[q4, nbf[h]])
            mm(K, ps_d, ps_d[:, 0:1], pt[:], cm.ones_b[0:64, 0:1], False, True, R=[pt, cm.ones_b])
            K.op(K.dve, lambda e: e.tensor_scalar(dq[:, 0:1], ps_d[:, 0:1], eb[0:64, h:h + 1], None, ALU.mult), R=[ps_d, g_], W=[dq])
            K.op(K.dve, lambda e: e.tensor_scalar(dq[:, 1:2], ps_d[:, 0:1], eb[0:64, h:h + 1], -1.0, ALU.mult, ALU.mult), R=[ps_d, g_], W=[dq])
            K.op(K.dve, lambda e: e.scalar_tensor_tensor(dq[:, 0:1], dq[:, 0:1], 1.0, dq[:, 1:2], ALU.max, ALU.max), R=[dq], W=[dq])
            K.op(K.dve, lambda e: e.reciprocal(dq[:, 0:1], dq[:, 0:1]), R=[dq], W=[dq])
            K.op(K.dve, lambda e: e.tensor_tensor(dq[:, 1:2], dq[:, 0:1], eb[0:64, h:h + 1], ALU.mult), R=[dq, g_], W=[dq])
            K.op(K.act, lambda e: e.activation(hc[:, h * 512:(h + 1) * 512], ps_n[:, :], AF.Copy, scale=dq[:, 1:2]), R=[ps_n, dq], W=[hc])
            K.op(K.pool, lambda e: e.tensor_scalar(kw[:], kt[:, h * 256:(h + 1) * 256], ea[0:64, h:h + 1], None, ALU.mult), R=[kt, g_], W=[kw])
            mm(K, pc0, pc0[:, :], kw[:, 0:128], vh, True, True, R=[kw, vt])
            mm(K, pc1, pc1[:, :], kw[:, 128:256], vh, True, True, R=[kw, vt])
            for c in range(2):
                mm(K, ps_dn, ps_dn[:, c:c + 1], kw[:, c * 128:(c + 1) * 128], cm.ones_b[0:64, 0:1], True, True, R=[kw, cm.ones_b])
            K.op(K.dve, lambda e: e.scalar_tensor_tensor(Cst[h][:, 0, :], Cst[h][:, 0, :], eg[:, h:h + 1], pc0[:, :], ALU.mult, ALU.add), R=[Cst[h], g_, pc0], W=[Cst[h]])
            K.op(K.dve, lambda e: e.scalar_tensor_tensor(Cst[h][:, 1, :], Cst[h][:, 1, :], eg[:, h:h + 1], pc1[:, :], ALU.mult, ALU.add), R=[Cst[h], g_, pc1], W=[Cst[h]])
            K.op(K.act, lambda e: e.copy(Cb[h][:], Cst[h][:]), R=[Cst[h]], W=[Cb[h]])
            K.op(K.dve, lambda e: e.scalar_tensor_tensor(nst[h][:], nst[h][:], eg[:, h:h + 1], ps_dn[:, :], ALU.mult, ALU.add), R=[nst[h], g_, ps_dn], W=[nst[h]])
            K.op(K.pool, lambda e: e.tensor_copy(nbf[h][:], nst[h][:]), R=[nst[h]], W=[nbf[h]])
        K.dma(K.sp, dst[r0:r0 + 64, :], hc[:], R=[hc])
    K.end_phase()


def phase_mlstm_post(C, K, l, need_ctx):
    nc, d, cm = C.nc, C.d, C.cm
    o = l // 2
    K.begin_phase()
    nwb = K.sbuf("nwb", [128, D], F32)
    K.dma(K.sp, nwb[:], d["ml_norm_w"][o:o + 1, :].broadcast_to([128, D]), W=[nwb])
    hf = [K.sbuf(f"hf{i}", [128, D], F32) for i in range(2)]
    hb = [K.sbuf(f"hb{i}", [128, D], F32) for i in range(2)]
    ot = [K.sbuf(f"ot{i}", [128, D], BF16) for i in range(2)]
    sg = K.sbuf("sg", [128, D], F32)
    jk = K.sbuf("jk", [128, 512], BF16)
    hnb = K.sbuf("hnb", [128, D], BF16)
    ss = K.sbuf("ss", [128, 2, 8], F32, partial=True)
    stg = [K.sbuf(f"stg{i}", [128, KC, 512], BF16, partial=True) for i in range(2)]
    tp = [K.psum(f"tp{i}", [128, 1024], BF16) for i in range(2)]
    tiles = TOK_TILES if need_ctx else TOK_TILES[1:]
    bi = 0
    ti_ = 0
    for ti, (tok0, ntok) in enumerate(tiles):
        st = stg[ti % 2]
        for sb in range(ntok // 128):
            r0 = tok0 + sb * 128
            a, b, og = hf[bi % 2], hb[bi % 2], ot[bi % 2]
            bi += 1
            K.dma(K.sp, a[:], d["hf"][r0:r0 + 128, :], W=[a])
            K.dma(K.sp, b[:], d["hb"][r0:r0 + 128, :], W=[b])
            K.dma(K.sp, og[:], d["mO"][r0:r0 + 128, :], W=[og])
            K.op(K.pool, lambda e: e.tensor_tensor(a[:], a[:], b[:], ALU.add), R=[a, b], W=[a])
            for h in range(8):
                K.op(K.act, lambda e, h=h: e.activation(jk[:], a[:, h * 512:(h + 1) * 512], AF.Square, accum_out=ss[:, 0, h:h + 1]), R=[a], W=[jk, ss])
            K.op(K.act, lambda e: e.activation(ss[:, 1, :], ss[:, 0, :], AF.Sqrt, scale=1.0 / 512, bias=cm.eps[:, 0:1]), R=[ss, cm.eps], W=[ss])
            K.op(K.dve, lambda e: e.reciprocal(ss[:, 1, :], ss[:, 1, :]), R=[ss], W=[ss])
            K.op(K.act, lambda e: e.activation(sg[:], og[:], AF.Sigmoid), R=[og], W=[sg])
            for h in range(8):
                hs = slice(h * 512, (h + 1) * 512)
                K.op(K.dve, lambda e, h=h, hs=hs: e.scalar_tensor_tensor(a[:, hs], a[:, hs], ss[:, 1, h:h + 1], nwb[:, hs], ALU.mult, ALU.mult), R=[a, ss, nwb], W=[a])
            K.op(K.pool, lambda e: e.tensor_tensor(hnb[:], a[:], sg[:], ALU.mult), R=[a, sg], W=[hnb])
            for g in range(4):
                t = tp[ti_ % 2]
                ti_ += 1
                for c in range(8):
                    ch = g * 8 + c
                    K.op(K.pe, lambda e, c=c, ch=ch, t=t: e.transpose(t[:, c * 128:(c + 1) * 128], hnb[:, ch * 128:(ch + 1) * 128], cm.ident_b[:]), R=[hnb, cm.ident_b], W=[t])
                o_ap = st[:, g * 8:(g + 1) * 8, sb * 128:(sb + 1) * 128]
                i_ap = t[:, :].rearrange("p (c q) -> p c q", c=8)
                if g % 2 == 0:
                    K.op(K.act, lambda e, o_ap=o_ap, i_ap=i_ap: e.copy(o_ap, i_ap), R=[t], W=[st])
                else:
                    K.op(K.dve, lambda e, o_ap=o_ap, i_ap=i_ap: e.tensor_copy(o_ap, i_ap), R=[t], W=[st])
        K.dma(K.sp, d["OT"][:, :, tok0:tok0 + ntok].rearrange("k p t -> p k t"), st[:, :, 0:ntok], R=[st])
    K.end_phase()


def host_tri():
    s = np.arange(64)[:, None]
    t = np.arange(64)[None, :]
    tri = np.zeros((64, 4, 64), np.float32)
    tri[:, 0, :] = (s <= t)
    tri[:, 1, :] = (s >= t)
    return tri


def build_full():
    C = Prog()
    C.declare()
    nc = C.nc
    k = K(nc)
    d = C.d
    with nc.Block():
        setup_common(C, k)
        phase_mods(C, k, [0, 1, 2, 3])
        for l in range(DEPTH):
            need_ctx = l < DEPTH - 1
            if l % 2 == 0:
                phase_inproj_attn(C, k, l)
                phase_na(C, k, l, need_ctx)
                phase_win(C, k, l, need_ctx)
                phase_outproj(C, k, l, d["ab_w_out"][l // 2], need_ctx)
            else:
                phase_inproj_mlstm(C, k, l)
                phase_mlstm_scan(C, k, l, 0)
                phase_mlstm_scan(C, k, l, 1)
                phase_mlstm_post(C, k, l, need_ctx)
                phase_outproj(C, k, l, d["ml_w_out"][l // 2], need_ctx)
            phase_router(C, k, l, need_ctx)
            phase_moe(C, k, l, need_ctx)
        phase_final(C, k)
    return C, k


def kernel(x, c, ctx, c_ctx, ada_w, ada_b, norm1_w, norm2_w, ab_w_in, ab_w_out, na_rpb, win_sink,
           ml_w_in, ml_b_gates, ml_norm_w, ml_w_out, moe_router, moe_w1, moe_w3, moe_w2, final_norm_w):
    f = lambda a: np.ascontiguousarray(np.asarray(a, dtype=np.float32))
    C, k = build_full()
    hc = host_consts()
    shared = {
        "ada_w": f(ada_w), "ada_b": f(ada_b), "norm1_w": f(norm1_w), "norm2_w": f(norm2_w),
        "ab_w_in": f(ab_w_in), "ab_w_out": f(ab_w_out), "win_sink": f(win_sink),
        "ml_w_in": f(ml_w_in), "ml_b_gates": f(ml_b_gates), "ml_norm_w": f(ml_norm_w), "ml_w_out": f(ml_w_out),
        "moe_router": f(moe_router), "moe_w1": f(moe_w1), "moe_w3": f(moe_w3), "moe_w2": f(moe_w2),
        "final_norm_w": f(final_norm_w).reshape(1, D),
        "c_ident": hc["c_ident"], "c_perm": hc["c_perm"], "c_cos": hc["c_cos"], "c_sin": hc["c_sin"],
        "c_bm": host_bm(f(na_rpb)), "c_wmask": host_wmask(), "c_tri": host_tri(),
    }
    x, c, ctx, c_ctx = f(x), f(c), f(ctx), f(c_ctx)
    B = x.shape[0]
    in_maps = []
    for s in range(B):
        m = dict(shared)
        m["x"] = x[s]
        m["ctx"] = ctx[s]
        m["cvec"] = np.stack([c[s], c_ctx]).astype(np.float32)
        in_maps.append(m)
    res = run_bass_kernel_spmd(C.nc, in_maps, core_ids=list(range(B)))
    return np.stack([res.results[s]["y"] for s in range(B)]).astype(np.float32)
```
